# Optimizing a Trainium2 kernel written in Bass

```python
import math
import jax, jax.numpy as jnp
from jax import lax
import numpy as np

D_MODEL = 2048
BATCH = 2
SEQ = 8192
DEPTH = 2

D_MIX = D_MODEL
D_LRU = D_MIX // 2
LRU_BLOCKS = 8
LRU_BLOCK_W = D_LRU // LRU_BLOCKS
LRU_C = 8.0
CONV_W = 4
D_DN = D_MIX - D_LRU
DN_HEAD_DIM = 128
DN_HEADS = D_DN // DN_HEAD_DIM
DN_CHUNK = 64
D_IN = 2 * D_LRU + 4 * D_DN + 2 * DN_HEADS
D_FF = 3 * D_MODEL
N_EXPERTS = 8
TOP_K = 2
D_FF_EXPERT = 3 * D_MODEL // 2
MOE_BLOCK = 128
N_DENSE = (DEPTH + 1) // 2
N_MOE = DEPTH // 2
EPS = 1e-6

kernel_name = 'hybrid_rglru_gdn_moe'


def rms_norm(x, gain):
    xf = x.astype(jnp.float32)
    y = xf * lax.rsqrt(jnp.mean(xf * xf, axis=-1, keepdims=True) + EPS)
    return (y * gain.astype(jnp.float32)).astype(x.dtype)


def causal_depthwise_conv(x, w):
    k_w = w.shape[0]
    s = x.shape[1]
    xp = jnp.pad(x, ((0, 0), (k_w - 1, 0), (0, 0)))
    y = xp[:, 0:s] * w[0]
    for k in range(1, k_w):
        y = y + xp[:, k:k + s] * w[k]
    return y


def l2_normalize(x):
    return x * lax.rsqrt(jnp.sum(x * x, axis=-1, keepdims=True) + EPS)


def lru_combine(c1, c2):
    a1, b1 = c1
    a2, b2 = c2
    return a1 * a2, a2 * b1 + b2


def rg_lru_group(x_in, gate_in, conv_w, conv_b, w_r, b_r, w_i, b_i, lam, out_norm):
    bsz, s, _ = x_in.shape
    xc = (causal_depthwise_conv(x_in, conv_w) + conv_b).astype(jnp.float32)
    xb = xc.reshape(bsz, s, LRU_BLOCKS, LRU_BLOCK_W)
    r = jax.nn.sigmoid(jnp.einsum('bsnc,ncd->bsnd', xb, w_r.astype(jnp.float32)) + b_r.astype(jnp.float32))
    i = jax.nn.sigmoid(jnp.einsum('bsnc,ncd->bsnd', xb, w_i.astype(jnp.float32)) + b_i.astype(jnp.float32))
    r = r.reshape(bsz, s, D_LRU)
    i = i.reshape(bsz, s, D_LRU)
    log_a = -LRU_C * r * jax.nn.softplus(-lam.astype(jnp.float32))
    a = jnp.exp(log_a)
    mult = jnp.sqrt(-jnp.expm1(2.0 * log_a))
    b = mult * (i * xc)
    _, h = lax.associative_scan(lru_combine, (a, b), axis=1)
    y = h * jax.nn.gelu(gate_in.astype(jnp.float32), approximate=True)
    return rms_norm(y, out_norm)


def chunk_gated_delta_rule(q, k, v, g, beta):
    bsz, nh, s, dk = q.shape
    dv = v.shape[-1]
    c = DN_CHUNK
    n = s // c
    q = q.reshape(bsz, nh, n, c, dk)
    k = k.reshape(bsz, nh, n, c, dk)
    v = v.reshape(bsz, nh, n, c, dv)
    g = jnp.cumsum(g.reshape(bsz, nh, n, c), axis=-1)
    beta = beta.reshape(bsz, nh, n, c)
    causal = jnp.tril(jnp.ones((c, c), dtype=bool))
    strict = jnp.tril(jnp.ones((c, c), dtype=bool), -1)
    diff = g[..., :, None] - g[..., None, :]
    decay = jnp.where(causal, jnp.exp(jnp.where(causal, diff, 0.0)), 0.0)
    k_beta = k * beta[..., None]
    v_beta = v * beta[..., None]
    lower = jnp.where(strict, jnp.einsum('bhncd,bhnmd->bhncm', k_beta, k) * decay, 0.0)
    a_mat = lower + jnp.eye(c, dtype=jnp.float32)
    rhs = jnp.concatenate([v_beta, k_beta * jnp.exp(g)[..., None]], axis=-1)
    sol = lax.linalg.triangular_solve(a_mat, rhs, left_side=True, lower=True, unit_diagonal=True)
    u = sol[..., :dv]
    w = sol[..., dv:]
    qk = jnp.where(causal, jnp.einsum('bhncd,bhnmd->bhncm', q, k) * decay, 0.0)
    q_dec = q * jnp.exp(g)[..., None]
    k_dec = k * jnp.exp(g[..., -1:] - g)[..., None]
    g_last = jnp.exp(g[..., -1])

    def step(state, inp):
        q_i, k_i, u_i, w_i, qk_i, gl_i = inp
        v_new = u_i - jnp.einsum('bhck,bhkv->bhcv', w_i, state)
        o = jnp.einsum('bhck,bhkv->bhcv', q_i, state) + jnp.einsum('bhcm,bhmv->bhcv', qk_i, v_new)
        state = state * gl_i[..., None, None] + jnp.einsum('bhck,bhcv->bhkv', k_i, v_new)
        return state, o

    xs = tuple(jnp.moveaxis(t, 2, 0) for t in (q_dec, k_dec, u, w, qk, g_last))
    state0 = jnp.zeros((bsz, nh, dk, dv), jnp.float32)
    _, o = lax.scan(step, state0, xs)
    return jnp.moveaxis(o, 0, 2).reshape(bsz, nh, s, dv)


def gated_deltanet_group(q_in, k_in, v_in, gate_in, beta_in, alpha_in, conv_w, a_log, dt_bias, out_norm):
    bsz, s, _ = q_in.shape
    qkv = jnp.concatenate([q_in, k_in, v_in], axis=-1)
    qkv = jax.nn.silu(causal_depthwise_conv(qkv, conv_w)).astype(jnp.float32)
    q, k, v = jnp.split(qkv, 3, axis=-1)
    to_heads = lambda t: t.reshape(bsz, s, DN_HEADS, DN_HEAD_DIM).transpose(0, 2, 1, 3)
    q = l2_normalize(to_heads(q)) * (DN_HEAD_DIM ** -0.5)
    k = l2_normalize(to_heads(k))
    v = to_heads(v)
    beta = jax.nn.sigmoid(beta_in.astype(jnp.float32)).transpose(0, 2, 1)
    g = (-jnp.exp(a_log.astype(jnp.float32))
         * jax.nn.softplus(alpha_in.astype(jnp.float32) + dt_bias.astype(jnp.float32))).transpose(0, 2, 1)
    o = chunk_gated_delta_rule(q, k, v, g, beta).transpose(0, 2, 1, 3)
    gate = gate_in.astype(jnp.float32).reshape(bsz, s, DN_HEADS, DN_HEAD_DIM)
    o = rms_norm(o, out_norm) * jax.nn.silu(gate)
    return o.reshape(bsz, s, D_DN)


def swiglu(h, w_gate, w_up, w_down):
    return (jax.nn.silu(h @ w_gate) * (h @ w_up)) @ w_down


def moe_swiglu(h, router, w_gate, w_up, w_down):
    bsz, s, d = h.shape
    t = bsz * s
    xt = h.reshape(t, d)
    logits = (xt @ router).astype(jnp.float32)
    top_logits, top_e = lax.top_k(logits, TOP_K)
    top_w = jax.nn.softmax(top_logits, axis=-1)
    n_assign = t * TOP_K
    flat_e = top_e.reshape(n_assign).astype(jnp.int32)
    flat_tok = jnp.repeat(jnp.arange(t, dtype=jnp.int32), TOP_K)
    flat_w = top_w.reshape(n_assign)
    order = jnp.argsort(flat_e)
    sorted_e = flat_e[order]
    sorted_tok = flat_tok[order]
    sorted_w = flat_w[order]
    counts = jnp.zeros((N_EXPERTS,), jnp.int32).at[flat_e].add(1)
    padded = (counts + MOE_BLOCK - 1) // MOE_BLOCK * MOE_BLOCK
    pad_end = jnp.cumsum(padded)
    pad_start = pad_end - padded
    start = jnp.cumsum(counts) - counts
    rank = jnp.arange(n_assign, dtype=jnp.int32) - start[sorted_e]
    dest = pad_start[sorted_e] + rank
    n_slots = (n_assign + MOE_BLOCK - 1) // MOE_BLOCK * MOE_BLOCK + N_EXPERTS * MOE_BLOCK
    n_blocks = n_slots // MOE_BLOCK
    slot_tok = jnp.zeros((n_slots,), jnp.int32).at[dest].set(sorted_tok)
    block_expert = jnp.minimum(
        jnp.searchsorted(pad_end, jnp.arange(n_blocks, dtype=jnp.int32) * MOE_BLOCK, side='right'),
        N_EXPERTS - 1).astype(jnp.int32)
    x_blocks = xt[slot_tok].reshape(n_blocks, MOE_BLOCK, d)

    def expert_fn(args):
        xb, e = args
        return swiglu(xb, w_gate[e], w_up[e], w_down[e])

    y_blocks = lax.map(expert_fn, (x_blocks, block_expert))
    y_assign = y_blocks.reshape(n_slots, d)[dest] * sorted_w[:, None].astype(h.dtype)
    out = jnp.zeros((t, d), h.dtype).at[sorted_tok].add(y_assign)
    return out.reshape(bsz, s, d)


def setup_inputs(seed: int = 0) -> dict:
    key = jax.random.key(seed)
    ks = jax.random.split(key, 24)
    nrm = lambda k, shape, scale: jax.random.normal(k, shape, jnp.float32) * scale
    lam_u = jax.random.uniform(ks[9], (DEPTH, D_LRU), jnp.float32, 0.9, 0.999)
    lam_p = lam_u ** (1.0 / LRU_C)
    dt = jnp.exp(jax.random.uniform(ks[13], (DEPTH, DN_HEADS), jnp.float32, math.log(1e-3), math.log(1e-1)))
    return {
        'x': jax.random.normal(ks[0], (BATCH, SEQ, D_MODEL), jnp.float32),
        'norm_mix': 1.0 + nrm(ks[1], (DEPTH, D_MODEL), 0.02),
        'w_in': nrm(ks[2], (DEPTH, D_MODEL, D_IN), D_MODEL ** -0.5),
        'conv_lru_w': nrm(ks[3], (DEPTH, CONV_W, D_LRU), CONV_W ** -0.5),
        'conv_lru_b': nrm(ks[4], (DEPTH, D_LRU), 0.02),
        'lru_w_r': nrm(ks[5], (DEPTH, LRU_BLOCKS, LRU_BLOCK_W, LRU_BLOCK_W), LRU_BLOCK_W ** -0.5),
        'lru_b_r': nrm(ks[6], (DEPTH, LRU_BLOCKS, LRU_BLOCK_W), 0.02),
        'lru_w_i': nrm(ks[7], (DEPTH, LRU_BLOCKS, LRU_BLOCK_W, LRU_BLOCK_W), LRU_BLOCK_W ** -0.5),
        'lru_b_i': nrm(ks[8], (DEPTH, LRU_BLOCKS, LRU_BLOCK_W), 0.02),
        'lru_lambda': jnp.log(lam_p) - jnp.log1p(-lam_p),
        'lru_out_norm': 1.0 + nrm(ks[10], (DEPTH, D_LRU), 0.02),
        'conv_qkv_w': nrm(ks[11], (DEPTH, CONV_W, 3 * D_DN), CONV_W ** -0.5),
        'dn_a_log': jnp.log(jax.random.uniform(ks[12], (DEPTH, DN_HEADS), jnp.float32, 1.0, 16.0)),
        'dn_dt_bias': dt + jnp.log(-jnp.expm1(-dt)),
        'dn_out_norm': 1.0 + nrm(ks[14], (DEPTH, DN_HEAD_DIM), 0.02),
        'w_out': nrm(ks[15], (DEPTH, D_MIX, D_MODEL), D_MIX ** -0.5),
        'norm_ffn': 1.0 + nrm(ks[16], (DEPTH, D_MODEL), 0.02),
        'ffn_w_gate': nrm(ks[17], (N_DENSE, D_MODEL, D_FF), D_MODEL ** -0.5),
        'ffn_w_up': nrm(ks[18], (N_DENSE, D_MODEL, D_FF), D_MODEL ** -0.5),
        'ffn_w_down': nrm(ks[19], (N_DENSE, D_FF, D_MODEL), D_FF ** -0.5),
        'moe_router': nrm(ks[20], (N_MOE, D_MODEL, N_EXPERTS), D_MODEL ** -0.5),
        'moe_w_gate': nrm(ks[21], (N_MOE, N_EXPERTS, D_MODEL, D_FF_EXPERT), D_MODEL ** -0.5),
        'moe_w_up': nrm(ks[22], (N_MOE, N_EXPERTS, D_MODEL, D_FF_EXPERT), D_MODEL ** -0.5),
        'moe_w_down': nrm(ks[23], (N_MOE, N_EXPERTS, D_FF_EXPERT, D_MODEL), D_FF_EXPERT ** -0.5),
        'norm_final': 1.0 + nrm(jax.random.fold_in(key, 99), (D_MODEL,), 0.02),
    }


def reference(x, norm_mix, w_in, conv_lru_w, conv_lru_b, lru_w_r, lru_b_r, lru_w_i, lru_b_i,
              lru_lambda, lru_out_norm, conv_qkv_w, dn_a_log, dn_dt_bias, dn_out_norm, w_out,
              norm_ffn, ffn_w_gate, ffn_w_up, ffn_w_down, moe_router, moe_w_gate, moe_w_up,
              moe_w_down, norm_final):
    split_points = list(np.cumsum([D_LRU, D_LRU, D_DN, D_DN, D_DN, D_DN, DN_HEADS]))
    for l in range(DEPTH):
        h = rms_norm(x, norm_mix[l])
        proj = h @ w_in[l]
        lru_x, lru_g, dn_q, dn_k, dn_v, dn_g, dn_beta, dn_alpha = jnp.split(proj, split_points, axis=-1)
        y_lru = rg_lru_group(lru_x, lru_g, conv_lru_w[l], conv_lru_b[l], lru_w_r[l], lru_b_r[l],
                             lru_w_i[l], lru_b_i[l], lru_lambda[l], lru_out_norm[l])
        y_dn = gated_deltanet_group(dn_q, dn_k, dn_v, dn_g, dn_beta, dn_alpha, conv_qkv_w[l],
                                    dn_a_log[l], dn_dt_bias[l], dn_out_norm[l])
        y_mix = jnp.concatenate([y_lru.astype(x.dtype), y_dn.astype(x.dtype)], axis=-1)
        x = x + y_mix @ w_out[l]
        h2 = rms_norm(x, norm_ffn[l])
        if l % 2 == 0:
            j = l // 2
            x = x + swiglu(h2, ffn_w_gate[j], ffn_w_up[j], ffn_w_down[j])
        else:
            j = l // 2
            x = x + moe_swiglu(h2, moe_router[j], moe_w_gate[j], moe_w_up[j], moe_w_down[j])
    return rms_norm(x, norm_final)
```

```python
import numpy as np
from contextlib import ExitStack
import concourse.bass as bass
import concourse.mybir as mybir
from concourse.bass_utils import run_bass_kernel_spmd


F32 = mybir.dt.float32
BF16 = mybir.dt.bfloat16
I32 = mybir.dt.int32
AF = mybir.ActivationFunctionType
ALU = mybir.AluOpType
AX = mybir.AxisListType

SEM_ROLL = 30000


class Prog:
    ENGS = ("pe", "dve", "act", "pool", "sp")

    def __init__(self, nc, stack):
        self.nc = nc
        self.stack = stack
        self.q = {e: [] for e in self.ENGS}
        self.cnt = {e: 0 for e in self.ENGS}
        self.sem = {}
        self.nsem = 0
        for e in self.ENGS:
            self.sem[e] = self._newsem("e_" + e)
        self.seen = {e: {} for e in self.ENGS}
        self.lastw = {}
        self.readers = {}
        self.dsem = {}
        self.n_ops = 0

    def _newsem(self, name):
        self.nsem += 1
        sm = self.stack.enter_context(self.nc.semaphore(f"{name}_{self.nsem}"))
        if not hasattr(self, "semname"):
            self.semname = {}
        self.semname[id(sm)] = f"{name}_{self.nsem}"
        return sm

    def sb(self, name, shape, dt):
        return self.stack.enter_context(self.nc.sbuf_tensor(name, list(shape), dt))

    def ps(self, name, shape, dt=F32):
        return self.stack.enter_context(self.nc.psum_tensor(name, list(shape), dt))

    def _deps(self, eng, reads, writes):
        need = []
        for k in reads:
            w = self.lastw.get(k)
            if w is not None:
                need.append(w)
        for k in writes:
            w = self.lastw.get(k)
            if w is not None:
                need.append(w)
            need.extend(self.readers.get(k, ()))
        best = {}
        for s, v in need:
            if best.get(id(s), (None, -1))[1] < v:
                best[id(s)] = (s, v)
        out = []
        seen = self.seen[eng]
        for sid, (s, v) in best.items():
            if eng == "pe" and s is self.sem["pe"]:
                continue
            if seen.get(sid, 0) >= v:
                continue
            seen[sid] = v
            out.append((s, v))
        return out

    def _commit(self, reads, writes, tok):
        for k in writes:
            self.lastw[k] = tok
            self.readers[k] = []
        for k in reads:
            self.readers.setdefault(k, []).append(tok)

    def op(self, eng, fn, reads=(), writes=()):
        if self.cnt[eng] >= SEM_ROLL:
            self.sem[eng] = self._newsem("e_" + eng)
            self.cnt[eng] = 0
        waits = self._deps(eng, reads, writes)
        self.cnt[eng] += 1
        sem = self.sem[eng]
        tok = (sem, self.cnt[eng])
        self.q[eng].append((fn, waits, sem, 1))
        self._commit(reads, writes, tok)
        self.n_ops += 1
        if getattr(self, "log", None) is not None:
            self.log.append((eng, self.cnt[eng], list(reads), list(writes), [(self.semname.get(id(s), "?"), v) for s, v in waits]))

    def dma(self, eng, out, in_, reads=(), writes=(), key=None, **kw):
        assert eng in ("sp", "pool", "act")
        if key is None:
            key = ("dma",) + tuple(writes) + tuple(reads)
        if key not in self.dsem:
            self.dsem[key] = [self._newsem("d"), 0]
        ent = self.dsem[key]
        sem = ent[0]
        waits = self._deps(eng, reads, writes)
        if ent[1] > 0 and self.seen[eng].get(id(sem), 0) < ent[1]:
            self.seen[eng][id(sem)] = ent[1]
            waits.append((sem, ent[1]))
        ent[1] += 16
        tok = (sem, ent[1])

        def fn(e, out=out, in_=in_, kw=kw):
            return e.dma_start(out=out, in_=in_, **kw)
        self.q[eng].append((fn, waits, sem, 16))
        self._commit(reads, writes, tok)
        self.n_ops += 1
        return tok

    def idma(self, out, out_off, in_, in_off, reads=(), writes=(), key=None, bounds=None):
        eng = "pool"
        if key not in self.dsem:
            self.dsem[key] = [self._newsem("d"), 0]
        ent = self.dsem[key]
        sem = ent[0]
        waits = self._deps(eng, reads, writes)
        if ent[1] > 0 and self.seen[eng].get(id(sem), 0) < ent[1]:
            self.seen[eng][id(sem)] = ent[1]
            waits.append((sem, ent[1]))
        ent[1] += 16
        tok = (sem, ent[1])

        def fn(e):
            oo = bass.IndirectOffsetOnAxis(ap=out_off, axis=0) if out_off is not None else None
            io = bass.IndirectOffsetOnAxis(ap=in_off, axis=0) if in_off is not None else None
            bc = None
            if bounds is not None:
                regs = self.__dict__.setdefault("_bregs", {})
                if bounds not in regs:
                    regs[bounds] = e.to_reg(bounds)
                bc = regs[bounds]
            return e.indirect_dma_start(out=out, out_offset=oo, in_=in_, in_offset=io, bounds_check=bc, oob_is_err=False)
        self.q[eng].append((fn, waits, sem, 16))
        self._commit(reads, writes, tok)
        return tok

    def wait_all_dma(self, eng="sp"):
        waits = []
        for key, (sem, val) in self.dsem.items():
            if val > 0:
                waits.append((sem, val))
        self.q[eng].append((None, waits, None, 0))

    def emit(self):
        nc = self.nc
        engmap = {"pe": "tensor", "dve": "vector", "act": "scalar", "pool": "gpsimd", "sp": "sync"}
        with nc.Block() as block:
            for ename in self.ENGS:
                lst = self.q[ename]

                def body(e, lst=lst):
                    for fn, waits, sem, inc in lst:
                        for s, v in waits:
                            e.wait_ge(s, v)
                        if fn is not None:
                            fn(e).then_inc(sem, inc)
                getattr(block, engmap[ename])(body)


EPS = 1e-6
D = 2048
KC = 16
NCOL = 1540
TB = 512
NCH = TB // 64


def consts_A():
    i = np.arange(64)
    ms = (i[:, None] > i[None, :]).astype(np.float32)
    msT = (i[:, None] < i[None, :]).astype(np.float32)
    miT = (i[:, None] <= i[None, :]).astype(np.float32)
    idn = np.eye(64, dtype=np.float32)
    c64 = np.stack([np.tile(m, (1, NCH)) for m in (ms, msT, miT, idn)], 0)
    c64p = np.zeros((4, 128, TB), np.float32)
    c64p[:, :64] = c64
    ident = np.eye(128, dtype=np.float32)
    small = np.zeros((128, 4 + 4 * 128 + TB), np.float32)
    small[:4, 0:4] = np.eye(4)
    for r in range(4):
        small[r, 4 + r * 128: 4 + (r + 1) * 128] = 1.0
    rm = np.ones(TB, np.float32)
    rm[::64] = 0.0
    small[:4, 4 + 512:] = rm[None, :]
    return {"c64": c64p, "ident": ident, "small": small}


HORDER = [0, 1]
DEBUG = False


def build_A(S):
    nc = bass.Bass("TRN2", target_bir_lowering=False)
    NB = S // TB
    dr = lambda n, s, k="ExternalInput": nc.dram_tensor(n, list(s), F32, kind=k).ap()
    xT = dr("xT", [KC, 128, S])
    wc = dr("wc", [D, NCOL])
    ngain = dr("ngain", [128, KC])
    lru_p = dr("lru_p", [128, 2, 8])
    lru_w = dr("lru_w", [128, 2, 2, 128])
    dn_cw = dr("dn_cw", [128, 6, 4])
    dn_p4 = dr("dn_p4", [4, 2])
    dn_g = dr("dn_g", [128, 1])
    c64 = dr("c64", [4, 128, TB])
    identd = dr("ident", [128, 128])
    smalld = dr("small", [128, 4 + 512 + TB])
    yT = dr("yT", [4, 128, S], "ExternalOutput")
    dbg = dr("dbg", [24, 128, TB], "ExternalOutput") if DEBUG else None

    xv = xT.rearrange("c p t -> p c t")
    wcv = wc.rearrange("(kc p) n -> p kc n", p=128)

    with ExitStack() as st:
        P = Prog(nc, st)
        sb, ps_ = P.sb, P.ps
        W = sb("W", [128, KC, NCOL], BF16)
        ng = sb("ng", [128, KC], F32)
        lp = sb("lp", [128, 2, 8], F32)
        lw32 = sb("lw32", [128, 2, 2, 128], F32)
        lw = sb("lw", [128, 2, 2, 128], BF16)
        c1 = sb("c1", [128, 2], F32)
        dcw = sb("dcw", [128, 6, 4], F32)
        p4 = sb("p4", [4, 2], F32)
        negA = sb("negA", [4, 1], F32)
        dng = sb("dng", [128, 1], F32)
        cm = sb("cm", [128, 4, TB], BF16)
        ident = sb("identf", [128, 128], F32)
        identb = sb("identb", [128, 128], BF16)
        small = sb("smallc", [128, 4 + 512 + TB], F32)
        ones = sb("ones", [128, 128], BF16)
        I4 = small[0:4, 0:4]
        sel = lambda r: small[0:4, 4 + r * 128: 4 + (r + 1) * 128]
        rmask = small[0:4, 4 + 512: 4 + 512 + TB]
        Ms, MsT, MiT, Id8 = cm[0:64, 0, :], cm[0:64, 1, :], cm[0:64, 2, :], cm[0:64, 3, :]

        x32 = [sb(f"x32_{k}", [128, 4, TB], F32) for k in range(2)]
        sq = sb("sq", [128, 4, TB], BF16)
        hT = sb("hT", [128, KC, TB], BF16)
        rs = sb("rs", [128, TB], F32)
        xbuf = [sb(f"xbuf{n}", [128, TB + 3], F32) for n in range(2)]
        hlast = [sb(f"hlast{n}", [128, 1], F32) for n in range(2)]
        gl = [sb(f"gl{n}", [128, TB], F32) for n in range(2)]
        cb = [sb(f"cb{k}", [128, TB + 3], F32) for k in range(6)]
        sgate = [sb(f"sgate{h}", [128, TB], F32) for h in range(2)]
        r4 = sb("r4", [4, 5, TB], F32)
        colt = sb("colt", [64, NCH, 16], F32)
        cole = sb("cole", [64, NCH, 2], F32)
        qkv = [sb(f"qkv{k}", [128, TB], F32) for k in range(3)]
        qkvb = [sb(f"qkvb{k}", [128, TB], BF16) for k in range(3)]
        Gb = sb("Gb", [128, TB], F32)
        betab = sb("betab", [64, TB], F32)
        EGb = sb("EGb", [128, TB], F32)
        qdTb = sb("qdTb", [128, TB], BF16)
        t1 = sb("t1", [64, TB], F32)
        eT = sb("eT", [64, TB], F32)
        eLf = sb("eLf", [128, TB], F32)
        eL = eLf[0:64, :]
        Nm = [sb(f"Nm{k}", [64, TB], F32) for k in range(2)]
        NmT = [sb(f"NmT{k}", [64, TB], F32) for k in range(2)]
        Pm = sb("Pm", [64, TB], F32)
        qkT = sb("qkT", [64, TB], BF16)
        vb = sb("vb", [64, NCH, 128], F32)
        kbg = sb("kbg", [64, NCH, 128], F32)
        kdec = sb("kdec", [64, NCH, 128], BF16)
        u_t = sb("u_t", [64, NCH, 128], F32)
        wTb = sb("wTb", [128, TB], BF16)
        vnew = sb("vnew", [64, 128], BF16)
        S32 = [sb(f"S32_{h}", [128, 128], F32) for h in range(2)]
        Sb = [sb(f"Sb_{h}", [128, 128], BF16) for h in range(2)]
        o32 = sb("o32", [128, TB], F32)
        yo = [sb(f"yo{k}", [128, TB], F32) for k in range(2)]
        l2r0 = sb("l2r0", [128, TB], F32)
        xc, r_t, i_t = qkv[0], qkv[1], qkv[2]
        xcb = qkvb[0]
        a_t, m_t, h_t = Gb, EGb, o32
        XC, XCB, RT, IT, ATk, MTk, HTk = "qkv0", "qkvb0", "qkv1", "qkv2", "Gb", "EGb", "o32"
        def t1_full(k):
            return l2r0[:] if k == 0 else eLf[:]
        L2K = ["l2r0", "eL"]

        psn = ps_("psn", [128, 512])
        pp = [ps_(f"pp{k}", [128, 512]) for k in range(2)]
        pA = ps_("pA", [128, 512])
        pB = ps_("pB", [128, 512])
        pC = ps_("pC", [128, 512])
        pTk = pC[:].bitcast(BF16)
        pR = ps_("pR", [128, 512])
        pO = ps_("pO", [128, 512])

        P.dma("sp", ng[:], ngain, writes=["ng"])
        P.dma("sp", lp[:], lru_p, writes=["lp"])
        P.dma("sp", lw32[:], lru_w, writes=["lw32"])
        P.dma("sp", dcw[:], dn_cw, writes=["dcw"])
        P.dma("sp", p4[:], dn_p4, writes=["p4"])
        P.dma("sp", dng[:], dn_g, writes=["dng"])
        P.dma("pool", cm[:], c64.rearrange("m p t -> p m t"), writes=["cm"])
        P.dma("sp", ident[:], identd, writes=["ident"])
        P.dma("sp", small[:], smalld, writes=["small"])
        for k4 in range(4):
            c0 = k4 * 385
            P.dma("pool", W[:, :, c0:c0 + 385], wcv[:, :, c0:c0 + 385], writes=[("W", k4)], key=("W", k4))
        Wk = [("W", k4) for k4 in range(4)]
        P.op("dve", lambda e: e.memset(ones[:], 1.0), writes=["ones"])
        P.op("dve", lambda e: e.tensor_copy(out=identb[:], in_=ident[:]), reads=["ident"], writes=["identb"])
        P.op("dve", lambda e: e.tensor_copy(out=lw[:], in_=lw32[:]), reads=["lw32"], writes=["lw"])
        P.op("act", lambda e: e.activation(out=c1[:], in_=lp[:, :, 7], func=AF.Exp, scale=-1.0), reads=["lp"], writes=["c1"])
        P.op("act", lambda e: e.activation(out=c1[:], in_=c1[:], func=AF.Ln, bias=1.0), reads=["c1"], writes=["c1"])
        P.op("dve", lambda e: e.tensor_scalar(out=c1[:], in0=c1[:], scalar1=-8.0, scalar2=None, op0=ALU.mult), reads=["c1"], writes=["c1"])
        P.op("act", lambda e: e.activation(out=negA[:], in_=p4[:, 1:2], func=AF.Exp), reads=["p4"], writes=["negA"])
        P.op("dve", lambda e: e.tensor_scalar(out=negA[:], in0=negA[:], scalar1=-1.0, scalar2=None, op0=ALU.mult), reads=["negA"], writes=["negA"])
        for n in range(2):
            P.op("pool", lambda e, n=n: e.memset(xbuf[n][:, 0:3], 0.0), writes=[f"xbuf{n}"])
            P.op("pool", lambda e, n=n: e.memset(hlast[n][:], 0.0), writes=[f"hlast{n}"])
            P.op("pool", lambda e, n=n: e.memset(S32[n][:], 0.0), writes=[f"S32_{n}"])
            P.op("pool", lambda e, n=n: e.memset(Sb[n][:], 0.0), writes=[f"Sb_{n}"])
        for k in range(6):
            P.op("pool", lambda e, k=k: e.memset(cb[k][:, 0:3], 0.0), writes=[f"cb{k}"])

        def conv(eng, out, buf, wtile, widx, bias, rk, wk, outk):
            if bias is None:
                P.op(eng, lambda e: e.tensor_scalar(out=out, in0=buf[:, 0:TB], scalar1=wtile[:, widx, 0:1], scalar2=None, op0=ALU.mult),
                     reads=[rk, wk], writes=[outk])
            else:
                P.op(eng, lambda e: e.tensor_scalar(out=out, in0=buf[:, 0:TB], scalar1=wtile[:, widx, 0:1], scalar2=bias, op0=ALU.mult, op1=ALU.add),
                     reads=[rk, wk], writes=[outk])
            for k in range(1, 4):
                P.op(eng, lambda e, k=k: e.scalar_tensor_tensor(out=out, in0=buf[:, k:k + TB], scalar=wtile[:, widx, k:k + 1], in1=out,
                                                              op0=ALU.mult, op1=ALU.add),
                     reads=[rk, wk, outk], writes=[outk])

        ycnt = [0]
        dcnt = [0]

        def dump(ap, key, npart=128):
            if not DEBUG:
                return
            slot = dcnt[0]
            dcnt[0] += 1
            P.dma("sp", dbg[slot, 0:npart, :], ap, reads=[key], key=("dbg", slot))

        def store_y(tile_idx, t0, src_key, src):
            P.dma("sp", yT[tile_idx, :, t0:t0 + TB], src, reads=[src_key], key=("yst", src_key))

        for blk in range(NB):
            t0 = blk * TB
            for q4 in range(4):
                xb = x32[q4 % 2]
                xk = f"x32_{q4 % 2}"
                P.dma("sp", xb[:], xv[:, q4 * 4:(q4 + 1) * 4, t0:t0 + TB], writes=[xk])
                P.op("act", lambda e, xb=xb: e.activation(out=sq[:], in_=xb[:], func=AF.Square), reads=[xk], writes=["sq"])
                for j in range(4):
                    jj = q4 * 4 + j
                    P.op("pe", lambda e, j=j, jj=jj: e.matmul(psn[:], ones[:], sq[:, j, :], start=(jj == 0), stop=(jj == KC - 1)),
                         reads=["ones", "sq"], writes=["psn"])
                    P.op("dve", lambda e, j=j, jj=jj, xb=xb: e.tensor_scalar(out=hT[:, jj, :], in0=xb[:, j, :], scalar1=ng[:, jj:jj + 1], scalar2=None, op0=ALU.mult),
                         reads=[xk, "ng"], writes=[("hT", jj)])
            P.op("act", lambda e: e.activation(out=rs[:], in_=psn[:], func=AF.Sqrt, bias=EPS, scale=1.0 / D), reads=["psn"], writes=["rs"])
            P.op("dve", lambda e: e.reciprocal(out=rs[:], in_=rs[:]), reads=["rs"], writes=["rs"])

            def proj(ct, M):
                pb = pp[ct % 2]
                c0 = ct * 128
                for kc in range(KC):
                    P.op("pe", lambda e, kc=kc: e.matmul(pb[0:M, :], W[:, kc, c0:c0 + M], hT[:, kc, :], start=(kc == 0), stop=(kc == KC - 1)),
                         reads=Wk + [("hT", kc)], writes=[f"pp{ct % 2}"])
                return pb, f"pp{ct % 2}"

            for n in range(2):
                pb, pk = proj(n, 128)
                P.op("dve", lambda e, n=n, pb=pb: e.tensor_tensor(out=xbuf[n][:, 3:3 + TB], in0=pb[:], in1=rs[:], op=ALU.mult),
                     reads=[pk, "rs"], writes=[f"xbuf{n}"])
            for n in range(2):
                pb, pk = proj(2 + n, 128)
                P.op("dve", lambda e, n=n, pb=pb: e.tensor_tensor(out=gl[n][:], in0=pb[:], in1=rs[:], op=ALU.mult),
                     reads=[pk, "rs"], writes=[f"gl{n}"])
                P.op("act", lambda e, n=n: e.activation(out=gl[n][:], in_=gl[n][:], func=AF.Gelu_apprx_tanh), reads=[f"gl{n}"], writes=[f"gl{n}"])
            for k in range(6):
                pb, pk = proj(4 + k, 128)
                P.op("dve", lambda e, k=k, pb=pb: e.tensor_tensor(out=cb[k][:, 3:3 + TB], in0=pb[:], in1=rs[:], op=ALU.mult),
                     reads=[pk, "rs"], writes=[f"cb{k}"])
            for h in range(2):
                pb, pk = proj(10 + h, 128)
                P.op("dve", lambda e, h=h, pb=pb: e.tensor_tensor(out=sgate[h][:], in0=pb[:], in1=rs[:], op=ALU.mult),
                     reads=[pk, "rs"], writes=[f"sgate{h}"])
                P.op("act", lambda e, h=h: e.activation(out=sgate[h][:], in_=sgate[h][:], func=AF.Silu), reads=[f"sgate{h}"], writes=[f"sgate{h}"])
            pb, pk = proj(12, 4)
            P.op("dve", lambda e, pb=pb: e.tensor_tensor(out=r4[:, 0, :], in0=pb[0:4, :], in1=rs[0:4, :], op=ALU.mult),
                 reads=[pk, "rs"], writes=["s4"])

            for n in range(2):
                conv("dve", xc[:], xbuf[n], lp, n, lp[:, n, 4:5], f"xbuf{n}", "lp", XC)
                P.op("pool", lambda e, n=n: e.tensor_copy(out=xbuf[n][:, 0:3], in_=xbuf[n][:, TB:TB + 3]), reads=[XC], writes=[f"xbuf{n}"])
                P.op("act", lambda e: e.activation(out=xcb[:], in_=xc[:], func=AF.Copy), reads=[XC], writes=[XCB])
                P.op("pe", lambda e, n=n: e.matmul(pA[:], lw[:, n, 0, :], xcb[:], start=True, stop=True), reads=["lw", XCB], writes=["pA"])
                P.op("pe", lambda e, n=n: e.matmul(pB[:], lw[:, n, 1, :], xcb[:], start=True, stop=True), reads=["lw", XCB], writes=["pB"])
                P.op("act", lambda e, n=n: e.activation(out=r_t[:], in_=pA[:], func=AF.Sigmoid, bias=lp[:, n, 5:6]), reads=["pA", "lp"], writes=[RT])
                P.op("act", lambda e, n=n: e.activation(out=i_t[:], in_=pB[:], func=AF.Sigmoid, bias=lp[:, n, 6:7]), reads=["pB", "lp"], writes=[IT])
                P.op("act", lambda e, n=n: e.activation(out=a_t[:], in_=r_t[:], func=AF.Exp, scale=c1[:, n:n + 1]), reads=[RT, "c1"], writes=[ATk])
                P.op("act", lambda e: e.activation(out=m_t[:], in_=a_t[:], func=AF.Square), reads=[ATk], writes=[MTk])
                P.op("act", lambda e: e.activation(out=m_t[:], in_=m_t[:], func=AF.Sqrt, bias=1.0, scale=-1.0), reads=[MTk], writes=[MTk])
                P.op("pool", lambda e: e.tensor_tensor(out=i_t[:], in0=i_t[:], in1=xc[:], op=ALU.mult), reads=[IT, XC], writes=[IT])
                P.op("pool", lambda e: e.tensor_tensor(out=m_t[:], in0=m_t[:], in1=i_t[:], op=ALU.mult), reads=[MTk, IT], writes=[MTk])
                P.op("dve", lambda e, n=n: e.tensor_tensor_scan(out=h_t[:], data0=a_t[:], data1=m_t[:], initial=hlast[n][:, 0:1],
                                                               op0=ALU.mult, op1=ALU.add),
                     reads=[ATk, MTk, f"hlast{n}"], writes=[HTk])
                P.op("pool", lambda e, n=n: e.tensor_copy(out=hlast[n][:], in_=h_t[:, TB - 1:TB]), reads=[HTk], writes=[f"hlast{n}"])
                yk = ycnt[0] % 2
                ycnt[0] += 1
                P.op("pool", lambda e, n=n, yk=yk: e.tensor_tensor(out=yo[yk][:], in0=h_t[:], in1=gl[n][:], op=ALU.mult),
                     reads=[HTk, f"gl{n}"], writes=[f"yo{yk}"])
                store_y(n, t0, f"yo{yk}", yo[yk][:])

            s4, B4, g4, G4, EG4 = (r4[:, k, :] for k in range(5))
            P.op("act", lambda e: e.activation(out=B4, in_=s4, func=AF.Sigmoid), reads=["s4"], writes=["B4"])
            P.op("act", lambda e: e.activation(out=g4, in_=s4, func=AF.Exp, bias=p4[:, 0:1]), reads=["s4", "p4"], writes=["g4"])
            P.op("act", lambda e: e.activation(out=g4, in_=g4, func=AF.Ln, bias=1.0), reads=["g4"], writes=["g4"])
            P.op("dve", lambda e: e.tensor_scalar(out=g4, in0=g4, scalar1=negA[:, 0:1], scalar2=None, op0=ALU.mult), reads=["g4", "negA"], writes=["g4"])
            P.op("dve", lambda e: e.tensor_tensor_scan(out=G4, data0=rmask, data1=g4, initial=0.0, op0=ALU.mult, op1=ALU.add),
                 reads=["g4", "small"], writes=["G4"])
            P.op("act", lambda e: e.activation(out=EG4, in_=G4, func=AF.Exp), reads=["G4"], writes=["EG4"])
            ED4 = s4
            G4c = G4.rearrange("p (c t) -> p c t", t=64)
            P.op("dve", lambda e: e.tensor_tensor(out=ED4.rearrange("p (c t) -> p c t", t=64), in0=G4c[:, :, 63:64].to_broadcast([4, NCH, 64]),
                                                  in1=G4c, op=ALU.subtract), reads=["G4"], writes=["s4"])
            P.op("act", lambda e: e.activation(out=ED4, in_=ED4, func=AF.Exp), reads=["s4"], writes=["s4"])
            quants = [(B4, "B4"), (G4, "G4"), (EG4, "EG4"), (ED4, "s4")]
            for c in range(NCH):
                for qi, (qt, qk_) in enumerate(quants):
                    P.op("pe", lambda e, c=c, qi=qi, qt=qt: e.matmul(pR[0:64, 256 + c * 16 + qi * 4: 256 + c * 16 + qi * 4 + 4],
                                                                     qt[:, c * 64:(c + 1) * 64], I4, start=True, stop=True),
                         reads=[qk_, "small"], writes=["pR"])
            P.op("dve", lambda e: e.tensor_copy(out=colt[:].rearrange("p c q -> p (c q)"), in_=pR[0:64, 256:256 + NCH * 16]), reads=["pR"], writes=["colt"])
            for h in range(2):
                P.op("dve", lambda e, h=h: e.tensor_tensor(out=cole[:, :, h:h + 1], in0=colt[:, :, h:h + 1], in1=colt[:, :, 10 + h:11 + h], op=ALU.mult),
                     reads=["colt"], writes=[("cole", h)])

            for h in HORDER:
                for k in range(3):
                    conv("dve", qkv[k][:], cb[k * 2 + h], dcw, k * 2 + h, None, f"cb{k * 2 + h}", "dcw", f"qkv{k}")
                    P.op("pool", lambda e, k=k, h=h: e.tensor_copy(out=cb[k * 2 + h][:, 0:3], in_=cb[k * 2 + h][:, TB:TB + 3]),
                         reads=[f"qkv{k}"], writes=[f"cb{k * 2 + h}"])
                    P.op("act", lambda e, k=k: e.activation(out=qkv[k][:], in_=qkv[k][:], func=AF.Silu), reads=[f"qkv{k}"], writes=[f"qkv{k}"])
                for k in range(2):
                    P.op("act", lambda e, k=k: e.activation(out=sq[:, 0, :], in_=qkv[k][:], func=AF.Square), reads=[f"qkv{k}"], writes=["sq"])
                    P.op("pe", lambda e: e.matmul(psn[:], ones[:], sq[:, 0, :], start=True, stop=True), reads=["ones", "sq"], writes=["psn"])
                    P.op("act", lambda e, k=k: e.activation(out=t1_full(k), in_=psn[:], func=AF.Sqrt, bias=EPS, scale=1.0), reads=["psn"], writes=[L2K[k]])
                    P.op("dve", lambda e, k=k: e.reciprocal(out=t1_full(k), in_=t1_full(k)), reads=[L2K[k]], writes=[L2K[k]])
                    sc = (128.0 ** -0.5) if k == 0 else 1.0
                    P.op("dve", lambda e, k=k, sc=sc: e.scalar_tensor_tensor(out=qkvb[k][:], in0=qkv[k][:], scalar=sc, in1=t1_full(k), op0=ALU.mult, op1=ALU.mult),
                         reads=[f"qkv{k}", L2K[k]], writes=[f"qkvb{k}"])
                    if k == 0:
                        P.op("pool", lambda e: e.tensor_tensor(out=qkv[0][:], in0=qkv[0][:], in1=t1_full(0), op=ALU.mult),
                             reads=["qkv0", "l2r0"], writes=["qkv0"])
                P.op("act", lambda e: e.activation(out=qkvb[2][:], in_=qkv[2][:], func=AF.Copy), reads=["qkv2"], writes=["qkvb2"])
                qnb, knb, vcb = qkvb
                P.op("pe", lambda e, h=h: e.matmul(pA[:], sel(2 + h), G4, start=True, stop=True), reads=["G4", "small"], writes=["pA"])
                P.op("act", lambda e: e.activation(out=Gb[:], in_=pA[:], func=AF.Copy), reads=["pA"], writes=["Gb"])
                P.op("act", lambda e: e.activation(out=EGb[:], in_=pA[:], func=AF.Exp), reads=["pA"], writes=["EGb"])
                P.op("pe", lambda e, h=h: e.matmul(pB[:], sel(h), B4, start=True, stop=True), reads=["B4", "small"], writes=["pB"])
                P.op("act", lambda e: e.activation(out=betab[:], in_=pB[0:64, :], func=AF.Copy), reads=["pB"], writes=["betab"])
                P.op("dve", lambda e: e.scalar_tensor_tensor(out=qdTb[:], in0=qkv[0][:], scalar=128.0 ** -0.5, in1=EGb[:], op0=ALU.mult, op1=ALU.mult),
                     reads=["qkv0", "EGb"], writes=["qdTb"])
                if blk == 0 and h == HORDER[0]:
                    dump(Gb[:], "Gb"); dump(EGb[:], "EGb"); dump(qkv[0][:], "qkv0"); dump(betab[:], "betab", 64)
                for c in range(NCH):
                    cs = slice(c * 64, (c + 1) * 64)
                    P.op("pe", lambda e, cs=cs: e.matmul(pA[0:64, cs], knb[:, cs], knb[:, cs], start=True, stop=True), reads=["qkvb1"], writes=["pA"])
                for c in range(NCH):
                    cs = slice(c * 64, (c + 1) * 64)
                    P.op("pe", lambda e, cs=cs: e.matmul(pB[0:64, cs], knb[:, cs], qnb[:, cs], start=True, stop=True), reads=["qkvb1", "qkvb0"], writes=["pB"])
                for c in range(NCH):
                    cs = slice(c * 64, (c + 1) * 64)
                    P.op("pe", lambda e, c=c, cs=cs: e.transpose(pTk[0:64, c * 128:(c + 1) * 128], knb[:, cs], identb[:]), reads=["qkvb1", "identb"], writes=["pC"])
                GcolB = colt[:, :, 6 + h:7 + h].to_broadcast([64, NCH, 64])
                bcolB = colt[:, :, h:h + 1].to_broadcast([64, NCH, 64])
                v3 = lambda t: t.rearrange("p (c t) -> p c t", t=64)
                P.op("dve", lambda e, GcolB=GcolB: e.tensor_tensor(out=v3(t1[:]), in0=v3(Gb[0:64, :]), in1=GcolB, op=ALU.subtract), reads=["Gb", "colt"], writes=["t1"])
                P.op("pool", lambda e: e.tensor_scalar(out=eT[:], in0=t1[:], scalar1=0.0, scalar2=None, op0=ALU.min), reads=["t1"], writes=["eT"])
                P.op("act", lambda e: e.activation(out=eT[:], in_=eT[:], func=AF.Exp), reads=["eT"], writes=["eT"])
                P.op("pool", lambda e: e.tensor_scalar(out=eL[:], in0=t1[:], scalar1=0.0, scalar2=None, op0=ALU.max), reads=["t1"], writes=["eL"])
                P.op("act", lambda e: e.activation(out=eL[:], in_=eL[:], func=AF.Exp, scale=-1.0), reads=["eL"], writes=["eL"])
                if blk == 0 and h == HORDER[0]:
                    dump(t1[:], "t1", 64); dump(eL, "eL", 64)
                P.op("pool", lambda e: e.tensor_tensor(out=eL[:], in0=eL[:], in1=Ms, op=ALU.mult), reads=["eL", "cm"], writes=["eL"])
                P.op("dve", lambda e, bcolB=bcolB: e.tensor_tensor(out=v3(eL[:]), in0=v3(eL[:]), in1=bcolB, op=ALU.mult), reads=["eL", "colt"], writes=["eL"])
                P.op("dve", lambda e: e.scalar_tensor_tensor(out=Nm[0][:], in0=pA[0:64, :], scalar=-1.0, in1=eL[:], op0=ALU.mult, op1=ALU.mult),
                     reads=["pA", "eL"], writes=["Nm0"])
                if blk == 0 and h == HORDER[0]:
                    dump(eL, "eL", 64)
                P.op("pool", lambda e: e.tensor_tensor(out=t1[:], in0=eT[:], in1=MiT, op=ALU.mult), reads=["eT", "cm"], writes=["t1"])
                P.op("dve", lambda e: e.tensor_tensor(out=qkT[:], in0=pB[0:64, :], in1=t1[:], op=ALU.mult), reads=["pB", "t1"], writes=["qkT"])
                P.op("pool", lambda e: e.tensor_tensor(out=eT[:], in0=eT[:], in1=MsT, op=ALU.mult), reads=["eT", "cm"], writes=["eT"])
                P.op("pool", lambda e: e.tensor_tensor(out=eT[:], in0=eT[:], in1=betab[:], op=ALU.mult), reads=["eT", "betab"], writes=["eT"])
                P.op("dve", lambda e: e.scalar_tensor_tensor(out=NmT[0][:], in0=pA[0:64, :], scalar=-1.0, in1=eT[:], op0=ALU.mult, op1=ALU.mult),
                     reads=["pA", "eT"], writes=["NmT0"])
                pk3 = pTk[0:64, :].rearrange("p (c d) -> p c d", d=128)
                P.op("dve", lambda e, h=h: e.tensor_tensor(out=kbg[:], in0=pk3, in1=cole[:, :, h:h + 1].to_broadcast([64, NCH, 128]), op=ALU.mult),
                     reads=["pC", ("cole", h)], writes=["kbg"])
                P.op("dve", lambda e, h=h: e.tensor_tensor(out=kdec[:], in0=pk3, in1=colt[:, :, 14 + h:15 + h].to_broadcast([64, NCH, 128]), op=ALU.mult),
                     reads=["pC", "colt"], writes=["kdec"])
                for c in range(NCH):
                    cs = slice(c * 64, (c + 1) * 64)
                    P.op("pe", lambda e, c=c, cs=cs: e.transpose(pTk[0:64, c * 128:(c + 1) * 128], vcb[:, cs], identb[:]), reads=["qkvb2", "identb"], writes=["pC"])
                P.op("dve", lambda e, h=h: e.tensor_tensor(out=vb[:], in0=pk3, in1=colt[:, :, h:h + 1].to_broadcast([64, NCH, 128]), op=ALU.mult),
                     reads=["pC", "colt"], writes=["vb"])
                if blk == 0 and h == HORDER[0]:
                    dump(Nm[0][:], "Nm0", 64); dump(NmT[0][:], "NmT0", 64); dump(vb[:, 0:4, :].rearrange("p c d -> p (c d)"), "vb", 64); dump(kbg[:, 0:4, :].rearrange("p c d -> p (c d)"), "kbg", 64)
                P.op("pool", lambda e: e.tensor_tensor(out=Pm[:], in0=NmT[0][:], in1=Id8, op=ALU.add), reads=["NmT0", "cm"], writes=["Pm"])
                cur = 0
                for lvl in range(1, 6):
                    nxt = 1 - cur
                    for c in range(NCH):
                        cs = slice(c * 64, (c + 1) * 64)
                        P.op("pe", lambda e, cs=cs, cur=cur: e.matmul(pA[0:64, cs], NmT[cur][:, cs], Nm[cur][:, cs], start=True, stop=True),
                             reads=[f"NmT{cur}", f"Nm{cur}"], writes=["pA"])
                    P.op("act", lambda e, nxt=nxt: e.activation(out=Nm[nxt][:], in_=pA[0:64, :], func=AF.Copy), reads=["pA"], writes=[f"Nm{nxt}"])
                    if lvl < 5:
                        for c in range(NCH):
                            cs = slice(c * 64, (c + 1) * 64)
                            P.op("pe", lambda e, cs=cs, cur=cur: e.matmul(pB[0:64, cs], Nm[cur][:, cs], NmT[cur][:, cs], start=True, stop=True),
                                 reads=[f"NmT{cur}", f"Nm{cur}"], writes=["pB"])
                        P.op("act", lambda e, nxt=nxt: e.activation(out=NmT[nxt][:], in_=pB[0:64, :], func=AF.Copy), reads=["pB"], writes=[f"NmT{nxt}"])
                    for c in range(NCH):
                        cs = slice(c * 64, (c + 1) * 64)
                        P.op("pe", lambda e, cs=cs, nxt=nxt: e.matmul(pC[0:64, cs], Nm[nxt][:, cs], Pm[:, cs], start=True, stop=True),
                             reads=[f"Nm{nxt}", "Pm"], writes=["pC"])
                    P.op("dve", lambda e: e.tensor_tensor(out=Pm[:], in0=Pm[:], in1=pC[0:64, :], op=ALU.add), reads=["pC", "Pm"], writes=["Pm"])
                    cur = nxt
                for half in range(2):
                    for c4 in range(4):
                        c = half * 4 + c4
                        cs = slice(c * 64, (c + 1) * 64)
                        pu = pA if half == 0 else pB
                        P.op("pe", lambda e, c=c, c4=c4, cs=cs, pu=pu: e.matmul(pu[0:64, c4 * 128:(c4 + 1) * 128], Pm[:, cs], vb[:, c, :], start=True, stop=True),
                             reads=["Pm", "vb"], writes=[("pA" if half == 0 else "pB")])
                    pu = pA if half == 0 else pB
                    P.op("act", lambda e, half=half, pu=pu: e.activation(out=u_t[:, half * 4:(half + 1) * 4, :].rearrange("p c d -> p (c d)"), in_=pu[0:64, :], func=AF.Copy),
                         reads=[("pA" if half == 0 else "pB")], writes=[("u", half)])
                for c in range(NCH):
                    cs = slice(c * 64, (c + 1) * 64)
                    P.op("pe", lambda e, c=c, cs=cs: e.matmul(pC[:, cs], kbg[:, c, :], Pm[:, cs], start=True, stop=True), reads=["Pm", "kbg"], writes=["pC"])
                P.op("act", lambda e: e.activation(out=wTb[:], in_=pC[:], func=AF.Copy), reads=["pC"], writes=["wTb"])
                if blk == 0 and h == HORDER[0]:
                    dump(Pm[:], "Pm", 64); dump(u_t[:, 0:4, :].rearrange("p c d -> p (c d)"), ("u", 0), 64)
                Sk, Sbk = f"S32_{h}", f"Sb_{h}"
                for c in range(NCH):
                    cs = slice(c * 64, (c + 1) * 64)
                    P.op("pe", lambda e, cs=cs, h=h: e.matmul(pR[0:64, 0:128], wTb[:, cs], Sb[h][:], start=True, stop=True), reads=["wTb", Sbk], writes=["pR"])
                    P.op("dve", lambda e, c=c: e.tensor_tensor(out=vnew[:], in0=u_t[:, c, :], in1=pR[0:64, 0:128], op=ALU.subtract),
                         reads=["pR", ("u", c // 4)], writes=["vnew"])
                    P.op("pe", lambda e, cs=cs, h=h: e.matmul(pO[:, cs], Sb[h][:], qdTb[:, cs], start=True, stop=False), reads=[Sbk, "qdTb"], writes=["pO"])
                    P.op("pe", lambda e, cs=cs: e.matmul(pO[:, cs], vnew[:], qkT[:, cs], start=False, stop=True), reads=["vnew", "qkT"], writes=["pO"])
                    P.op("pe", lambda e, c=c: e.matmul(pR[:, 128:256], kdec[:, c, :], vnew[:], start=True, stop=True), reads=["vnew", "kdec"], writes=["pR"])
                    P.op("dve", lambda e, h=h, c=c: e.scalar_tensor_tensor(out=S32[h][:], in0=S32[h][:], scalar=EGb[:, c * 64 + 63:c * 64 + 64], in1=pR[:, 128:256],
                                                                          op0=ALU.mult, op1=ALU.add), reads=["pR", Sk, "EGb"], writes=[Sk])
                    P.op("act", lambda e, h=h: e.activation(out=Sb[h][:], in_=S32[h][:], func=AF.Copy), reads=[Sk], writes=[Sbk])
                P.op("act", lambda e: e.activation(out=o32[:], in_=pO[:], func=AF.Copy), reads=["pO"], writes=["o32"])
                P.op("act", lambda e: e.activation(out=sq[:, 0, :], in_=pO[:], func=AF.Square), reads=["pO"], writes=["sq"])
                P.op("pe", lambda e: e.matmul(psn[:], ones[:], sq[:, 0, :], start=True, stop=True), reads=["ones", "sq"], writes=["psn"])
                P.op("act", lambda e: e.activation(out=t1_full(0), in_=psn[:], func=AF.Sqrt, bias=EPS, scale=1.0 / 128), reads=["psn"], writes=["l2r0"])
                P.op("dve", lambda e: e.reciprocal(out=t1_full(0), in_=t1_full(0)), reads=["l2r0"], writes=["l2r0"])
                if blk == 0 and h == HORDER[0]:
                    dump(o32[:], "o32")
                yk = ycnt[0] % 2
                ycnt[0] += 1
                P.op("dve", lambda e, yk=yk: e.scalar_tensor_tensor(out=yo[yk][:], in0=o32[:], scalar=dng[:, 0:1], in1=t1_full(0), op0=ALU.mult, op1=ALU.mult),
                     reads=["o32", "dng", "l2r0"], writes=[f"yo{yk}"])
                P.op("pool", lambda e, yk=yk, h=h: e.tensor_tensor(out=yo[yk][:], in0=yo[yk][:], in1=sgate[h][:], op=ALU.mult),
                     reads=[f"yo{yk}", f"sgate{h}"], writes=[f"yo{yk}"])
                store_y(2 + h, t0, f"yo{yk}", yo[yk][:])
        P.wait_all_dma("sp")
        P.emit()
    return nc


EPS = 1e-6
D = 2048
KC = 16


def build_B(NT, FF, mode="dense", final_norm=False, PASS=1024):
    nc = bass.Bass("TRN2", target_bir_lowering=False)
    TT = 512
    PASS = min(PASS, NT)
    npass = NT // PASS
    tpp = PASS // TT
    NF = FF // 128
    ymT = nc.dram_tensor("ymT", [KC, 128, NT], F32, kind="ExternalInput").ap()
    xT = nc.dram_tensor("xT", [KC, 128, NT], F32, kind="ExternalInput").ap()
    wout = nc.dram_tensor("wout", [D, D], F32, kind="ExternalInput").ap()
    lgain = nc.dram_tensor("lgain", [128, 8], F32, kind="ExternalInput").ap()
    ngain = nc.dram_tensor("ngain", [128, KC], F32, kind="ExternalInput").ap()
    wg = nc.dram_tensor("wg", [D, FF], F32, kind="ExternalInput").ap()
    wu = nc.dram_tensor("wu", [D, FF], F32, kind="ExternalInput").ap()
    wd = nc.dram_tensor("wd", [FF, D], F32, kind="ExternalInput").ap()
    x2T = nc.dram_tensor("x2T", [KC, 128, NT], F32, kind="ExternalOutput").ap()
    x1s = nc.dram_tensor("x1s", [KC, 128, NT], F32, kind="Internal").ap()

    ymv = ymT.rearrange("c p t -> p c t")
    xv = xT.rearrange("c p t -> p c t")
    x1v = x1s.rearrange("c p t -> p c t")
    woutv = wout.rearrange("(kc p) n -> p kc n", p=128)
    wgv = wg.rearrange("(kc p) n -> p kc n", p=128)
    wuv = wu.rearrange("(kc p) n -> p kc n", p=128)
    wdv = wd.rearrange("(f p) n -> p f n", p=128)

    with ExitStack() as st:
        P = Prog(nc, st)
        ones = P.sb("ones", [128, 128], BF16)
        lg = P.sb("lg", [128, 8], F32)
        ng = P.sb("ng", [128, KC], F32)
        AR = max(24576, NF * PASS // 2)
        arena = P.sb("arena", [128, AR], F32)
        ym32 = arena[:, 0:8192].rearrange("p (c t) -> p c t", c=KC)
        x32 = arena[:, 8192:16384].rearrange("p (c t) -> p c t", c=KC)
        ymb = arena[:, 16384:20480].bitcast(BF16).rearrange("p (c t) -> p c t", c=KC)
        sq = arena[:, 20480:24576].bitcast(BF16).rearrange("p (c t) -> p c t", c=KC)
        actT = arena[:, 0:NF * PASS // 2].bitcast(BF16).rearrange("p (f t) -> p f t", f=NF)
        h2T = P.sb("h2T", [128, KC, PASS], BF16)
        rs = P.sb("rs", [128, TT], F32)
        wo = [P.sb(f"wo{i}", [128, KC, 128], BF16) for i in range(2)]
        wbuf = [P.sb(f"wbuf{i}", [128, 8192], BF16) for i in range(2)]
        wgt = [w[:, 0:4096].rearrange("p (c n) -> p c n", c=KC) for w in wbuf]
        wut = [w[:, 4096:8192].rearrange("p (c n) -> p c n", c=KC) for w in wbuf]
        wdt = [w[:, 0:NF * 128].rearrange("p (f n) -> p f n", f=NF) for w in wbuf]
        dummy = P.sb("dmy", [128, 8], F32)
        sg = [P.sb(f"sg{i}", [128, TT], F32) for i in range(2)]
        xr = [P.sb(f"xr{i}", [128, TT], F32) for i in range(2)]
        xo = [P.sb(f"xo{i}", [128, TT], F32) for i in range(2)]
        ps = [P.ps(f"ps{i}", [128, 512]) for i in range(8)]

        P.op("dve", lambda e: e.memset(ones[:], 1.0), writes=["ones"])
        P.dma("sp", lg[:], lgain, writes=["lg"])
        P.dma("sp", ng[:], ngain, writes=["ng"])

        cnt = {"wo": 0, "w": 0, "wd": 0, "po": 0, "g": 0, "x": 0}
        for ps_i in range(npass):
            for tq in range(tpp):
                t0 = ps_i * PASS + tq * TT
                P.dma("sp", ym32, ymv[:, :, t0:t0 + TT], writes=["ym32"])
                P.dma("sp", x32, xv[:, :, t0:t0 + TT], writes=["x32"] + [("x1", j) for j in range(KC)])
                P.op("act", lambda e: e.activation(out=sq[:, 0:8, :], in_=ym32[:, 0:8, :], func=AF.Square),
                     reads=["ym32"], writes=["sq"])
                for j in range(8):
                    P.op("pe", lambda e, j=j: e.matmul(ps[0][:], ones[:], sq[:, j, :], start=(j == 0), stop=(j == 7)),
                         reads=["ones", "sq"], writes=["ps0"])
                P.op("act", lambda e: e.activation(out=rs[:], in_=ps[0][:], func=AF.Sqrt, bias=EPS, scale=1.0 / 1024),
                     reads=["ps0"], writes=["rs"])
                P.op("dve", lambda e: e.reciprocal(out=rs[:], in_=rs[:]), reads=["rs"], writes=["rs"])
                for j in range(8):
                    P.op("dve", lambda e, j=j: e.scalar_tensor_tensor(out=ymb[:, j, :], in0=ym32[:, j, :], scalar=lg[:, j:j + 1],
                                                                        in1=rs[:], op0=ALU.mult, op1=ALU.mult),
                         reads=["ym32", "lg", "rs"], writes=[("ymb", j)])
                P.op("pool", lambda e: e.tensor_copy(out=ymb[:, 8:16, :], in_=ym32[:, 8:16, :]),
                     reads=["ym32"], writes=[("ymb", j) for j in range(8, 16)])
                for dt in range(KC):
                    b = cnt["wo"] % 2
                    cnt["wo"] += 1
                    P.dma("pool", wo[b][:], woutv[:, :, dt * 128:dt * 128 + 128], writes=[f"wo{b}"])
                    pb = 1 + cnt["po"] % 2
                    cnt["po"] += 1
                    for kc in range(KC):
                        P.op("pe", lambda e, kc=kc, b=b, pb=pb: e.matmul(
                            ps[pb][:], wo[b][:, kc, :], ymb[:, kc, :], start=(kc == 0), stop=(kc == KC - 1)),
                            reads=[f"wo{b}", ("ymb", kc)], writes=[f"ps{pb}"])
                    P.op("dve", lambda e, dt=dt, pb=pb: e.tensor_tensor(out=x32[:, dt, :], in0=x32[:, dt, :], in1=ps[pb][:], op=ALU.add),
                         reads=[f"ps{pb}", "x32"], writes=[("x1", dt)])
                x1keys = [("x1", dt) for dt in range(KC)]
                P.dma("sp", x1v[:, :, t0:t0 + TT], x32, reads=x1keys, writes=["x1s"], key="x1st")
                P.op("act", lambda e: e.activation(out=sq[:], in_=x32[:], func=AF.Square),
                     reads=x1keys, writes=["sq"])
                for j in range(KC):
                    P.op("pe", lambda e, j=j: e.matmul(ps[0][:], ones[:], sq[:, j, :], start=(j == 0), stop=(j == KC - 1)),
                         reads=["ones", "sq"], writes=["ps0"])
                P.op("act", lambda e: e.activation(out=rs[:], in_=ps[0][:], func=AF.Sqrt, bias=EPS, scale=1.0 / D),
                     reads=["ps0"], writes=["rs"])
                P.op("dve", lambda e: e.reciprocal(out=rs[:], in_=rs[:]), reads=["rs"], writes=["rs"])
                for j in range(KC):
                    P.op("dve", lambda e, j=j, tq=tq: e.scalar_tensor_tensor(
                        out=h2T[:, j, tq * TT:(tq + 1) * TT], in0=x32[:, j, :], scalar=ng[:, j:j + 1],
                        in1=rs[:], op0=ALU.mult, op1=ALU.mult),
                        reads=[("x1", j), "ng", "rs"], writes=[("h2T", tq)])
            b1keys = ["ym32", "x32", "sq"] + [("ymb", j) for j in range(KC)] + [("x1", j) for j in range(KC)]
            P.op("pool", lambda e: e.memset(dummy[:], 1.0), writes=b1keys + ["actT"])
            for fc in range(FF // 256):
                b = cnt["w"] % 2
                cnt["w"] += 1
                P.dma("pool", wgt[b], wgv[:, :, fc * 256:(fc + 1) * 256], writes=[f"wbuf{b}"], key=f"wg{b}")
                P.dma("pool", wut[b], wuv[:, :, fc * 256:(fc + 1) * 256], writes=[f"wbufu{b}"], key=f"wu{b}")
                for half in range(2):
                    f = fc * 2 + half
                    for tq in range(tpp):
                        g = cnt["g"] % 2
                        cnt["g"] += 1
                        pg, pu = 3 + g, 5 + g
                        for kc in range(KC):
                            P.op("pe", lambda e, kc=kc, b=b, pg=pg, half=half, tq=tq: e.matmul(
                                ps[pg][:], wgt[b][:, kc, half * 128:(half + 1) * 128], h2T[:, kc, tq * TT:(tq + 1) * TT],
                                start=(kc == 0), stop=(kc == KC - 1)),
                                reads=[f"wbuf{b}", ("h2T", tq)], writes=[f"ps{pg}"])
                        for kc in range(KC):
                            P.op("pe", lambda e, kc=kc, b=b, pu=pu, half=half, tq=tq: e.matmul(
                                ps[pu][:], wut[b][:, kc, half * 128:(half + 1) * 128], h2T[:, kc, tq * TT:(tq + 1) * TT],
                                start=(kc == 0), stop=(kc == KC - 1)),
                                reads=[f"wbufu{b}", ("h2T", tq)], writes=[f"ps{pu}"])
                        P.op("act", lambda e, g=g, pg=pg: e.activation(out=sg[g][:], in_=ps[pg][:], func=AF.Silu),
                             reads=[f"ps{pg}"], writes=[f"sg{g}"])
                        P.op("dve", lambda e, g=g, pu=pu, f=f, tq=tq: e.tensor_tensor(
                            out=actT[:, f, tq * TT:(tq + 1) * TT], in0=sg[g][:], in1=ps[pu][:], op=ALU.mult),
                            reads=[f"sg{g}", f"ps{pu}", "actT"], writes=[("act", f, tq)])
            for dt in range(KC):
                b = cnt["wd"] % 2
                cnt["wd"] += 1
                P.dma("pool", wdt[b], wdv[:, :, dt * 128:(dt + 1) * 128], writes=[f"wbuf{b}", f"wbufu{b}"], key=f"wg{b}")
                for tq in range(tpp):
                    t0 = ps_i * PASS + tq * TT
                    pb = 1 + cnt["po"] % 2
                    cnt["po"] += 1
                    xb = cnt["x"] % 2
                    cnt["x"] += 1
                    P.dma("sp", xr[xb][:], x1v[:, dt, t0:t0 + TT], reads=["x1s"], writes=[f"xr{xb}"])
                    for f in range(NF):
                        P.op("pe", lambda e, f=f, b=b, pb=pb, tq=tq: e.matmul(
                            ps[pb][:], wdt[b][:, f, :], actT[:, f, tq * TT:(tq + 1) * TT],
                            start=(f == 0), stop=(f == NF - 1)),
                            reads=[f"wbuf{b}", ("act", f, tq)], writes=[f"ps{pb}"])
                    P.op("dve", lambda e, xb=xb, pb=pb: e.tensor_tensor(out=xo[xb][:], in0=xr[xb][:], in1=ps[pb][:], op=ALU.add),
                         reads=[f"xr{xb}", f"ps{pb}"], writes=[f"xo{xb}"])
                    P.dma("sp", x2T[dt, :, t0:t0 + TT], xo[xb][:], reads=[f"xo{xb}"], key=f"xo{xb}")
            allact = [("act", f, tq) for f in range(NF) for tq in range(tpp)]
            P.op("pool", lambda e: e.memset(dummy[:], 1.0), writes=allact + b1keys + ["actT"])
        P.wait_all_dma("sp")
        P.emit()
    return nc


EPS = 1e-6
D = 2048
KC = 16
NE = 8


def consts_M(C):
    t = np.arange(128)
    U = (t[:, None] < t[None, :]).astype(np.float32)
    ebase = np.tile((np.arange(NE) * C).astype(np.float32)[None, :], (128, 1))
    return {"Umat": U, "identm": np.eye(128, dtype=np.float32), "ebase": ebase}


def build_M(NT, FE, C):
    nc = bass.Bass("TRN2", target_bir_lowering=False)
    TT = 512
    ntile = NT // TT
    NS = NT // 128
    NF = FE // 128
    NSB = C // 128
    CH = C // 2
    dr = lambda n, s, k="ExternalInput", dt=F32: nc.dram_tensor(n, list(s), dt, kind=k).ap()
    ymT = dr("ymT", [KC, 128, NT])
    xT = dr("xT", [KC, 128, NT])
    wout = dr("wout", [D, D])
    lgain = dr("lgain", [128, 8])
    ngain = dr("ngain", [128, KC])
    ngrow = dr("ngrow", [1, D])
    fgrow = dr("fgrow", [1, D])
    router = dr("router", [D, NE])
    wg = dr("wg", [NE, D, FE])
    wu = dr("wu", [NE, D, FE])
    wd = dr("wd", [NE, FE, D])
    Umat = dr("Umat", [128, 128])
    identm = dr("identm", [128, 128])
    ebased = dr("ebase", [128, NE])
    out = dr("out", [NT, D], "ExternalOutput")
    cnt_out = dr("cnt_out", [128, NE], "ExternalOutput")
    x1tok_s = dr("x1tok_s", [NT, D], "Internal")
    Xe = dr("Xe", [NE * C, D], "Internal", BF16)
    Yd = dr("Yd", [NE * C, D], "Internal")

    ymv = ymT.rearrange("c p t -> p c t")
    xv = xT.rearrange("c p t -> p c t")
    woutv = wout.rearrange("(kc p) n -> p kc n", p=128)

    with ExitStack() as st:
        P = Prog(nc, st)
        sb, ps_ = P.sb, P.ps
        ones = sb("ones", [128, 128], BF16)
        ones32 = sb("ones32", [128, 128], F32)
        U = sb("U", [128, 128], F32)
        ident = sb("ident_sb", [128, 128], F32)
        identb = sb("identb", [128, 128], BF16)
        ebase = sb("ebase_sb", [128, NE], F32)
        lg = sb("lg", [128, 8], F32)
        ng = sb("ng", [128, KC], F32)
        ngb = sb("ngb", [128, D], F32)
        fgb = sb("fgb", [128, D], F32)
        rt = sb("rt", [128, KC, NE], F32)
        gr = sb("gr", [128, KC, NE], F32)
        arena = sb("arena", [128, 24576], F32)
        ym32 = arena[:, 0:8192].rearrange("p (c t) -> p c t", c=KC)
        x32 = arena[:, 8192:16384].rearrange("p (c t) -> p c t", c=KC)
        ymb = arena[:, 16384:20480].bitcast(BF16).rearrange("p (c t) -> p c t", c=KC)
        sq = arena[:, 20480:24576].bitcast(BF16).rearrange("p (c t) -> p c t", c=KC)
        o1 = 8 * C
        XeT = arena[:, 0:o1].bitcast(BF16).rearrange("p (c t) -> p c t", c=KC)
        xrow = [arena[:, o1 + i * 1024: o1 + (i + 1) * 1024].bitcast(BF16) for i in range(2)]
        o2 = o1 + 2048
        o3 = o2 + NF * C // 2
        actT = arena[:, o2:o3].bitcast(BF16).rearrange("p (f t) -> p f t", f=NF)
        ystg = [arena[:, o3 + i * 512: o3 + (i + 1) * 512] for i in range(3)]
        assert o3 + 1536 <= 24576
        cy1 = [arena[:, i * 8192: i * 8192 + 2048] for i in range(2)]
        cy2 = [arena[:, i * 8192 + 2048: i * 8192 + 4096] for i in range(2)]
        cx1 = [arena[:, i * 8192 + 4096: i * 8192 + 6144] for i in range(2)]
        cjunk = arena[:, 16384:18432]
        rs = sb("rs", [128, TT], F32)
        wo = [sb(f"wo{i}", [128, KC, 128], BF16) for i in range(2)]
        wbuf = [sb(f"wbuf{i}", [128, 8192], BF16) for i in range(2)]
        wgt = [w[:, 0:4096].rearrange("p (c n) -> p c n", c=KC) for w in wbuf]
        wut = [w[:, 4096:8192].rearrange("p (c n) -> p c n", c=KC) for w in wbuf]
        wdt = [w[:, 0:NF * 256].rearrange("p (f n) -> p f n", f=NF) for w in wbuf]
        dmy = sb("dmy", [128, 8], F32)
        x1tok = sb("x1tok", [128, D], F32)
        h2tok = sb("h2tok", [128, D], BF16)
        sg = [sb(f"sg{i}", [128, CH], F32) for i in range(2)]
        cnt = sb("cnt", [128, NE], F32)
        lgt = sb("lgt", [128, NE], F32)
        lg2 = sb("lg2", [128, NE], F32)
        mk1 = sb("mk1", [128, NE], F32)
        mk2 = sb("mk2", [128, NE], F32)
        mk = sb("mk", [128, NE], F32)
        slot = sb("slot", [128, NE], F32)
        tmp8 = sb("tmp8", [128, NE], F32)
        m12 = sb("m12", [128, 4], F32)
        ssum = sb("ssum", [128, 2], F32)
        rstd = sb("rstd", [128, 1], F32)
        dstf = sb("dstf", [128, 2], F32)
        dst = sb("dst", [128, NS, 2], I32)
        wts = sb("wts", [128, NS, 2], F32)
        ps = [ps_(f"ps{i}", [128, 512]) for i in range(8)]

        P.op("dve", lambda e: e.memset(ones[:], 1.0), writes=["ones"])
        P.op("dve", lambda e: e.memset(ones32[:], 1.0), writes=["ones32"])
        P.op("dve", lambda e: e.memset(cnt[:], 0.0), writes=["cnt"])
        P.dma("sp", lg[:], lgain, writes=["lg"])
        P.dma("sp", ng[:], ngain, writes=["ng"])
        P.dma("sp", U[:], Umat, writes=["U"])
        P.dma("sp", ident[:], identm, writes=["ident"])
        P.dma("sp", ebase[:], ebased, writes=["ebase"])
        P.dma("sp", ngb[:], ngrow.partition_broadcast(128), writes=["ngb"])
        P.dma("sp", fgb[:], fgrow.partition_broadcast(128), writes=["fgb"])
        P.dma("sp", rt[:], router.rearrange("(kc p) e -> p kc e", p=128), writes=["rt"])
        P.op("dve", lambda e: e.tensor_copy(out=identb[:], in_=ident[:]), reads=["ident"], writes=["identb"])
        for kc in range(KC):
            P.op("dve", lambda e, kc=kc: e.tensor_scalar(out=gr[:, kc, :], in0=rt[:, kc, :], scalar1=ng[:, kc:kc + 1], scalar2=None, op0=ALU.mult),
                 reads=["rt", "ng"], writes=["gr"])

        cntr = {"wo": 0, "po": 0}
        b1keys = ["ym32", "x32", "sq"] + [("ymb", j) for j in range(KC)] + [("x1", j) for j in range(KC)]
        for tq in range(ntile):
            t0 = tq * TT
            P.dma("sp", ym32, ymv[:, :, t0:t0 + TT], writes=["ym32"])
            P.dma("sp", x32, xv[:, :, t0:t0 + TT], writes=["x32"] + [("x1", j) for j in range(KC)])
            P.op("act", lambda e: e.activation(out=sq[:, 0:8, :], in_=ym32[:, 0:8, :], func=AF.Square), reads=["ym32"], writes=["sq"])
            for j in range(8):
                P.op("pe", lambda e, j=j: e.matmul(ps[0][:], ones[:], sq[:, j, :], start=(j == 0), stop=(j == 7)), reads=["ones", "sq"], writes=["ps0"])
            P.op("act", lambda e: e.activation(out=rs[:], in_=ps[0][:], func=AF.Sqrt, bias=EPS, scale=1.0 / 1024), reads=["ps0"], writes=["rs"])
            P.op("dve", lambda e: e.reciprocal(out=rs[:], in_=rs[:]), reads=["rs"], writes=["rs"])
            for j in range(8):
                P.op("dve", lambda e, j=j: e.scalar_tensor_tensor(out=ymb[:, j, :], in0=ym32[:, j, :], scalar=lg[:, j:j + 1], in1=rs[:], op0=ALU.mult, op1=ALU.mult),
                     reads=["ym32", "lg", "rs"], writes=[("ymb", j)])
            P.op("pool", lambda e: e.tensor_copy(out=ymb[:, 8:16, :], in_=ym32[:, 8:16, :]), reads=["ym32"], writes=[("ymb", j) for j in range(8, 16)])
            for dt in range(KC):
                b = cntr["wo"] % 2
                cntr["wo"] += 1
                P.dma("pool", wo[b][:], woutv[:, :, dt * 128:dt * 128 + 128], writes=[f"wo{b}"])
                pb = 1 + cntr["po"] % 2
                cntr["po"] += 1
                for kc in range(KC):
                    P.op("pe", lambda e, kc=kc, b=b, pb=pb: e.matmul(ps[pb][:], wo[b][:, kc, :], ymb[:, kc, :], start=(kc == 0), stop=(kc == KC - 1)),
                         reads=[f"wo{b}", ("ymb", kc)], writes=[f"ps{pb}"])
                P.op("dve", lambda e, dt=dt, pb=pb: e.tensor_tensor(out=x32[:, dt, :], in0=x32[:, dt, :], in1=ps[pb][:], op=ALU.add),
                     reads=[f"ps{pb}", "x32"], writes=[("x1", dt)])
            x1keys = [("x1", dt) for dt in range(KC)]
            for s4 in range(4):
                sidx = tq * 4 + s4
                ts = slice(s4 * 128, (s4 + 1) * 128)
                for kc in range(KC):
                    P.op("pe", lambda e, kc=kc, ts=ts: e.matmul(ps[3][:, 0:NE], x32[:, kc, ts], gr[:, kc, :], start=(kc == 0), stop=(kc == KC - 1)),
                         reads=[("x1", kc), "gr"], writes=["ps3"])
                for q in range(4):
                    pb = 4 + q % 2
                    for d4 in range(4):
                        dc = q * 4 + d4
                        P.op("pe", lambda e, dc=dc, d4=d4, ts=ts, pb=pb: e.transpose(ps[pb][:, d4 * 128:(d4 + 1) * 128], x32[:, dc, ts], ident[:]),
                             reads=[("x1", dc), "ident"], writes=[f"ps{pb}"])
                    P.op("act", lambda e, q=q, pb=pb: e.activation(out=x1tok[:, q * 512:(q + 1) * 512], in_=ps[pb][:], func=AF.Copy),
                         reads=[f"ps{pb}"], writes=[("x1tok", q)])
                xtk = [("x1tok", q) for q in range(4)]
                P.dma("sp", x1tok_s[tq * TT + s4 * 128: tq * TT + (s4 + 1) * 128, :], x1tok[:], reads=xtk, writes=["x1tok_s"], key="x1tst")
                P.op("act", lambda e: e.activation(out=h2tok[:], in_=x1tok[:], func=AF.Square, accum_out=ssum[:, 0:1]), reads=xtk, writes=["h2tok", "ssum"])
                P.op("act", lambda e: e.activation(out=rstd[:], in_=ssum[:, 0:1], func=AF.Sqrt, bias=EPS, scale=1.0 / D), reads=["ssum"], writes=["rstd"])
                P.op("dve", lambda e: e.reciprocal(out=rstd[:], in_=rstd[:]), reads=["rstd"], writes=["rstd"])
                P.op("dve", lambda e: e.scalar_tensor_tensor(out=h2tok[:], in0=x1tok[:], scalar=rstd[:, 0:1], in1=ngb[:], op0=ALU.mult, op1=ALU.mult),
                     reads=xtk + ["rstd", "ngb"], writes=["h2tok"])
                P.op("dve", lambda e: e.tensor_scalar(out=lgt[:], in0=ps[3][:, 0:NE], scalar1=rstd[:, 0:1], scalar2=None, op0=ALU.mult), reads=["ps3", "rstd"], writes=["lgt"])
                P.op("dve", lambda e: e.tensor_reduce(out=m12[:, 0:1], in_=lgt[:], axis=AX.X, op=ALU.max), reads=["lgt"], writes=["m1"])
                P.op("dve", lambda e: e.tensor_scalar(out=mk1[:], in0=lgt[:], scalar1=m12[:, 0:1], scalar2=None, op0=ALU.is_equal), reads=["lgt", "m1"], writes=["mk1"])
                P.op("dve", lambda e: e.scalar_tensor_tensor(out=lg2[:], in0=mk1[:], scalar=-1e30, in1=lgt[:], op0=ALU.mult, op1=ALU.add), reads=["mk1", "lgt"], writes=["lg2"])
                P.op("dve", lambda e: e.tensor_reduce(out=m12[:, 1:2], in_=lg2[:], axis=AX.X, op=ALU.max), reads=["lg2"], writes=["m2"])
                P.op("dve", lambda e: e.tensor_scalar(out=mk2[:], in0=lg2[:], scalar1=m12[:, 1:2], scalar2=None, op0=ALU.is_equal), reads=["lg2", "m2"], writes=["mk2"])
                P.op("dve", lambda e: e.tensor_tensor(out=m12[:, 2:3], in0=m12[:, 0:1], in1=m12[:, 1:2], op=ALU.subtract), reads=["m1", "m2"], writes=["md"])
                P.op("act", lambda e, sidx=sidx: e.activation(out=wts[:, sidx, 0:1], in_=m12[:, 2:3], func=AF.Sigmoid), reads=["md"], writes=[("wts", sidx)])
                P.op("dve", lambda e, sidx=sidx: e.tensor_scalar(out=wts[:, sidx, 1:2], in0=wts[:, sidx, 0:1], scalar1=-1.0, scalar2=1.0, op0=ALU.mult, op1=ALU.add),
                     reads=[("wts", sidx)], writes=[("wts2", sidx)])
                P.op("dve", lambda e: e.tensor_tensor(out=mk[:], in0=mk1[:], in1=mk2[:], op=ALU.add), reads=["mk1", "mk2"], writes=["mk"])
                P.op("pe", lambda e: e.matmul(ps[6][:, 0:NE], U[:], mk[:], start=True, stop=True), reads=["U", "mk"], writes=["ps6"])
                P.op("pe", lambda e: e.matmul(ps[6][:, NE:2 * NE], ones32[:], mk[:], start=True, stop=True), reads=["ones32", "mk"], writes=["ps6"])
                P.op("dve", lambda e: e.tensor_tensor(out=slot[:], in0=ps[6][:, 0:NE], in1=cnt[:], op=ALU.add), reads=["ps6", "cnt"], writes=["slot"])
                P.op("dve", lambda e: e.tensor_tensor(out=slot[:], in0=slot[:], in1=ebase[:], op=ALU.add), reads=["slot", "ebase"], writes=["slot"])
                P.op("dve", lambda e: e.tensor_tensor(out=cnt[:], in0=ps[6][:, NE:2 * NE], in1=cnt[:], op=ALU.add), reads=["ps6", "cnt", "slot"], writes=["cnt"])
                for k2, mkk in enumerate((mk1, mk2)):
                    P.op("dve", lambda e, mkk=mkk: e.tensor_tensor(out=tmp8[:], in0=mkk[:], in1=slot[:], op=ALU.mult), reads=["slot", "mk1", "mk2"], writes=["tmp8"])
                    P.op("dve", lambda e, k2=k2: e.tensor_reduce(out=dstf[:, k2:k2 + 1], in_=tmp8[:], axis=AX.X, op=ALU.add), reads=["tmp8"], writes=[("dstf", k2)])
                P.op("dve", lambda e, sidx=sidx: e.tensor_copy(out=dst[:, sidx, :], in_=dstf[:]), reads=[("dstf", 0), ("dstf", 1)], writes=[("dst", sidx)])
                for k2 in range(2):
                    P.idma(Xe, dst[:, sidx, k2:k2 + 1], h2tok[:], None, reads=["h2tok", ("dst", sidx)], writes=["Xe"], key=("xsc", k2), bounds=NE * C - 1)
        P.dma("sp", cnt_out, cnt[:], reads=["cnt"], key="cntout")
        P.op("pool", lambda e: e.memset(dmy[:], 1.0), writes=b1keys + ["earena"])
        ec = {"xr": 0, "w": 0, "g": 0, "y": 0}
        for ex in range(NE):
            for sbk in range(NSB):
                xb = ec["xr"] % 2
                ec["xr"] += 1
                P.dma("sp", xrow[xb], Xe[ex * C + sbk * 128: ex * C + (sbk + 1) * 128, :], reads=["Xe", "earena"], writes=[f"xrow{xb}"])
                for half in range(2):
                    pbT = ps[1 + half][:].bitcast(BF16)
                    for d8 in range(8):
                        dc = half * 8 + d8
                        P.op("pe", lambda e, xb=xb, dc=dc, d8=d8, pbT=pbT: e.transpose(pbT[:, d8 * 128:(d8 + 1) * 128], xrow[xb][:, dc * 128:(dc + 1) * 128], identb[:]),
                             reads=[f"xrow{xb}", "identb"], writes=[f"ps{1 + half}"])
                    P.op("act" if half == 0 else "dve",
                         (lambda e, half=half, sbk=sbk, pbT=pbT: e.activation(out=XeT[:, half * 8:(half + 1) * 8, sbk * 128:(sbk + 1) * 128],
                                                                             in_=pbT.rearrange("p (c t) -> p c t", c=8), func=AF.Copy)) if half == 0 else
                         (lambda e, half=half, sbk=sbk, pbT=pbT: e.tensor_copy(out=XeT[:, half * 8:(half + 1) * 8, sbk * 128:(sbk + 1) * 128],
                                                                              in_=pbT.rearrange("p (c t) -> p c t", c=8))),
                         reads=[f"ps{1 + half}", "earena"], writes=[("XeT", sbk, half)])
            xek = [("XeT", sbk, half) for sbk in range(NSB) for half in range(2)]
            for fc in range(FE // 256):
                b = ec["w"] % 2
                ec["w"] += 1
                P.dma("pool", wgt[b], wg[ex].rearrange("(kc p) n -> p kc n", p=128)[:, :, fc * 256:(fc + 1) * 256], writes=[f"wbuf{b}"], key=f"wg{b}")
                P.dma("pool", wut[b], wu[ex].rearrange("(kc p) n -> p kc n", p=128)[:, :, fc * 256:(fc + 1) * 256], writes=[f"wbufu{b}"], key=f"wu{b}")
                for half in range(2):
                    f = fc * 2 + half
                    for ch in range(2):
                        g = ec["g"] % 2
                        ec["g"] += 1
                        pg, pu = 3 + g, 5 + g
                        cs = slice(ch * CH, (ch + 1) * CH)
                        for kc in range(KC):
                            P.op("pe", lambda e, kc=kc, b=b, pg=pg, half=half, cs=cs: e.matmul(ps[pg][:, 0:CH], wgt[b][:, kc, half * 128:(half + 1) * 128], XeT[:, kc, cs],
                                                                                               start=(kc == 0), stop=(kc == KC - 1)),
                                 reads=[f"wbuf{b}"] + xek, writes=[f"ps{pg}"])
                        for kc in range(KC):
                            P.op("pe", lambda e, kc=kc, b=b, pu=pu, half=half, cs=cs: e.matmul(ps[pu][:, 0:CH], wut[b][:, kc, half * 128:(half + 1) * 128], XeT[:, kc, cs],
                                                                                               start=(kc == 0), stop=(kc == KC - 1)),
                                 reads=[f"wbufu{b}"] + xek, writes=[f"ps{pu}"])
                        P.op("act", lambda e, g=g, pg=pg: e.activation(out=sg[g][:], in_=ps[pg][:, 0:CH], func=AF.Silu), reads=[f"ps{pg}"], writes=[f"sg{g}"])
                        P.op("dve", lambda e, g=g, pu=pu, f=f, cs=cs: e.tensor_tensor(out=actT[:, f, cs], in0=sg[g][:], in1=ps[pu][:, 0:CH], op=ALU.mult),
                             reads=[f"sg{g}", f"ps{pu}", "earena"], writes=[("act", f)])
            actk = [("act", f) for f in range(NF)]
            for dc in range(D // 256):
                b = ec["w"] % 2
                ec["w"] += 1
                P.dma("pool", wdt[b], wd[ex].rearrange("(f p) n -> p f n", p=128)[:, :, dc * 256:(dc + 1) * 256], writes=[f"wbuf{b}", f"wbufu{b}"], key=f"wg{b}")
                for sbk in range(NSB):
                    pb = 1 + cntr["po"] % 2
                    cntr["po"] += 1
                    for f in range(NF):
                        P.op("pe", lambda e, f=f, b=b, pb=pb, sbk=sbk: e.matmul(ps[pb][:, 0:256], actT[:, f, sbk * 128:(sbk + 1) * 128], wdt[b][:, f, :],
                                                                               start=(f == 0), stop=(f == NF - 1)),
                             reads=[f"wbuf{b}"] + actk, writes=[f"ps{pb}"])
                    yb = ec["y"] % 3
                    ec["y"] += 1
                    P.op("act", lambda e, yb=yb, pb=pb: e.activation(out=ystg[yb][:, 0:256], in_=ps[pb][:, 0:256], func=AF.Copy),
                         reads=[f"ps{pb}", "earena"], writes=[f"ystg{yb}"])
                    P.dma("sp", Yd[ex * C + sbk * 128: ex * C + (sbk + 1) * 128, dc * 256:(dc + 1) * 256], ystg[yb][:, 0:256], reads=[f"ystg{yb}"], writes=[("Yd", ex)], key=f"yst{yb}")
        retire = [("XeT", sbk, half) for sbk in range(NSB) for half in range(2)] + [("act", f) for f in range(NF)] + \
                 [f"ystg{i}" for i in range(3)] + ["xrow0", "xrow1", "earena"]
        P.op("pool", lambda e: e.memset(dmy[:], 1.0), writes=retire + ["carena"])
        for sidx in range(NS):
            cb_ = sidx % 2
            P.idma(cy1[cb_], None, Yd, dst[:, sidx, 0:1], reads=[("Yd", ex_) for ex_ in range(NE)] + [("dst", sidx), "carena"], writes=[f"cy1_{cb_}"], key=("g1", cb_), bounds=NE * C - 1)
            P.idma(cy2[cb_], None, Yd, dst[:, sidx, 1:2], reads=[("Yd", ex_) for ex_ in range(NE)] + [("dst", sidx), "carena"], writes=[f"cy2_{cb_}"], key=("g2", cb_), bounds=NE * C - 1)
            P.dma("sp", cx1[cb_], x1tok_s[sidx * 128:(sidx + 1) * 128, :], reads=["x1tok_s", "carena"], writes=[f"cx1_{cb_}"])
            P.op("dve", lambda e, cb_=cb_, sidx=sidx: e.scalar_tensor_tensor(out=cx1[cb_], in0=cy1[cb_], scalar=wts[:, sidx, 0:1], in1=cx1[cb_], op0=ALU.mult, op1=ALU.add),
                 reads=[f"cy1_{cb_}", f"cx1_{cb_}", ("wts", sidx)], writes=[f"cx1_{cb_}"])
            P.op("dve", lambda e, cb_=cb_, sidx=sidx: e.scalar_tensor_tensor(out=cx1[cb_], in0=cy2[cb_], scalar=wts[:, sidx, 1:2], in1=cx1[cb_], op0=ALU.mult, op1=ALU.add),
                 reads=[f"cy2_{cb_}", f"cx1_{cb_}", ("wts2", sidx)], writes=[f"cx1_{cb_}"])
            P.op("act", lambda e, cb_=cb_: e.activation(out=cy1[cb_], in_=cx1[cb_], func=AF.Square, accum_out=ssum[:, 1:2]), reads=[f"cx1_{cb_}"], writes=[f"cy1_{cb_}", "ssum2"])
            P.op("act", lambda e: e.activation(out=rstd[:], in_=ssum[:, 1:2], func=AF.Sqrt, bias=EPS, scale=1.0 / D), reads=["ssum2"], writes=["rstd"])
            P.op("dve", lambda e: e.reciprocal(out=rstd[:], in_=rstd[:]), reads=["rstd"], writes=["rstd"])
            P.op("dve", lambda e, cb_=cb_: e.scalar_tensor_tensor(out=cy2[cb_], in0=cx1[cb_], scalar=rstd[:, 0:1], in1=fgb[:], op0=ALU.mult, op1=ALU.mult),
                 reads=[f"cx1_{cb_}", "rstd", "fgb"], writes=[f"cy2_{cb_}"])
            P.dma("sp", out[sidx * 128:(sidx + 1) * 128, :], cy2[cb_], reads=[f"cy2_{cb_}"], key=("ost", cb_))
        P.wait_all_dma("sp")
        P.emit()
    return nc


def fm(a):
    T, C = a.shape
    return np.ascontiguousarray(a.T.reshape(C // 128, 128, T))

def pcols(v):
    return np.ascontiguousarray(v.reshape(-1, 128).T)

def prep_A(inp, l, j):
    DL = 1024
    w_in = inp["w_in"][l]
    blk = [2 * j, 2 * j + 1]
    cols = []
    for n in blk: cols.append(np.arange(n * 128, (n + 1) * 128))
    for n in blk: cols.append(DL + np.arange(n * 128, (n + 1) * 128))
    for part in range(3):
        for h in blk: cols.append(2 * DL + part * 1024 + np.arange(h * 128, (h + 1) * 128))
    for h in blk: cols.append(2 * DL + 3 * 1024 + np.arange(h * 128, (h + 1) * 128))
    base = 2 * DL + 4 * 1024
    cols.append(np.array([base + blk[0], base + blk[1], base + 8 + blk[0], base + 8 + blk[1]]))
    cols = np.concatenate(cols)
    wc = np.ascontiguousarray(w_in[:, cols])
    lru_p = np.zeros((128, 2, 8), np.float32)
    lru_w = np.zeros((128, 2, 2, 128), np.float32)
    for i, n in enumerate(blk):
        sl = slice(n * 128, (n + 1) * 128)
        lru_p[:, i, 0:4] = inp["conv_lru_w"][l][:, sl].T
        lru_p[:, i, 4] = inp["conv_lru_b"][l][sl]
        lru_p[:, i, 5] = inp["lru_b_r"][l][n]
        lru_p[:, i, 6] = inp["lru_b_i"][l][n]
        lru_p[:, i, 7] = inp["lru_lambda"][l][sl]
        lru_w[:, i, 0, :] = inp["lru_w_r"][l][n]
        lru_w[:, i, 1, :] = inp["lru_w_i"][l][n]
    dn_cw = np.zeros((128, 6, 4), np.float32)
    cq = inp["conv_qkv_w"][l]
    for part in range(3):
        for i, h in enumerate(blk):
            dn_cw[:, part * 2 + i, :] = cq[:, part * 1024 + h * 128: part * 1024 + (h + 1) * 128].T
    dn_p4 = np.zeros((4, 2), np.float32)
    dn_p4[2:4, 0] = inp["dn_dt_bias"][l][blk]
    dn_p4[2:4, 1] = inp["dn_a_log"][l][blk]
    return {"wc": wc, "ngain": pcols(inp["norm_mix"][l]), "lru_p": lru_p, "lru_w": lru_w, "dn_cw": dn_cw,
            "dn_p4": dn_p4, "dn_g": np.ascontiguousarray(inp["dn_out_norm"][l].reshape(128, 1))}

S_FULL = 8192
NTC = 2048
CAP = 1024
FE_ = 3072
FF_ = 6144


def _fm(a):
    T, C = a.shape
    return np.ascontiguousarray(a.T.reshape(C // 128, 128, T))


def _pcols(v):
    return np.ascontiguousarray(v.reshape(-1, 128).T)


_NC_CACHE = {}
LAST_COUNTS = None


def _get(name, fn):
    if name not in _NC_CACHE:
        _NC_CACHE[name] = fn()
    return _NC_CACHE[name]


def kernel(**inp):
    inp = {k: np.asarray(v) for k, v in inp.items()}
    x = inp["x"].astype(np.float32)
    cores = list(range(8))
    cstA = consts_A()
    cstM = consts_M(CAP)
    out = None
    for l in range(2):
        ncA = _get("A", lambda: build_A(S_FULL))
        xTb = [_fm(x[b]) for b in range(2)]
        maps = []
        for c in cores:
            b, j = c // 4, c % 4
            m = {"xT": xTb[b]}
            m.update(prep_A(inp, l, j))
            m.update(cstA)
            maps.append(m)
        resA = run_bass_kernel_spmd(ncA, maps, core_ids=cores)
        yA = [r["yT"] for r in resA.results]
        del maps
        mapsB = []
        for c in cores:
            b, q = c // 4, c % 4
            ts = slice(q * NTC, (q + 1) * NTC)
            ymT = np.empty((16, 128, NTC), np.float32)
            for n in range(8):
                ymT[n] = yA[b * 4 + n // 2][n % 2][:, ts]
                ymT[8 + n] = yA[b * 4 + n // 2][2 + n % 2][:, ts]
            m = {"ymT": ymT, "xT": np.ascontiguousarray(xTb[b][:, :, ts]), "wout": inp["w_out"][l],
                 "lgain": _pcols(inp["lru_out_norm"][l]), "ngain": _pcols(inp["norm_ffn"][l])}
            if l == 0:
                m.update({"wg": inp["ffn_w_gate"][0], "wu": inp["ffn_w_up"][0], "wd": inp["ffn_w_down"][0]})
            else:
                m.update({"ngrow": np.ascontiguousarray(inp["norm_ffn"][l].reshape(1, -1)),
                          "fgrow": np.ascontiguousarray(inp["norm_final"].reshape(1, -1)),
                          "router": inp["moe_router"][0], "wg": inp["moe_w_gate"][0], "wu": inp["moe_w_up"][0],
                          "wd": inp["moe_w_down"][0]})
                m.update(cstM)
            mapsB.append(m)
        del yA
        if l == 0:
            ncB = _get("B", lambda: build_B(NTC, FF_))
            resB = run_bass_kernel_spmd(ncB, mapsB, core_ids=cores)
            xn = np.empty_like(x)
            for c in cores:
                b, q = c // 4, c % 4
                xn[b, q * NTC:(q + 1) * NTC, :] = resB.results[c]["x2T"].reshape(2048, NTC).T
            x = xn
        else:
            ncM = _get("M", lambda: build_M(NTC, FE_, CAP))
            resM = run_bass_kernel_spmd(ncM, mapsB, core_ids=cores)
            out = np.empty((2, S_FULL, 2048), np.float32)
            for c in cores:
                b, q = c // 4, c % 4
                out[b, q * NTC:(q + 1) * NTC, :] = resM.results[c]["out"]
            global LAST_COUNTS
            LAST_COUNTS = np.stack([resM.results[c]["cnt_out"][0] for c in cores])
        del mapsB
    return out
```

```python
import numpy as np
from contextlib import ExitStack
import concourse.bass as bass
import concourse.mybir as mybir
from concourse.bass_utils import run_bass_kernel_spmd


F32 = mybir.dt.float32
BF16 = mybir.dt.bfloat16
I32 = mybir.dt.int32
AF = mybir.ActivationFunctionType
ALU = mybir.AluOpType
AX = mybir.AxisListType

SEM_ROLL = 30000


class Prog:
    ENGS = ("pe", "dve", "act", "pool", "sp")

    def __init__(self, nc, stack):
        self.nc = nc
        self.stack = stack
        self.q = {e: [] for e in self.ENGS}
        self.cnt = {e: 0 for e in self.ENGS}
        self.sem = {}
        self.nsem = 0
        for e in self.ENGS:
            self.sem[e] = self._newsem("e_" + e)
        self.seen = {e: {} for e in self.ENGS}
        self.lastw = {}
        self.readers = {}
        self.dsem = {}
        self.n_ops = 0

    def _newsem(self, name):
        self.nsem += 1
        sm = self.stack.enter_context(self.nc.semaphore(f"{name}_{self.nsem}"))
        if not hasattr(self, "semname"):
            self.semname = {}
        self.semname[id(sm)] = f"{name}_{self.nsem}"
        return sm

    def sb(self, name, shape, dt):
        return self.stack.enter_context(self.nc.sbuf_tensor(name, list(shape), dt))

    def ps(self, name, shape, dt=F32):
        return self.stack.enter_context(self.nc.psum_tensor(name, list(shape), dt))

    def _deps(self, eng, reads, writes):
        need = []
        for k in reads:
            w = self.lastw.get(k)
            if w is not None:
                need.append(w)
        for k in writes:
            w = self.lastw.get(k)
            if w is not None:
                need.append(w)
            need.extend(self.readers.get(k, ()))
        best = {}
        for s, v in need:
            if best.get(id(s), (None, -1))[1] < v:
                best[id(s)] = (s, v)
        out = []
        seen = self.seen[eng]
        for sid, (s, v) in best.items():
            if eng == "pe" and s is self.sem["pe"]:
                continue
            if seen.get(sid, 0) >= v:
                continue
            seen[sid] = v
            out.append((s, v))
        return out

    def _commit(self, reads, writes, tok):
        for k in writes:
            self.lastw[k] = tok
            self.readers[k] = []
        for k in reads:
            self.readers.setdefault(k, []).append(tok)

    def op(self, eng, fn, reads=(), writes=()):
        if self.cnt[eng] >= SEM_ROLL:
            self.sem[eng] = self._newsem("e_" + eng)
            self.cnt[eng] = 0
        waits = self._deps(eng, reads, writes)
        self.cnt[eng] += 1
        sem = self.sem[eng]
        tok = (sem, self.cnt[eng])
        self.q[eng].append((fn, waits, sem, 1))
        self._commit(reads, writes, tok)
        self.n_ops += 1
        if getattr(self, "log", None) is not None:
            self.log.append((eng, self.cnt[eng], list(reads), list(writes), [(self.semname.get(id(s), "?"), v) for s, v in waits]))

    def dma(self, eng, out, in_, reads=(), writes=(), key=None, **kw):
        assert eng in ("sp", "pool", "act")
        if key is None:
            key = ("dma",) + tuple(writes) + tuple(reads)
        if key not in self.dsem:
            self.dsem[key] = [self._newsem("d"), 0]
        ent = self.dsem[key]
        sem = ent[0]
        waits = self._deps(eng, reads, writes)
        if ent[1] > 0 and self.seen[eng].get(id(sem), 0) < ent[1]:
            self.seen[eng][id(sem)] = ent[1]
            waits.append((sem, ent[1]))
        ent[1] += 16
        tok = (sem, ent[1])

        def fn(e, out=out, in_=in_, kw=kw):
            o = out(e) if callable(out) else out
            i = in_(e) if callable(in_) else in_
            return e.dma_start(out=o, in_=i, **kw)
        self.q[eng].append((fn, waits, sem, 16))
        self._commit(reads, writes, tok)
        self.n_ops += 1
        return tok

    def idma(self, out, out_off, in_, in_off, reads=(), writes=(), key=None, bounds=None):
        eng = "pool"
        if key not in self.dsem:
            self.dsem[key] = [self._newsem("d"), 0]
        ent = self.dsem[key]
        sem = ent[0]
        waits = self._deps(eng, reads, writes)
        if ent[1] > 0 and self.seen[eng].get(id(sem), 0) < ent[1]:
            self.seen[eng][id(sem)] = ent[1]
            waits.append((sem, ent[1]))
        ent[1] += 16
        tok = (sem, ent[1])

        def fn(e):
            oo = bass.IndirectOffsetOnAxis(ap=out_off, axis=0) if out_off is not None else None
            io = bass.IndirectOffsetOnAxis(ap=in_off, axis=0) if in_off is not None else None
            bc = None
            if bounds is not None:
                regs = self.__dict__.setdefault("_bregs", {})
                if bounds not in regs:
                    regs[bounds] = e.to_reg(bounds)
                bc = regs[bounds]
            return e.indirect_dma_start(out=out, out_offset=oo, in_=in_, in_offset=io, bounds_check=bc, oob_is_err=False)
        self.q[eng].append((fn, waits, sem, 16))
        self._commit(reads, writes, tok)
        return tok

    def wait_all_dma(self, eng="sp"):
        waits = []
        for key, (sem, val) in self.dsem.items():
            if val > 0:
                waits.append((sem, val))
        self.q[eng].append((None, waits, None, 0))

    def emit(self):
        nc = self.nc
        engmap = {"pe": "tensor", "dve": "vector", "act": "scalar", "pool": "gpsimd", "sp": "sync"}
        with nc.Block() as block:
            for ename in self.ENGS:
                lst = self.q[ename]

                def body(e, lst=lst):
                    for fn, waits, sem, inc in lst:
                        for s, v in waits:
                            e.wait_ge(s, v)
                        if fn is not None:
                            fn(e).then_inc(sem, inc)
                getattr(block, engmap[ename])(body)


EPS = 1e-6
D = 2048
KC = 16
NCOL = 1540
TB = 512
NCH = TB // 64


def consts_A():
    i = np.arange(64)
    ms = (i[:, None] > i[None, :]).astype(np.float32)
    msT = (i[:, None] < i[None, :]).astype(np.float32)
    miT = (i[:, None] <= i[None, :]).astype(np.float32)
    idn = np.eye(64, dtype=np.float32)
    c64 = np.stack([np.tile(m, (1, NCH)) for m in (ms, msT, miT, idn)], 0)
    c64p = np.zeros((4, 128, TB), np.float32)
    c64p[:, :64] = c64
    ident = np.eye(128, dtype=np.float32)
    small = np.zeros((128, 4 + 4 * 128 + TB), np.float32)
    small[:4, 0:4] = np.eye(4)
    for r in range(4):
        small[r, 4 + r * 128: 4 + (r + 1) * 128] = 1.0
    rm = np.ones(TB, np.float32)
    rm[::64] = 0.0
    small[:4, 4 + 512:] = rm[None, :]
    return {"c64": c64p, "ident": ident, "small": small}


HORDER = [0, 1]
INV_BF16 = True
DEBUG = False


def build_A(S):
    nc = bass.Bass("TRN2", target_bir_lowering=False)
    NB = S // TB
    dr = lambda n, s, k="ExternalInput": nc.dram_tensor(n, list(s), F32, kind=k).ap()
    xT = dr("xT", [KC, 128, S])
    wc = dr("wc", [D, NCOL])
    ngain = dr("ngain", [128, KC])
    lru_p = dr("lru_p", [128, 2, 8])
    lru_w = dr("lru_w", [128, 2, 2, 128])
    dn_cw = dr("dn_cw", [128, 6, 4])
    dn_p4 = dr("dn_p4", [4, 2])
    dn_g = dr("dn_g", [128, 1])
    c64 = dr("c64", [4, 128, TB])
    identd = dr("ident", [128, 128])
    smalld = dr("small", [128, 4 + 512 + TB])
    yT = dr("yT", [4, 128, S], "ExternalOutput")
    dbg = dr("dbg", [24, 128, TB], "ExternalOutput") if DEBUG else None

    xv = xT.rearrange("c p t -> p c t")
    wcv = wc.rearrange("(kc p) n -> p kc n", p=128)

    with ExitStack() as st:
        P = Prog(nc, st)
        sb, ps_ = P.sb, P.ps
        W = sb("W", [128, KC, NCOL], BF16)
        ng = sb("ng", [128, KC], F32)
        lp = sb("lp", [128, 2, 8], F32)
        lw32 = sb("lw32", [128, 2, 2, 128], F32)
        lw = sb("lw", [128, 2, 2, 128], BF16)
        c1 = sb("c1", [128, 2], F32)
        dcw = sb("dcw", [128, 6, 4], F32)
        p4 = sb("p4", [4, 2], F32)
        negA = sb("negA", [4, 1], F32)
        dng = sb("dng", [128, 1], F32)
        cm = sb("cm", [128, 4, TB], BF16)
        ident = sb("identf", [128, 128], F32)
        identb = sb("identb", [128, 128], BF16)
        small = sb("smallc", [128, 4 + 512 + TB], F32)
        ones = sb("ones", [128, 128], BF16)
        I4 = small[0:4, 0:4]
        sel = lambda r: small[0:4, 4 + r * 128: 4 + (r + 1) * 128]
        rmask = small[0:4, 4 + 512: 4 + 512 + TB]
        Ms, MsT, MiT, Id8 = cm[0:64, 0, :], cm[0:64, 1, :], cm[0:64, 2, :], cm[0:64, 3, :]

        x32 = [sb(f"x32_{k}", [128, 4, TB], F32) for k in range(2)]
        sq = sb("sq", [128, 4, TB], BF16)
        hT = sb("hT", [128, KC, TB], BF16)
        rs = sb("rs", [128, TB], F32)
        xbuf = [sb(f"xbuf{n}", [128, TB + 3], F32) for n in range(2)]
        hlast = [sb(f"hlast{n}", [128, 1], F32) for n in range(2)]
        gl = [sb(f"gl{n}", [128, TB], F32) for n in range(2)]
        cb = [sb(f"cb{k}", [128, TB + 3], F32) for k in range(6)]
        sgate = [sb(f"sgate{h}", [128, TB], F32) for h in range(2)]
        r4 = sb("r4", [4, 5, TB], F32)
        colt = sb("colt", [64, NCH, 16], F32)
        cole = sb("cole", [64, NCH, 2], F32)
        qkv = [sb(f"qkv{k}", [128, TB], F32) for k in range(3)]
        qkvb = [sb(f"qkvb{k}", [128, TB], BF16) for k in range(3)]
        Gb = sb("Gb", [128, TB], F32)
        betab = sb("betab", [64, TB], F32)
        EGb = sb("EGb", [128, TB], F32)
        qdTb = sb("qdTb", [128, TB], BF16)
        t1 = sb("t1", [64, TB], F32)
        eT = sb("eT", [64, TB], F32)
        eLf = sb("eLf", [128, TB], F32)
        eL = eLf[0:64, :]
        IDT = BF16 if INV_BF16 else F32
        Nm = [sb(f"Nm{k}", [64, TB], IDT) for k in range(2)]
        NmT = [sb(f"NmT{k}", [64, TB], IDT) for k in range(2)]
        Pm = sb("Pm", [64, TB], IDT)
        qkT = sb("qkT", [64, TB], BF16)
        vb = sb("vb", [64, NCH, 128], IDT)
        kbg = sb("kbg", [64, NCH, 128], IDT)
        kdec = sb("kdec", [64, NCH, 128], BF16)
        u_t = sb("u_t", [64, NCH, 128], F32)
        wTb = sb("wTb", [128, TB], BF16)
        vnew = sb("vnew", [64, 128], BF16)
        S32 = [sb(f"S32_{h}", [128, 128], F32) for h in range(2)]
        Sb = [sb(f"Sb_{h}", [128, 128], BF16) for h in range(2)]
        o32 = sb("o32", [128, TB], F32)
        yo = [sb(f"yo{k}", [128, TB], F32) for k in range(2)]
        l2r0 = sb("l2r0", [128, TB], F32)
        xc, r_t, i_t = qkv[0], qkv[1], qkv[2]
        xcb = qkvb[0]
        a_t, m_t, h_t = Gb, EGb, o32
        XC, XCB, RT, IT, ATk, MTk, HTk = "qkv0", "qkvb0", "qkv1", "qkv2", "Gb", "EGb", "o32"
        def t1_full(k):
            return l2r0[:] if k == 0 else eLf[:]
        L2K = ["l2r0", "eL"]

        psn = ps_("psn", [128, 512])
        pp = [ps_(f"pp{k}", [128, 512]) for k in range(2)]
        pA = ps_("pA", [128, 512])
        pB = ps_("pB", [128, 512])
        pC = ps_("pC", [128, 512])
        pTk = pC[:].bitcast(BF16)
        pR = ps_("pR", [128, 512])
        pO = ps_("pO", [128, 512])

        P.dma("sp", ng[:], ngain, writes=["ng"])
        P.dma("sp", lp[:], lru_p, writes=["lp"])
        P.dma("sp", lw32[:], lru_w, writes=["lw32"])
        P.dma("sp", dcw[:], dn_cw, writes=["dcw"])
        P.dma("sp", p4[:], dn_p4, writes=["p4"])
        P.dma("sp", dng[:], dn_g, writes=["dng"])
        P.dma("pool", cm[:], c64.rearrange("m p t -> p m t"), writes=["cm"])
        P.dma("sp", ident[:], identd, writes=["ident"])
        P.dma("sp", small[:], smalld, writes=["small"])
        for k4 in range(4):
            c0 = k4 * 385
            P.dma("pool", W[:, :, c0:c0 + 385], wcv[:, :, c0:c0 + 385], writes=[("W", k4)], key=("W", k4))
        Wk = [("W", k4) for k4 in range(4)]
        P.op("dve", lambda e: e.memset(ones[:], 1.0), writes=["ones"])
        P.op("dve", lambda e: e.tensor_copy(out=identb[:], in_=ident[:]), reads=["ident"], writes=["identb"])
        P.op("dve", lambda e: e.tensor_copy(out=lw[:], in_=lw32[:]), reads=["lw32"], writes=["lw"])
        P.op("act", lambda e: e.activation(out=c1[:], in_=lp[:, :, 7], func=AF.Exp, scale=-1.0), reads=["lp"], writes=["c1"])
        P.op("act", lambda e: e.activation(out=c1[:], in_=c1[:], func=AF.Ln, bias=1.0), reads=["c1"], writes=["c1"])
        P.op("dve", lambda e: e.tensor_scalar(out=c1[:], in0=c1[:], scalar1=-8.0, scalar2=None, op0=ALU.mult), reads=["c1"], writes=["c1"])
        P.op("act", lambda e: e.activation(out=negA[:], in_=p4[:, 1:2], func=AF.Exp), reads=["p4"], writes=["negA"])
        P.op("dve", lambda e: e.tensor_scalar(out=negA[:], in0=negA[:], scalar1=-1.0, scalar2=None, op0=ALU.mult), reads=["negA"], writes=["negA"])
        for n in range(2):
            P.op("pool", lambda e, n=n: e.memset(xbuf[n][:, 0:3], 0.0), writes=[f"xbuf{n}"])
            P.op("pool", lambda e, n=n: e.memset(hlast[n][:], 0.0), writes=[f"hlast{n}"])
            P.op("pool", lambda e, n=n: e.memset(S32[n][:], 0.0), writes=[f"S32_{n}"])
            P.op("pool", lambda e, n=n: e.memset(Sb[n][:], 0.0), writes=[f"Sb_{n}"])
        for k in range(6):
            P.op("pool", lambda e, k=k: e.memset(cb[k][:, 0:3], 0.0), writes=[f"cb{k}"])

        def conv(eng, out, buf, wtile, widx, bias, rk, wk, outk):
            if bias is None:
                P.op(eng, lambda e: e.tensor_scalar(out=out, in0=buf[:, 0:TB], scalar1=wtile[:, widx, 0:1], scalar2=None, op0=ALU.mult),
                     reads=[rk, wk], writes=[outk])
            else:
                P.op(eng, lambda e: e.tensor_scalar(out=out, in0=buf[:, 0:TB], scalar1=wtile[:, widx, 0:1], scalar2=bias, op0=ALU.mult, op1=ALU.add),
                     reads=[rk, wk], writes=[outk])
            for k in range(1, 4):
                P.op(eng, lambda e, k=k: e.scalar_tensor_tensor(out=out, in0=buf[:, k:k + TB], scalar=wtile[:, widx, k:k + 1], in1=out,
                                                              op0=ALU.mult, op1=ALU.add),
                     reads=[rk, wk, outk], writes=[outk])

        ycnt = [0]
        dcnt = [0]

        def dump(ap, key, npart=128):
            if not DEBUG:
                return
            slot = dcnt[0]
            dcnt[0] += 1
            P.dma("sp", dbg[slot, 0:npart, :], ap, reads=[key], key=("dbg", slot))

        def store_y(tile_idx, t0, src_key, src):
            P.dma("sp", yT[tile_idx, :, t0:t0 + TB], src, reads=[src_key], key=("yst", src_key))

        for blk in range(NB):
            t0 = blk * TB
            for q4 in range(4):
                xb = x32[q4 % 2]
                xk = f"x32_{q4 % 2}"
                P.dma("sp", xb[:], xv[:, q4 * 4:(q4 + 1) * 4, t0:t0 + TB], writes=[xk])
                P.op("act", lambda e, xb=xb: e.activation(out=sq[:], in_=xb[:], func=AF.Square), reads=[xk], writes=["sq"])
                for j in range(4):
                    jj = q4 * 4 + j
                    P.op("pe", lambda e, j=j, jj=jj: e.matmul(psn[:], ones[:], sq[:, j, :], start=(jj == 0), stop=(jj == KC - 1)),
                         reads=["ones", "sq"], writes=["psn"])
                    P.op("dve", lambda e, j=j, jj=jj, xb=xb: e.tensor_scalar(out=hT[:, jj, :], in0=xb[:, j, :], scalar1=ng[:, jj:jj + 1], scalar2=None, op0=ALU.mult),
                         reads=[xk, "ng"], writes=[("hT", jj)])
            P.op("act", lambda e: e.activation(out=rs[:], in_=psn[:], func=AF.Ln, bias=EPS, scale=1.0 / D), reads=["psn"], writes=["rs"])
            P.op("act", lambda e: e.activation(out=rs[:], in_=rs[:], func=AF.Exp, scale=-0.5), reads=["rs"], writes=["rs"])

            def proj(ct, M):
                pb = pp[ct % 2]
                c0 = ct * 128
                for kc in range(KC):
                    P.op("pe", lambda e, kc=kc: e.matmul(pb[0:M, :], W[:, kc, c0:c0 + M], hT[:, kc, :], start=(kc == 0), stop=(kc == KC - 1)),
                         reads=Wk + [("hT", kc)], writes=[f"pp{ct % 2}"])
                return pb, f"pp{ct % 2}"

            for n in range(2):
                pb, pk = proj(n, 128)
                P.op("dve", lambda e, n=n, pb=pb: e.tensor_tensor(out=xbuf[n][:, 3:3 + TB], in0=pb[:], in1=rs[:], op=ALU.mult),
                     reads=[pk, "rs"], writes=[f"xbuf{n}"])
            for n in range(2):
                pb, pk = proj(2 + n, 128)
                P.op("dve", lambda e, n=n, pb=pb: e.tensor_tensor(out=gl[n][:], in0=pb[:], in1=rs[:], op=ALU.mult),
                     reads=[pk, "rs"], writes=[f"gl{n}"])
                P.op("act", lambda e, n=n: e.activation(out=gl[n][:], in_=gl[n][:], func=AF.Gelu_apprx_tanh), reads=[f"gl{n}"], writes=[f"gl{n}"])
            for k in range(6):
                pb, pk = proj(4 + k, 128)
                P.op("dve", lambda e, k=k, pb=pb: e.tensor_tensor(out=cb[k][:, 3:3 + TB], in0=pb[:], in1=rs[:], op=ALU.mult),
                     reads=[pk, "rs"], writes=[f"cb{k}"])
            for h in range(2):
                pb, pk = proj(10 + h, 128)
                P.op("dve", lambda e, h=h, pb=pb: e.tensor_tensor(out=sgate[h][:], in0=pb[:], in1=rs[:], op=ALU.mult),
                     reads=[pk, "rs"], writes=[f"sgate{h}"])
                P.op("act", lambda e, h=h: e.activation(out=sgate[h][:], in_=sgate[h][:], func=AF.Silu), reads=[f"sgate{h}"], writes=[f"sgate{h}"])
            pb, pk = proj(12, 4)
            P.op("dve", lambda e, pb=pb: e.tensor_tensor(out=r4[:, 0, :], in0=pb[0:4, :], in1=rs[0:4, :], op=ALU.mult),
                 reads=[pk, "rs"], writes=["s4"])

            for n in range(2):
                conv("dve", xc[:], xbuf[n], lp, n, lp[:, n, 4:5], f"xbuf{n}", "lp", XC)
                P.op("pool", lambda e, n=n: e.tensor_copy(out=xbuf[n][:, 0:3], in_=xbuf[n][:, TB:TB + 3]), reads=[XC], writes=[f"xbuf{n}"])
                P.op("act", lambda e: e.activation(out=xcb[:], in_=xc[:], func=AF.Copy), reads=[XC], writes=[XCB])
                P.op("pe", lambda e, n=n: e.matmul(pA[:], lw[:, n, 0, :], xcb[:], start=True, stop=True), reads=["lw", XCB], writes=["pA"])
                P.op("pe", lambda e, n=n: e.matmul(pB[:], lw[:, n, 1, :], xcb[:], start=True, stop=True), reads=["lw", XCB], writes=["pB"])
                P.op("act", lambda e, n=n: e.activation(out=r_t[:], in_=pA[:], func=AF.Sigmoid, bias=lp[:, n, 5:6]), reads=["pA", "lp"], writes=[RT])
                P.op("act", lambda e, n=n: e.activation(out=i_t[:], in_=pB[:], func=AF.Sigmoid, bias=lp[:, n, 6:7]), reads=["pB", "lp"], writes=[IT])
                P.op("act", lambda e, n=n: e.activation(out=a_t[:], in_=r_t[:], func=AF.Exp, scale=c1[:, n:n + 1]), reads=[RT, "c1"], writes=[ATk])
                P.op("act", lambda e: e.activation(out=m_t[:], in_=a_t[:], func=AF.Square), reads=[ATk], writes=[MTk])
                P.op("act", lambda e: e.activation(out=m_t[:], in_=m_t[:], func=AF.Sqrt, bias=1.0, scale=-1.0), reads=[MTk], writes=[MTk])
                P.op("dve", lambda e: e.tensor_tensor(out=i_t[:], in0=i_t[:], in1=xc[:], op=ALU.mult), reads=[IT, XC], writes=[IT])
                P.op("dve", lambda e: e.tensor_tensor(out=m_t[:], in0=m_t[:], in1=i_t[:], op=ALU.mult), reads=[MTk, IT], writes=[MTk])
                P.op("dve", lambda e, n=n: e.tensor_tensor_scan(out=h_t[:], data0=a_t[:], data1=m_t[:], initial=hlast[n][:, 0:1],
                                                               op0=ALU.mult, op1=ALU.add),
                     reads=[ATk, MTk, f"hlast{n}"], writes=[HTk])
                P.op("pool", lambda e, n=n: e.tensor_copy(out=hlast[n][:], in_=h_t[:, TB - 1:TB]), reads=[HTk], writes=[f"hlast{n}"])
                yk = ycnt[0] % 2
                ycnt[0] += 1
                P.op("dve", lambda e, n=n, yk=yk: e.tensor_tensor(out=yo[yk][:], in0=h_t[:], in1=gl[n][:], op=ALU.mult),
                     reads=[HTk, f"gl{n}"], writes=[f"yo{yk}"])
                store_y(n, t0, f"yo{yk}", yo[yk][:])

            s4, B4, g4, G4, EG4 = (r4[:, k, :] for k in range(5))
            P.op("act", lambda e: e.activation(out=B4, in_=s4, func=AF.Sigmoid), reads=["s4"], writes=["B4"])
            P.op("act", lambda e: e.activation(out=g4, in_=s4, func=AF.Exp, bias=p4[:, 0:1]), reads=["s4", "p4"], writes=["g4"])
            P.op("act", lambda e: e.activation(out=g4, in_=g4, func=AF.Ln, bias=1.0), reads=["g4"], writes=["g4"])
            P.op("dve", lambda e: e.tensor_scalar(out=g4, in0=g4, scalar1=negA[:, 0:1], scalar2=None, op0=ALU.mult), reads=["g4", "negA"], writes=["g4"])
            P.op("dve", lambda e: e.tensor_tensor_scan(out=G4, data0=rmask, data1=g4, initial=0.0, op0=ALU.mult, op1=ALU.add),
                 reads=["g4", "small"], writes=["G4"])
            P.op("act", lambda e: e.activation(out=EG4, in_=G4, func=AF.Exp), reads=["G4"], writes=["EG4"])
            ED4 = s4
            G4c = G4.rearrange("p (c t) -> p c t", t=64)
            P.op("dve", lambda e: e.tensor_tensor(out=ED4.rearrange("p (c t) -> p c t", t=64), in0=G4c[:, :, 63:64].to_broadcast([4, NCH, 64]),
                                                  in1=G4c, op=ALU.subtract), reads=["G4"], writes=["s4"])
            P.op("act", lambda e: e.activation(out=ED4, in_=ED4, func=AF.Exp), reads=["s4"], writes=["s4"])
            quants = [(B4, "B4"), (G4, "G4"), (EG4, "EG4"), (ED4, "s4")]
            for c in range(NCH):
                for qi, (qt, qk_) in enumerate(quants):
                    P.op("pe", lambda e, c=c, qi=qi, qt=qt: e.matmul(pR[0:64, 256 + c * 16 + qi * 4: 256 + c * 16 + qi * 4 + 4],
                                                                     qt[:, c * 64:(c + 1) * 64], I4, start=True, stop=True),
                         reads=[qk_, "small"], writes=["pR"])
            P.op("dve", lambda e: e.tensor_copy(out=colt[:].rearrange("p c q -> p (c q)"), in_=pR[0:64, 256:256 + NCH * 16]), reads=["pR"], writes=["colt"])
            for h in range(2):
                P.op("dve", lambda e, h=h: e.tensor_tensor(out=cole[:, :, h:h + 1], in0=colt[:, :, h:h + 1], in1=colt[:, :, 10 + h:11 + h], op=ALU.mult),
                     reads=["colt"], writes=[("cole", h)])

            for h in HORDER:
                for k in range(3):
                    conv("dve", qkv[k][:], cb[k * 2 + h], dcw, k * 2 + h, None, f"cb{k * 2 + h}", "dcw", f"qkv{k}")
                    P.op("pool", lambda e, k=k, h=h: e.tensor_copy(out=cb[k * 2 + h][:, 0:3], in_=cb[k * 2 + h][:, TB:TB + 3]),
                         reads=[f"qkv{k}"], writes=[f"cb{k * 2 + h}"])
                    P.op("act", lambda e, k=k: e.activation(out=qkv[k][:], in_=qkv[k][:], func=AF.Silu), reads=[f"qkv{k}"], writes=[f"qkv{k}"])
                for k in range(2):
                    P.op("act", lambda e, k=k: e.activation(out=sq[:, 0, :], in_=qkv[k][:], func=AF.Square), reads=[f"qkv{k}"], writes=["sq"])
                    P.op("pe", lambda e: e.matmul(psn[:], ones[:], sq[:, 0, :], start=True, stop=True), reads=["ones", "sq"], writes=["psn"])
                    P.op("act", lambda e, k=k: e.activation(out=t1_full(k), in_=psn[:], func=AF.Ln, bias=EPS, scale=1.0), reads=["psn"], writes=[L2K[k]])
                    P.op("act", lambda e, k=k: e.activation(out=t1_full(k), in_=t1_full(k), func=AF.Exp, scale=-0.5), reads=[L2K[k]], writes=[L2K[k]])
                    sc = (128.0 ** -0.5) if k == 0 else 1.0
                    P.op("dve", lambda e, k=k, sc=sc: e.scalar_tensor_tensor(out=qkvb[k][:], in0=qkv[k][:], scalar=sc, in1=t1_full(k), op0=ALU.mult, op1=ALU.mult),
                         reads=[f"qkv{k}", L2K[k]], writes=[f"qkvb{k}"])
                    if k == 0:
                        P.op("dve", lambda e: e.tensor_tensor(out=qkv[0][:], in0=qkv[0][:], in1=t1_full(0), op=ALU.mult),
                             reads=["qkv0", "l2r0"], writes=["qkv0"])
                P.op("act", lambda e: e.activation(out=qkvb[2][:], in_=qkv[2][:], func=AF.Copy), reads=["qkv2"], writes=["qkvb2"])
                qnb, knb, vcb = qkvb
                P.op("pe", lambda e, h=h: e.matmul(pA[:], sel(2 + h), G4, start=True, stop=True), reads=["G4", "small"], writes=["pA"])
                P.op("act", lambda e: e.activation(out=Gb[:], in_=pA[:], func=AF.Copy), reads=["pA"], writes=["Gb"])
                P.op("act", lambda e: e.activation(out=EGb[:], in_=pA[:], func=AF.Exp), reads=["pA"], writes=["EGb"])
                P.op("pe", lambda e, h=h: e.matmul(pB[:], sel(h), B4, start=True, stop=True), reads=["B4", "small"], writes=["pB"])
                P.op("act", lambda e: e.activation(out=betab[:], in_=pB[0:64, :], func=AF.Copy), reads=["pB"], writes=["betab"])
                P.op("dve", lambda e: e.scalar_tensor_tensor(out=qdTb[:], in0=qkv[0][:], scalar=128.0 ** -0.5, in1=EGb[:], op0=ALU.mult, op1=ALU.mult),
                     reads=["qkv0", "EGb"], writes=["qdTb"])
                if blk == 0 and h == HORDER[0]:
                    dump(Gb[:], "Gb"); dump(EGb[:], "EGb"); dump(qkv[0][:], "qkv0"); dump(betab[:], "betab", 64)
                for c in range(NCH):
                    cs = slice(c * 64, (c + 1) * 64)
                    P.op("pe", lambda e, cs=cs: e.matmul(pA[0:64, cs], knb[:, cs], knb[:, cs], start=True, stop=True), reads=["qkvb1"], writes=["pA"])
                for c in range(NCH):
                    cs = slice(c * 64, (c + 1) * 64)
                    P.op("pe", lambda e, cs=cs: e.matmul(pB[0:64, cs], knb[:, cs], qnb[:, cs], start=True, stop=True), reads=["qkvb1", "qkvb0"], writes=["pB"])
                for c in range(NCH):
                    cs = slice(c * 64, (c + 1) * 64)
                    P.op("pe", lambda e, c=c, cs=cs: e.transpose(pTk[0:64, c * 128:(c + 1) * 128], knb[:, cs], identb[:]), reads=["qkvb1", "identb"], writes=["pC"])
                GcolB = colt[:, :, 6 + h:7 + h].to_broadcast([64, NCH, 64])
                bcolB = colt[:, :, h:h + 1].to_broadcast([64, NCH, 64])
                v3 = lambda t: t.rearrange("p (c t) -> p c t", t=64)
                P.op("dve", lambda e, GcolB=GcolB: e.tensor_tensor(out=v3(t1[:]), in0=v3(Gb[0:64, :]), in1=GcolB, op=ALU.subtract), reads=["Gb", "colt"], writes=["t1"])
                P.op("dve", lambda e: e.tensor_scalar(out=eT[:], in0=t1[:], scalar1=0.0, scalar2=None, op0=ALU.min), reads=["t1"], writes=["eT"])
                P.op("act", lambda e: e.activation(out=eT[:], in_=eT[:], func=AF.Exp), reads=["eT"], writes=["eT"])
                P.op("dve", lambda e: e.tensor_scalar(out=eL[:], in0=t1[:], scalar1=0.0, scalar2=None, op0=ALU.max), reads=["t1"], writes=["eL"])
                P.op("act", lambda e: e.activation(out=eL[:], in_=eL[:], func=AF.Exp, scale=-1.0), reads=["eL"], writes=["eL"])
                if blk == 0 and h == HORDER[0]:
                    dump(t1[:], "t1", 64); dump(eL, "eL", 64)
                P.op("dve", lambda e: e.tensor_tensor(out=eL[:], in0=eL[:], in1=Ms, op=ALU.mult), reads=["eL", "cm"], writes=["eL"])
                P.op("dve", lambda e, bcolB=bcolB: e.tensor_tensor(out=v3(eL[:]), in0=v3(eL[:]), in1=bcolB, op=ALU.mult), reads=["eL", "colt"], writes=["eL"])
                P.op("dve", lambda e: e.scalar_tensor_tensor(out=Nm[0][:], in0=pA[0:64, :], scalar=-1.0, in1=eL[:], op0=ALU.mult, op1=ALU.mult),
                     reads=["pA", "eL"], writes=["Nm0"])
                if blk == 0 and h == HORDER[0]:
                    dump(eL, "eL", 64)
                P.op("dve", lambda e: e.tensor_tensor(out=t1[:], in0=eT[:], in1=MiT, op=ALU.mult), reads=["eT", "cm"], writes=["t1"])
                P.op("dve", lambda e: e.tensor_tensor(out=qkT[:], in0=pB[0:64, :], in1=t1[:], op=ALU.mult), reads=["pB", "t1"], writes=["qkT"])
                P.op("dve", lambda e: e.tensor_tensor(out=eT[:], in0=eT[:], in1=MsT, op=ALU.mult), reads=["eT", "cm"], writes=["eT"])
                P.op("dve", lambda e: e.tensor_tensor(out=eT[:], in0=eT[:], in1=betab[:], op=ALU.mult), reads=["eT", "betab"], writes=["eT"])
                P.op("dve", lambda e: e.scalar_tensor_tensor(out=NmT[0][:], in0=pA[0:64, :], scalar=-1.0, in1=eT[:], op0=ALU.mult, op1=ALU.mult),
                     reads=["pA", "eT"], writes=["NmT0"])
                pk3 = pTk[0:64, :].rearrange("p (c d) -> p c d", d=128)
                P.op("dve", lambda e, h=h: e.tensor_tensor(out=kbg[:], in0=pk3, in1=cole[:, :, h:h + 1].to_broadcast([64, NCH, 128]), op=ALU.mult),
                     reads=["pC", ("cole", h)], writes=["kbg"])
                P.op("dve", lambda e, h=h: e.tensor_tensor(out=kdec[:], in0=pk3, in1=colt[:, :, 14 + h:15 + h].to_broadcast([64, NCH, 128]), op=ALU.mult),
                     reads=["pC", "colt"], writes=["kdec"])
                for c in range(NCH):
                    cs = slice(c * 64, (c + 1) * 64)
                    P.op("pe", lambda e, c=c, cs=cs: e.transpose(pTk[0:64, c * 128:(c + 1) * 128], vcb[:, cs], identb[:]), reads=["qkvb2", "identb"], writes=["pC"])
                P.op("dve", lambda e, h=h: e.tensor_tensor(out=vb[:], in0=pk3, in1=colt[:, :, h:h + 1].to_broadcast([64, NCH, 128]), op=ALU.mult),
                     reads=["pC", "colt"], writes=["vb"])
                if blk == 0 and h == HORDER[0]:
                    dump(Nm[0][:], "Nm0", 64); dump(NmT[0][:], "NmT0", 64); dump(vb[:, 0:4, :].rearrange("p c d -> p (c d)"), "vb", 64); dump(kbg[:, 0:4, :].rearrange("p c d -> p (c d)"), "kbg", 64)
                P.op("dve", lambda e: e.tensor_tensor(out=Pm[:], in0=NmT[0][:], in1=Id8, op=ALU.add), reads=["NmT0", "cm"], writes=["Pm"])
                cur = 0
                for lvl in range(1, 6):
                    nxt = 1 - cur
                    for c in range(NCH):
                        cs = slice(c * 64, (c + 1) * 64)
                        P.op("pe", lambda e, cs=cs, cur=cur: e.matmul(pA[0:64, cs], NmT[cur][:, cs], Nm[cur][:, cs], start=True, stop=True),
                             reads=[f"NmT{cur}", f"Nm{cur}"], writes=["pA"])
                    P.op("act", lambda e, nxt=nxt: e.activation(out=Nm[nxt][:], in_=pA[0:64, :], func=AF.Copy), reads=["pA"], writes=[f"Nm{nxt}"])
                    if lvl < 5:
                        for c in range(NCH):
                            cs = slice(c * 64, (c + 1) * 64)
                            P.op("pe", lambda e, cs=cs, cur=cur: e.matmul(pB[0:64, cs], Nm[cur][:, cs], NmT[cur][:, cs], start=True, stop=True),
                                 reads=[f"NmT{cur}", f"Nm{cur}"], writes=["pB"])
                        P.op("act", lambda e, nxt=nxt: e.activation(out=NmT[nxt][:], in_=pB[0:64, :], func=AF.Copy), reads=["pB"], writes=[f"NmT{nxt}"])
                    for c in range(NCH):
                        cs = slice(c * 64, (c + 1) * 64)
                        P.op("pe", lambda e, cs=cs, nxt=nxt: e.matmul(pC[0:64, cs], Nm[nxt][:, cs], Pm[:, cs], start=True, stop=True),
                             reads=[f"Nm{nxt}", "Pm"], writes=["pC"])
                    P.op("dve", lambda e: e.tensor_tensor(out=Pm[:], in0=Pm[:], in1=pC[0:64, :], op=ALU.add), reads=["pC", "Pm"], writes=["Pm"])
                    cur = nxt
                for half in range(2):
                    for c4 in range(4):
                        c = half * 4 + c4
                        cs = slice(c * 64, (c + 1) * 64)
                        pu = pA if half == 0 else pB
                        P.op("pe", lambda e, c=c, c4=c4, cs=cs, pu=pu: e.matmul(pu[0:64, c4 * 128:(c4 + 1) * 128], Pm[:, cs], vb[:, c, :], start=True, stop=True),
                             reads=["Pm", "vb"], writes=[("pA" if half == 0 else "pB")])
                    pu = pA if half == 0 else pB
                    P.op("act", lambda e, half=half, pu=pu: e.activation(out=u_t[:, half * 4:(half + 1) * 4, :].rearrange("p c d -> p (c d)"), in_=pu[0:64, :], func=AF.Copy),
                         reads=[("pA" if half == 0 else "pB")], writes=[("u", half)])
                for c in range(NCH):
                    cs = slice(c * 64, (c + 1) * 64)
                    P.op("pe", lambda e, c=c, cs=cs: e.matmul(pC[:, cs], kbg[:, c, :], Pm[:, cs], start=True, stop=True), reads=["Pm", "kbg"], writes=["pC"])
                P.op("act", lambda e: e.activation(out=wTb[:], in_=pC[:], func=AF.Copy), reads=["pC"], writes=["wTb"])
                if blk == 0 and h == HORDER[0]:
                    dump(Pm[:], "Pm", 64); dump(u_t[:, 0:4, :].rearrange("p c d -> p (c d)"), ("u", 0), 64)
                Sk, Sbk = f"S32_{h}", f"Sb_{h}"
                for c in range(NCH):
                    cs = slice(c * 64, (c + 1) * 64)
                    P.op("pe", lambda e, cs=cs, h=h: e.matmul(pR[0:64, 0:128], wTb[:, cs], Sb[h][:], start=True, stop=True), reads=["wTb", Sbk], writes=["pR"])
                    P.op("dve", lambda e, c=c: e.tensor_tensor(out=vnew[:], in0=u_t[:, c, :], in1=pR[0:64, 0:128], op=ALU.subtract),
                         reads=["pR", ("u", c // 4)], writes=["vnew"])
                    P.op("pe", lambda e, cs=cs, h=h: e.matmul(pO[:, cs], Sb[h][:], qdTb[:, cs], start=True, stop=False), reads=[Sbk, "qdTb"], writes=["pO"])
                    P.op("pe", lambda e, cs=cs: e.matmul(pO[:, cs], vnew[:], qkT[:, cs], start=False, stop=True), reads=["vnew", "qkT"], writes=["pO"])
                    P.op("pe", lambda e, c=c: e.matmul(pR[:, 128:256], kdec[:, c, :], vnew[:], start=True, stop=True), reads=["vnew", "kdec"], writes=["pR"])
                    P.op("dve", lambda e, h=h, c=c: e.scalar_tensor_tensor(out=S32[h][:], in0=S32[h][:], scalar=EGb[:, c * 64 + 63:c * 64 + 64], in1=pR[:, 128:256],
                                                                          op0=ALU.mult, op1=ALU.add), reads=["pR", Sk, "EGb"], writes=[Sk])
                    P.op("act", lambda e, h=h: e.activation(out=Sb[h][:], in_=S32[h][:], func=AF.Copy), reads=[Sk], writes=[Sbk])
                P.op("act", lambda e: e.activation(out=o32[:], in_=pO[:], func=AF.Copy), reads=["pO"], writes=["o32"])
                P.op("act", lambda e: e.activation(out=sq[:, 0, :], in_=pO[:], func=AF.Square), reads=["pO"], writes=["sq"])
                P.op("pe", lambda e: e.matmul(psn[:], ones[:], sq[:, 0, :], start=True, stop=True), reads=["ones", "sq"], writes=["psn"])
                P.op("act", lambda e: e.activation(out=t1_full(0), in_=psn[:], func=AF.Ln, bias=EPS, scale=1.0 / 128), reads=["psn"], writes=["l2r0"])
                P.op("act", lambda e: e.activation(out=t1_full(0), in_=t1_full(0), func=AF.Exp, scale=-0.5), reads=["l2r0"], writes=["l2r0"])
                if blk == 0 and h == HORDER[0]:
                    dump(o32[:], "o32")
                yk = ycnt[0] % 2
                ycnt[0] += 1
                P.op("dve", lambda e, yk=yk: e.scalar_tensor_tensor(out=yo[yk][:], in0=o32[:], scalar=dng[:, 0:1], in1=t1_full(0), op0=ALU.mult, op1=ALU.mult),
                     reads=["o32", "dng", "l2r0"], writes=[f"yo{yk}"])
                P.op("dve", lambda e, yk=yk, h=h: e.tensor_tensor(out=yo[yk][:], in0=yo[yk][:], in1=sgate[h][:], op=ALU.mult),
                     reads=[f"yo{yk}", f"sgate{h}"], writes=[f"yo{yk}"])
                store_y(2 + h, t0, f"yo{yk}", yo[yk][:])
        P.wait_all_dma("sp")
        P.emit()
    return nc


EPS = 1e-6
D = 2048
KC = 16


def build_B(NT, FF, mode="dense", final_norm=False, PASS=1024):
    nc = bass.Bass("TRN2", target_bir_lowering=False)
    TT = 512
    PASS = min(PASS, NT)
    npass = NT // PASS
    tpp = PASS // TT
    NF = FF // 128
    ymT = nc.dram_tensor("ymT", [KC, 128, NT], F32, kind="ExternalInput").ap()
    xT = nc.dram_tensor("xT", [KC, 128, NT], F32, kind="ExternalInput").ap()
    wout = nc.dram_tensor("wout", [D, D], F32, kind="ExternalInput").ap()
    lgain = nc.dram_tensor("lgain", [128, 8], F32, kind="ExternalInput").ap()
    ngain = nc.dram_tensor("ngain", [128, KC], F32, kind="ExternalInput").ap()
    wg = nc.dram_tensor("wg", [D, FF], F32, kind="ExternalInput").ap()
    wu = nc.dram_tensor("wu", [D, FF], F32, kind="ExternalInput").ap()
    wd = nc.dram_tensor("wd", [FF, D], F32, kind="ExternalInput").ap()
    x2T = nc.dram_tensor("x2T", [KC, 128, NT], F32, kind="ExternalOutput").ap()
    x1s = nc.dram_tensor("x1s", [KC, 128, NT], F32, kind="Internal").ap()

    ymv = ymT.rearrange("c p t -> p c t")
    xv = xT.rearrange("c p t -> p c t")
    x1v = x1s.rearrange("c p t -> p c t")
    woutv = wout.rearrange("(kc p) n -> p kc n", p=128)
    wgv = wg.rearrange("(kc p) n -> p kc n", p=128)
    wuv = wu.rearrange("(kc p) n -> p kc n", p=128)
    wdv = wd.rearrange("(f p) n -> p f n", p=128)

    with ExitStack() as st:
        P = Prog(nc, st)
        ones = P.sb("ones", [128, 128], BF16)
        lg = P.sb("lg", [128, 8], F32)
        ng = P.sb("ng", [128, KC], F32)
        AR = max(24576, NF * PASS // 2)
        arena = P.sb("arena", [128, AR], F32)
        ym32 = arena[:, 0:8192].rearrange("p (c t) -> p c t", c=KC)
        x32 = arena[:, 8192:16384].rearrange("p (c t) -> p c t", c=KC)
        ymb = arena[:, 16384:20480].bitcast(BF16).rearrange("p (c t) -> p c t", c=KC)
        sq = arena[:, 20480:24576].bitcast(BF16).rearrange("p (c t) -> p c t", c=KC)
        actT = arena[:, 0:NF * PASS // 2].bitcast(BF16).rearrange("p (f t) -> p f t", f=NF)
        h2T = P.sb("h2T", [128, KC, PASS], BF16)
        rs = P.sb("rs", [128, TT], F32)
        wo = [P.sb(f"wo{i}", [128, KC, 128], BF16) for i in range(2)]
        wbuf = [P.sb(f"wbuf{i}", [128, 8192], BF16) for i in range(2)]
        wgt = [w[:, 0:4096].rearrange("p (c n) -> p c n", c=KC) for w in wbuf]
        wut = [w[:, 4096:8192].rearrange("p (c n) -> p c n", c=KC) for w in wbuf]
        wdt = [w[:, 0:NF * 128].rearrange("p (f n) -> p f n", f=NF) for w in wbuf]
        dummy = P.sb("dmy", [128, 8], F32)
        sg = [P.sb(f"sg{i}", [128, TT], F32) for i in range(2)]
        xr = [P.sb(f"xr{i}", [128, TT], F32) for i in range(2)]
        xo = [P.sb(f"xo{i}", [128, TT], F32) for i in range(2)]
        ps = [P.ps(f"ps{i}", [128, 512]) for i in range(8)]

        P.op("dve", lambda e: e.memset(ones[:], 1.0), writes=["ones"])
        P.dma("sp", lg[:], lgain, writes=["lg"])
        P.dma("sp", ng[:], ngain, writes=["ng"])

        cnt = {"wo": 0, "w": 0, "wd": 0, "po": 0, "g": 0, "x": 0}
        for ps_i in range(npass):
            for tq in range(tpp):
                t0 = ps_i * PASS + tq * TT
                P.dma("sp", ym32, ymv[:, :, t0:t0 + TT], writes=["ym32"])
                P.dma("sp", x32, xv[:, :, t0:t0 + TT], writes=["x32"] + [("x1", j) for j in range(KC)])
                P.op("act", lambda e: e.activation(out=sq[:, 0:8, :], in_=ym32[:, 0:8, :], func=AF.Square),
                     reads=["ym32"], writes=["sq"])
                for j in range(8):
                    P.op("pe", lambda e, j=j: e.matmul(ps[0][:], ones[:], sq[:, j, :], start=(j == 0), stop=(j == 7)),
                         reads=["ones", "sq"], writes=["ps0"])
                P.op("act", lambda e: e.activation(out=rs[:], in_=ps[0][:], func=AF.Sqrt, bias=EPS, scale=1.0 / 1024),
                     reads=["ps0"], writes=["rs"])
                P.op("dve", lambda e: e.reciprocal(out=rs[:], in_=rs[:]), reads=["rs"], writes=["rs"])
                for j in range(8):
                    P.op("dve", lambda e, j=j: e.scalar_tensor_tensor(out=ymb[:, j, :], in0=ym32[:, j, :], scalar=lg[:, j:j + 1],
                                                                        in1=rs[:], op0=ALU.mult, op1=ALU.mult),
                         reads=["ym32", "lg", "rs"], writes=[("ymb", j)])
                P.op("pool", lambda e: e.tensor_copy(out=ymb[:, 8:16, :], in_=ym32[:, 8:16, :]),
                     reads=["ym32"], writes=[("ymb", j) for j in range(8, 16)])
                for dt in range(KC):
                    b = cnt["wo"] % 2
                    cnt["wo"] += 1
                    P.dma("pool", wo[b][:], woutv[:, :, dt * 128:dt * 128 + 128], writes=[f"wo{b}"])
                    pb = 1 + cnt["po"] % 2
                    cnt["po"] += 1
                    for kc in range(KC):
                        P.op("pe", lambda e, kc=kc, b=b, pb=pb: e.matmul(
                            ps[pb][:], wo[b][:, kc, :], ymb[:, kc, :], start=(kc == 0), stop=(kc == KC - 1)),
                            reads=[f"wo{b}", ("ymb", kc)], writes=[f"ps{pb}"])
                    P.op("dve", lambda e, dt=dt, pb=pb: e.tensor_tensor(out=x32[:, dt, :], in0=x32[:, dt, :], in1=ps[pb][:], op=ALU.add),
                         reads=[f"ps{pb}", "x32"], writes=[("x1", dt)])
                x1keys = [("x1", dt) for dt in range(KC)]
                P.dma("sp", x1v[:, :, t0:t0 + TT], x32, reads=x1keys, writes=["x1s"], key="x1st")
                P.op("act", lambda e: e.activation(out=sq[:], in_=x32[:], func=AF.Square),
                     reads=x1keys, writes=["sq"])
                for j in range(KC):
                    P.op("pe", lambda e, j=j: e.matmul(ps[0][:], ones[:], sq[:, j, :], start=(j == 0), stop=(j == KC - 1)),
                         reads=["ones", "sq"], writes=["ps0"])
                P.op("act", lambda e: e.activation(out=rs[:], in_=ps[0][:], func=AF.Sqrt, bias=EPS, scale=1.0 / D),
                     reads=["ps0"], writes=["rs"])
                P.op("dve", lambda e: e.reciprocal(out=rs[:], in_=rs[:]), reads=["rs"], writes=["rs"])
                for j in range(KC):
                    P.op("dve", lambda e, j=j, tq=tq: e.scalar_tensor_tensor(
                        out=h2T[:, j, tq * TT:(tq + 1) * TT], in0=x32[:, j, :], scalar=ng[:, j:j + 1],
                        in1=rs[:], op0=ALU.mult, op1=ALU.mult),
                        reads=[("x1", j), "ng", "rs"], writes=[("h2T", tq)])
            b1keys = ["ym32", "x32", "sq"] + [("ymb", j) for j in range(KC)] + [("x1", j) for j in range(KC)]
            P.op("pool", lambda e: e.memset(dummy[:], 1.0), writes=b1keys + ["actT"])
            for fc in range(FF // 256):
                b = cnt["w"] % 2
                cnt["w"] += 1
                P.dma("pool", wgt[b], wgv[:, :, fc * 256:(fc + 1) * 256], writes=[f"wbuf{b}"], key=f"wg{b}")
                P.dma("pool", wut[b], wuv[:, :, fc * 256:(fc + 1) * 256], writes=[f"wbufu{b}"], key=f"wu{b}")
                for half in range(2):
                    f = fc * 2 + half
                    for tq in range(tpp):
                        g = cnt["g"] % 2
                        cnt["g"] += 1
                        pg, pu = 3 + g, 5 + g
                        for kc in range(KC):
                            P.op("pe", lambda e, kc=kc, b=b, pg=pg, half=half, tq=tq: e.matmul(
                                ps[pg][:], wgt[b][:, kc, half * 128:(half + 1) * 128], h2T[:, kc, tq * TT:(tq + 1) * TT],
                                start=(kc == 0), stop=(kc == KC - 1)),
                                reads=[f"wbuf{b}", ("h2T", tq)], writes=[f"ps{pg}"])
                        for kc in range(KC):
                            P.op("pe", lambda e, kc=kc, b=b, pu=pu, half=half, tq=tq: e.matmul(
                                ps[pu][:], wut[b][:, kc, half * 128:(half + 1) * 128], h2T[:, kc, tq * TT:(tq + 1) * TT],
                                start=(kc == 0), stop=(kc == KC - 1)),
                                reads=[f"wbufu{b}", ("h2T", tq)], writes=[f"ps{pu}"])
                        P.op("act", lambda e, g=g, pg=pg: e.activation(out=sg[g][:], in_=ps[pg][:], func=AF.Silu),
                             reads=[f"ps{pg}"], writes=[f"sg{g}"])
                        P.op("dve", lambda e, g=g, pu=pu, f=f, tq=tq: e.tensor_tensor(
                            out=actT[:, f, tq * TT:(tq + 1) * TT], in0=sg[g][:], in1=ps[pu][:], op=ALU.mult),
                            reads=[f"sg{g}", f"ps{pu}", "actT"], writes=[("act", f, tq)])
            for dt in range(KC):
                b = cnt["wd"] % 2
                cnt["wd"] += 1
                P.dma("pool", wdt[b], wdv[:, :, dt * 128:(dt + 1) * 128], writes=[f"wbuf{b}", f"wbufu{b}"], key=f"wg{b}")
                for tq in range(tpp):
                    t0 = ps_i * PASS + tq * TT
                    pb = 1 + cnt["po"] % 2
                    cnt["po"] += 1
                    xb = cnt["x"] % 2
                    cnt["x"] += 1
                    P.dma("sp", xr[xb][:], x1v[:, dt, t0:t0 + TT], reads=["x1s"], writes=[f"xr{xb}"])
                    for f in range(NF):
                        P.op("pe", lambda e, f=f, b=b, pb=pb, tq=tq: e.matmul(
                            ps[pb][:], wdt[b][:, f, :], actT[:, f, tq * TT:(tq + 1) * TT],
                            start=(f == 0), stop=(f == NF - 1)),
                            reads=[f"wbuf{b}", ("act", f, tq)], writes=[f"ps{pb}"])
                    P.op("dve", lambda e, xb=xb, pb=pb: e.tensor_tensor(out=xo[xb][:], in0=xr[xb][:], in1=ps[pb][:], op=ALU.add),
                         reads=[f"xr{xb}", f"ps{pb}"], writes=[f"xo{xb}"])
                    P.dma("sp", x2T[dt, :, t0:t0 + TT], xo[xb][:], reads=[f"xo{xb}"], key=f"xo{xb}")
            allact = [("act", f, tq) for f in range(NF) for tq in range(tpp)]
            P.op("pool", lambda e: e.memset(dummy[:], 1.0), writes=allact + b1keys + ["actT"])
        P.wait_all_dma("sp")
        P.emit()
    return nc


EPS = 1e-6
D = 2048
KC = 16
NE = 8


def consts_M(C):
    t = np.arange(128)
    U = (t[:, None] < t[None, :]).astype(np.float32)
    ebase = np.tile((np.arange(NE) * C).astype(np.float32)[None, :], (128, 1))
    return {"Umat": U, "identm": np.eye(128, dtype=np.float32), "ebase": ebase}


def build_M(NT, FE, C):
    nc = bass.Bass("TRN2", target_bir_lowering=False)
    TT = 512
    ntile = NT // TT
    NS = NT // 128
    NF = FE // 128
    NSB = C // 128
    CH = C // 2
    dr = lambda n, s, k="ExternalInput", dt=F32: nc.dram_tensor(n, list(s), dt, kind=k).ap()
    ymT = dr("ymT", [KC, 128, NT])
    xT = dr("xT", [KC, 128, NT])
    wout = dr("wout", [D, D])
    lgain = dr("lgain", [128, 8])
    ngain = dr("ngain", [128, KC])
    ngrow = dr("ngrow", [1, D])
    fgrow = dr("fgrow", [1, D])
    router = dr("router", [D, NE])
    wg = dr("wg", [NE, D, FE])
    wu = dr("wu", [NE, D, FE])
    wd = dr("wd", [NE, FE, D])
    Umat = dr("Umat", [128, 128])
    identm = dr("identm", [128, 128])
    ebased = dr("ebase", [128, NE])
    out = dr("out", [NT, D], "ExternalOutput")
    cnt_out = dr("cnt_out", [128, NE], "ExternalOutput")
    x1tok_s = dr("x1tok_s", [NT, D], "Internal")
    Xe = dr("Xe", [NE * C, D], "Internal", BF16)
    Yd = dr("Yd", [NE * C, D], "Internal")

    ymv = ymT.rearrange("c p t -> p c t")
    xv = xT.rearrange("c p t -> p c t")
    woutv = wout.rearrange("(kc p) n -> p kc n", p=128)

    with ExitStack() as st:
        P = Prog(nc, st)
        sb, ps_ = P.sb, P.ps
        ones = sb("ones", [128, 128], BF16)
        ones32 = sb("ones32", [128, 128], F32)
        U = sb("U", [128, 128], F32)
        ident = sb("ident_sb", [128, 128], F32)
        identb = sb("identb", [128, 128], BF16)
        ebase = sb("ebase_sb", [128, NE], F32)
        lg = sb("lg", [128, 8], F32)
        ng = sb("ng", [128, KC], F32)
        ngb = sb("ngb", [128, D], F32)
        fgb = sb("fgb", [128, D], F32)
        rt = sb("rt", [128, KC, NE], F32)
        gr = sb("gr", [128, KC, NE], F32)
        arena = sb("arena", [128, 24576], F32)
        ym32 = arena[:, 0:8192].rearrange("p (c t) -> p c t", c=KC)
        x32 = arena[:, 8192:16384].rearrange("p (c t) -> p c t", c=KC)
        ymb = arena[:, 16384:20480].bitcast(BF16).rearrange("p (c t) -> p c t", c=KC)
        sq = arena[:, 20480:24576].bitcast(BF16).rearrange("p (c t) -> p c t", c=KC)
        o1 = 8 * C
        XeT = arena[:, 0:o1].bitcast(BF16).rearrange("p (c t) -> p c t", c=KC)
        xrow = [arena[:, o1 + i * 1024: o1 + (i + 1) * 1024].bitcast(BF16) for i in range(2)]
        o2 = o1 + 2048
        o3 = o2 + NF * C // 2
        actT = arena[:, o2:o3].bitcast(BF16).rearrange("p (f t) -> p f t", f=NF)
        ystg = [arena[:, o3 + i * 512: o3 + (i + 1) * 512] for i in range(3)]
        assert o3 + 1536 <= 24576
        cy1 = [arena[:, i * 8192: i * 8192 + 2048] for i in range(2)]
        cy2 = [arena[:, i * 8192 + 2048: i * 8192 + 4096] for i in range(2)]
        cx1 = [arena[:, i * 8192 + 4096: i * 8192 + 6144] for i in range(2)]
        cjunk = arena[:, 16384:18432]
        rs = sb("rs", [128, TT], F32)
        wo = [sb(f"wo{i}", [128, KC, 128], BF16) for i in range(2)]
        wbuf = [sb(f"wbuf{i}", [128, 8192], BF16) for i in range(2)]
        wgt = [w[:, 0:4096].rearrange("p (c n) -> p c n", c=KC) for w in wbuf]
        wut = [w[:, 4096:8192].rearrange("p (c n) -> p c n", c=KC) for w in wbuf]
        wdt = [w[:, 0:NF * 256].rearrange("p (f n) -> p f n", f=NF) for w in wbuf]
        dmy = sb("dmy", [128, 8], F32)
        x1tok = sb("x1tok", [128, D], F32)
        h2tok = sb("h2tok", [128, D], BF16)
        sg = [sb(f"sg{i}", [128, CH], F32) for i in range(2)]
        cnt = sb("cnt", [128, NE], F32)
        lgt = sb("lgt", [128, NE], F32)
        lg2 = sb("lg2", [128, NE], F32)
        mk1 = sb("mk1", [128, NE], F32)
        mk2 = sb("mk2", [128, NE], F32)
        mk = sb("mk", [128, NE], F32)
        slot = sb("slot", [128, NE], F32)
        tmp8 = sb("tmp8", [128, NE], F32)
        m12 = sb("m12", [128, 4], F32)
        ssum = sb("ssum", [128, 2], F32)
        rstd = sb("rstd", [128, 1], F32)
        dstf = sb("dstf", [128, 2], F32)
        dst = sb("dst", [128, NS, 2], I32)
        wts = sb("wts", [128, NS, 2], F32)
        ps = [ps_(f"ps{i}", [128, 512]) for i in range(8)]

        P.op("dve", lambda e: e.memset(ones[:], 1.0), writes=["ones"])
        P.op("dve", lambda e: e.memset(ones32[:], 1.0), writes=["ones32"])
        P.op("dve", lambda e: e.memset(cnt[:], 0.0), writes=["cnt"])
        P.dma("sp", lg[:], lgain, writes=["lg"])
        P.dma("sp", ng[:], ngain, writes=["ng"])
        P.dma("sp", U[:], Umat, writes=["U"])
        P.dma("sp", ident[:], identm, writes=["ident"])
        P.dma("sp", ebase[:], ebased, writes=["ebase"])
        P.dma("sp", ngb[:], ngrow.partition_broadcast(128), writes=["ngb"])
        P.dma("sp", fgb[:], fgrow.partition_broadcast(128), writes=["fgb"])
        P.dma("sp", rt[:], router.rearrange("(kc p) e -> p kc e", p=128), writes=["rt"])
        P.op("dve", lambda e: e.tensor_copy(out=identb[:], in_=ident[:]), reads=["ident"], writes=["identb"])
        for kc in range(KC):
            P.op("dve", lambda e, kc=kc: e.tensor_scalar(out=gr[:, kc, :], in0=rt[:, kc, :], scalar1=ng[:, kc:kc + 1], scalar2=None, op0=ALU.mult),
                 reads=["rt", "ng"], writes=["gr"])

        cntr = {"wo": 0, "po": 0}
        b1keys = ["ym32", "x32", "sq"] + [("ymb", j) for j in range(KC)] + [("x1", j) for j in range(KC)]
        for tq in range(ntile):
            t0 = tq * TT
            P.dma("sp", ym32, ymv[:, :, t0:t0 + TT], writes=["ym32"])
            P.dma("sp", x32, xv[:, :, t0:t0 + TT], writes=["x32"] + [("x1", j) for j in range(KC)])
            P.op("act", lambda e: e.activation(out=sq[:, 0:8, :], in_=ym32[:, 0:8, :], func=AF.Square), reads=["ym32"], writes=["sq"])
            for j in range(8):
                P.op("pe", lambda e, j=j: e.matmul(ps[0][:], ones[:], sq[:, j, :], start=(j == 0), stop=(j == 7)), reads=["ones", "sq"], writes=["ps0"])
            P.op("act", lambda e: e.activation(out=rs[:], in_=ps[0][:], func=AF.Sqrt, bias=EPS, scale=1.0 / 1024), reads=["ps0"], writes=["rs"])
            P.op("dve", lambda e: e.reciprocal(out=rs[:], in_=rs[:]), reads=["rs"], writes=["rs"])
            for j in range(8):
                P.op("dve", lambda e, j=j: e.scalar_tensor_tensor(out=ymb[:, j, :], in0=ym32[:, j, :], scalar=lg[:, j:j + 1], in1=rs[:], op0=ALU.mult, op1=ALU.mult),
                     reads=["ym32", "lg", "rs"], writes=[("ymb", j)])
            P.op("pool", lambda e: e.tensor_copy(out=ymb[:, 8:16, :], in_=ym32[:, 8:16, :]), reads=["ym32"], writes=[("ymb", j) for j in range(8, 16)])
            for dt in range(KC):
                b = cntr["wo"] % 2
                cntr["wo"] += 1
                P.dma("pool", wo[b][:], woutv[:, :, dt * 128:dt * 128 + 128], writes=[f"wo{b}"])
                pb = 1 + cntr["po"] % 2
                cntr["po"] += 1
                for kc in range(KC):
                    P.op("pe", lambda e, kc=kc, b=b, pb=pb: e.matmul(ps[pb][:], wo[b][:, kc, :], ymb[:, kc, :], start=(kc == 0), stop=(kc == KC - 1)),
                         reads=[f"wo{b}", ("ymb", kc)], writes=[f"ps{pb}"])
                P.op("dve", lambda e, dt=dt, pb=pb: e.tensor_tensor(out=x32[:, dt, :], in0=x32[:, dt, :], in1=ps[pb][:], op=ALU.add),
                     reads=[f"ps{pb}", "x32"], writes=[("x1", dt)])
            x1keys = [("x1", dt) for dt in range(KC)]
            for s4 in range(4):
                sidx = tq * 4 + s4
                ts = slice(s4 * 128, (s4 + 1) * 128)
                for kc in range(KC):
                    P.op("pe", lambda e, kc=kc, ts=ts: e.matmul(ps[3][:, 0:NE], x32[:, kc, ts], gr[:, kc, :], start=(kc == 0), stop=(kc == KC - 1)),
                         reads=[("x1", kc), "gr"], writes=["ps3"])
                for q in range(4):
                    pb = 4 + q % 2
                    for d4 in range(4):
                        dc = q * 4 + d4
                        P.op("pe", lambda e, dc=dc, d4=d4, ts=ts, pb=pb: e.transpose(ps[pb][:, d4 * 128:(d4 + 1) * 128], x32[:, dc, ts], ident[:]),
                             reads=[("x1", dc), "ident"], writes=[f"ps{pb}"])
                    P.op("act", lambda e, q=q, pb=pb: e.activation(out=x1tok[:, q * 512:(q + 1) * 512], in_=ps[pb][:], func=AF.Copy),
                         reads=[f"ps{pb}"], writes=[("x1tok", q)])
                xtk = [("x1tok", q) for q in range(4)]
                P.dma("sp", x1tok_s[tq * TT + s4 * 128: tq * TT + (s4 + 1) * 128, :], x1tok[:], reads=xtk, writes=["x1tok_s"], key="x1tst")
                P.op("act", lambda e: e.activation(out=h2tok[:], in_=x1tok[:], func=AF.Square, accum_out=ssum[:, 0:1]), reads=xtk, writes=["h2tok", "ssum"])
                P.op("act", lambda e: e.activation(out=rstd[:], in_=ssum[:, 0:1], func=AF.Sqrt, bias=EPS, scale=1.0 / D), reads=["ssum"], writes=["rstd"])
                P.op("dve", lambda e: e.reciprocal(out=rstd[:], in_=rstd[:]), reads=["rstd"], writes=["rstd"])
                P.op("dve", lambda e: e.scalar_tensor_tensor(out=h2tok[:], in0=x1tok[:], scalar=rstd[:, 0:1], in1=ngb[:], op0=ALU.mult, op1=ALU.mult),
                     reads=xtk + ["rstd", "ngb"], writes=["h2tok"])
                P.op("dve", lambda e: e.tensor_scalar(out=lgt[:], in0=ps[3][:, 0:NE], scalar1=rstd[:, 0:1], scalar2=None, op0=ALU.mult), reads=["ps3", "rstd"], writes=["lgt"])
                P.op("dve", lambda e: e.tensor_reduce(out=m12[:, 0:1], in_=lgt[:], axis=AX.X, op=ALU.max), reads=["lgt"], writes=["m1"])
                P.op("dve", lambda e: e.tensor_scalar(out=mk1[:], in0=lgt[:], scalar1=m12[:, 0:1], scalar2=None, op0=ALU.is_equal), reads=["lgt", "m1"], writes=["mk1"])
                P.op("dve", lambda e: e.scalar_tensor_tensor(out=lg2[:], in0=mk1[:], scalar=-1e30, in1=lgt[:], op0=ALU.mult, op1=ALU.add), reads=["mk1", "lgt"], writes=["lg2"])
                P.op("dve", lambda e: e.tensor_reduce(out=m12[:, 1:2], in_=lg2[:], axis=AX.X, op=ALU.max), reads=["lg2"], writes=["m2"])
                P.op("dve", lambda e: e.tensor_scalar(out=mk2[:], in0=lg2[:], scalar1=m12[:, 1:2], scalar2=None, op0=ALU.is_equal), reads=["lg2", "m2"], writes=["mk2"])
                P.op("dve", lambda e: e.tensor_tensor(out=m12[:, 2:3], in0=m12[:, 0:1], in1=m12[:, 1:2], op=ALU.subtract), reads=["m1", "m2"], writes=["md"])
                P.op("act", lambda e, sidx=sidx: e.activation(out=wts[:, sidx, 0:1], in_=m12[:, 2:3], func=AF.Sigmoid), reads=["md"], writes=[("wts", sidx)])
                P.op("dve", lambda e, sidx=sidx: e.tensor_scalar(out=wts[:, sidx, 1:2], in0=wts[:, sidx, 0:1], scalar1=-1.0, scalar2=1.0, op0=ALU.mult, op1=ALU.add),
                     reads=[("wts", sidx)], writes=[("wts2", sidx)])
                P.op("dve", lambda e: e.tensor_tensor(out=mk[:], in0=mk1[:], in1=mk2[:], op=ALU.add), reads=["mk1", "mk2"], writes=["mk"])
                P.op("pe", lambda e: e.matmul(ps[6][:, 0:NE], U[:], mk[:], start=True, stop=True), reads=["U", "mk"], writes=["ps6"])
                P.op("pe", lambda e: e.matmul(ps[6][:, NE:2 * NE], ones32[:], mk[:], start=True, stop=True), reads=["ones32", "mk"], writes=["ps6"])
                P.op("dve", lambda e: e.tensor_tensor(out=slot[:], in0=ps[6][:, 0:NE], in1=cnt[:], op=ALU.add), reads=["ps6", "cnt"], writes=["slot"])
                P.op("dve", lambda e: e.tensor_tensor(out=slot[:], in0=slot[:], in1=ebase[:], op=ALU.add), reads=["slot", "ebase"], writes=["slot"])
                P.op("dve", lambda e: e.tensor_tensor(out=cnt[:], in0=ps[6][:, NE:2 * NE], in1=cnt[:], op=ALU.add), reads=["ps6", "cnt", "slot"], writes=["cnt"])
                for k2, mkk in enumerate((mk1, mk2)):
                    P.op("dve", lambda e, mkk=mkk: e.tensor_tensor(out=tmp8[:], in0=mkk[:], in1=slot[:], op=ALU.mult), reads=["slot", "mk1", "mk2"], writes=["tmp8"])
                    P.op("dve", lambda e, k2=k2: e.tensor_reduce(out=dstf[:, k2:k2 + 1], in_=tmp8[:], axis=AX.X, op=ALU.add), reads=["tmp8"], writes=[("dstf", k2)])
                P.op("dve", lambda e, sidx=sidx: e.tensor_copy(out=dst[:, sidx, :], in_=dstf[:]), reads=[("dstf", 0), ("dstf", 1)], writes=[("dst", sidx)])
                for k2 in range(2):
                    P.idma(Xe, dst[:, sidx, k2:k2 + 1], h2tok[:], None, reads=["h2tok", ("dst", sidx)], writes=["Xe"], key=("xsc", k2), bounds=NE * C - 1)
        P.dma("sp", cnt_out, cnt[:], reads=["cnt"], key="cntout")
        P.op("pool", lambda e: e.memset(dmy[:], 1.0), writes=b1keys + ["earena"])
        ec = {"xr": 0, "w": 0, "g": 0, "y": 0}
        for ex in range(NE):
            for sbk in range(NSB):
                xb = ec["xr"] % 2
                ec["xr"] += 1
                P.dma("sp", xrow[xb], Xe[ex * C + sbk * 128: ex * C + (sbk + 1) * 128, :], reads=["Xe", "earena"], writes=[f"xrow{xb}"])
                for half in range(2):
                    pbT = ps[1 + half][:].bitcast(BF16)
                    for d8 in range(8):
                        dc = half * 8 + d8
                        P.op("pe", lambda e, xb=xb, dc=dc, d8=d8, pbT=pbT: e.transpose(pbT[:, d8 * 128:(d8 + 1) * 128], xrow[xb][:, dc * 128:(dc + 1) * 128], identb[:]),
                             reads=[f"xrow{xb}", "identb"], writes=[f"ps{1 + half}"])
                    P.op("act" if half == 0 else "dve",
                         (lambda e, half=half, sbk=sbk, pbT=pbT: e.activation(out=XeT[:, half * 8:(half + 1) * 8, sbk * 128:(sbk + 1) * 128],
                                                                             in_=pbT.rearrange("p (c t) -> p c t", c=8), func=AF.Copy)) if half == 0 else
                         (lambda e, half=half, sbk=sbk, pbT=pbT: e.tensor_copy(out=XeT[:, half * 8:(half + 1) * 8, sbk * 128:(sbk + 1) * 128],
                                                                              in_=pbT.rearrange("p (c t) -> p c t", c=8))),
                         reads=[f"ps{1 + half}", "earena"], writes=[("XeT", sbk, half)])
            xek = [("XeT", sbk, half) for sbk in range(NSB) for half in range(2)]
            for fc in range(FE // 256):
                b = ec["w"] % 2
                ec["w"] += 1
                P.dma("pool", wgt[b], wg[ex].rearrange("(kc p) n -> p kc n", p=128)[:, :, fc * 256:(fc + 1) * 256], writes=[f"wbuf{b}"], key=f"wg{b}")
                P.dma("pool", wut[b], wu[ex].rearrange("(kc p) n -> p kc n", p=128)[:, :, fc * 256:(fc + 1) * 256], writes=[f"wbufu{b}"], key=f"wu{b}")
                for half in range(2):
                    f = fc * 2 + half
                    for ch in range(2):
                        g = ec["g"] % 2
                        ec["g"] += 1
                        pg, pu = 3 + g, 5 + g
                        cs = slice(ch * CH, (ch + 1) * CH)
                        for kc in range(KC):
                            P.op("pe", lambda e, kc=kc, b=b, pg=pg, half=half, cs=cs: e.matmul(ps[pg][:, 0:CH], wgt[b][:, kc, half * 128:(half + 1) * 128], XeT[:, kc, cs],
                                                                                               start=(kc == 0), stop=(kc == KC - 1)),
                                 reads=[f"wbuf{b}"] + xek, writes=[f"ps{pg}"])
                        for kc in range(KC):
                            P.op("pe", lambda e, kc=kc, b=b, pu=pu, half=half, cs=cs: e.matmul(ps[pu][:, 0:CH], wut[b][:, kc, half * 128:(half + 1) * 128], XeT[:, kc, cs],
                                                                                               start=(kc == 0), stop=(kc == KC - 1)),
                                 reads=[f"wbufu{b}"] + xek, writes=[f"ps{pu}"])
                        P.op("act", lambda e, g=g, pg=pg: e.activation(out=sg[g][:], in_=ps[pg][:, 0:CH], func=AF.Silu), reads=[f"ps{pg}"], writes=[f"sg{g}"])
                        P.op("dve", lambda e, g=g, pu=pu, f=f, cs=cs: e.tensor_tensor(out=actT[:, f, cs], in0=sg[g][:], in1=ps[pu][:, 0:CH], op=ALU.mult),
                             reads=[f"sg{g}", f"ps{pu}", "earena"], writes=[("act", f)])
            actk = [("act", f) for f in range(NF)]
            for dc in range(D // 256):
                b = ec["w"] % 2
                ec["w"] += 1
                P.dma("pool", wdt[b], wd[ex].rearrange("(f p) n -> p f n", p=128)[:, :, dc * 256:(dc + 1) * 256], writes=[f"wbuf{b}", f"wbufu{b}"], key=f"wg{b}")
                for sbk in range(NSB):
                    pb = 1 + cntr["po"] % 2
                    cntr["po"] += 1
                    for f in range(NF):
                        P.op("pe", lambda e, f=f, b=b, pb=pb, sbk=sbk: e.matmul(ps[pb][:, 0:256], actT[:, f, sbk * 128:(sbk + 1) * 128], wdt[b][:, f, :],
                                                                               start=(f == 0), stop=(f == NF - 1)),
                             reads=[f"wbuf{b}"] + actk, writes=[f"ps{pb}"])
                    yb = ec["y"] % 3
                    ec["y"] += 1
                    P.op("act", lambda e, yb=yb, pb=pb: e.activation(out=ystg[yb][:, 0:256], in_=ps[pb][:, 0:256], func=AF.Copy),
                         reads=[f"ps{pb}", "earena"], writes=[f"ystg{yb}"])
                    P.dma("sp", Yd[ex * C + sbk * 128: ex * C + (sbk + 1) * 128, dc * 256:(dc + 1) * 256], ystg[yb][:, 0:256], reads=[f"ystg{yb}"], writes=[("Yd", ex)], key=f"yst{yb}")
        retire = [("XeT", sbk, half) for sbk in range(NSB) for half in range(2)] + [("act", f) for f in range(NF)] + \
                 [f"ystg{i}" for i in range(3)] + ["xrow0", "xrow1", "earena"]
        P.op("pool", lambda e: e.memset(dmy[:], 1.0), writes=retire + ["carena"])
        for sidx in range(NS):
            cb_ = sidx % 2
            P.idma(cy1[cb_], None, Yd, dst[:, sidx, 0:1], reads=[("Yd", ex_) for ex_ in range(NE)] + [("dst", sidx), "carena"], writes=[f"cy1_{cb_}"], key=("g1", cb_), bounds=NE * C - 1)
            P.idma(cy2[cb_], None, Yd, dst[:, sidx, 1:2], reads=[("Yd", ex_) for ex_ in range(NE)] + [("dst", sidx), "carena"], writes=[f"cy2_{cb_}"], key=("g2", cb_), bounds=NE * C - 1)
            P.dma("sp", cx1[cb_], x1tok_s[sidx * 128:(sidx + 1) * 128, :], reads=["x1tok_s", "carena"], writes=[f"cx1_{cb_}"])
            P.op("dve", lambda e, cb_=cb_, sidx=sidx: e.scalar_tensor_tensor(out=cx1[cb_], in0=cy1[cb_], scalar=wts[:, sidx, 0:1], in1=cx1[cb_], op0=ALU.mult, op1=ALU.add),
                 reads=[f"cy1_{cb_}", f"cx1_{cb_}", ("wts", sidx)], writes=[f"cx1_{cb_}"])
            P.op("dve", lambda e, cb_=cb_, sidx=sidx: e.scalar_tensor_tensor(out=cx1[cb_], in0=cy2[cb_], scalar=wts[:, sidx, 1:2], in1=cx1[cb_], op0=ALU.mult, op1=ALU.add),
                 reads=[f"cy2_{cb_}", f"cx1_{cb_}", ("wts2", sidx)], writes=[f"cx1_{cb_}"])
            P.op("act", lambda e, cb_=cb_: e.activation(out=cy1[cb_], in_=cx1[cb_], func=AF.Square, accum_out=ssum[:, 1:2]), reads=[f"cx1_{cb_}"], writes=[f"cy1_{cb_}", "ssum2"])
            P.op("act", lambda e: e.activation(out=rstd[:], in_=ssum[:, 1:2], func=AF.Sqrt, bias=EPS, scale=1.0 / D), reads=["ssum2"], writes=["rstd"])
            P.op("dve", lambda e: e.reciprocal(out=rstd[:], in_=rstd[:]), reads=["rstd"], writes=["rstd"])
            P.op("dve", lambda e, cb_=cb_: e.scalar_tensor_tensor(out=cy2[cb_], in0=cx1[cb_], scalar=rstd[:, 0:1], in1=fgb[:], op0=ALU.mult, op1=ALU.mult),
                 reads=[f"cx1_{cb_}", "rstd", "fgb"], writes=[f"cy2_{cb_}"])
            P.dma("sp", out[sidx * 128:(sidx + 1) * 128, :], cy2[cb_], reads=[f"cy2_{cb_}"], key=("ost", cb_))
        P.wait_all_dma("sp")
        P.emit()
    return nc


def fm(a):
    T, C = a.shape
    return np.ascontiguousarray(a.T.reshape(C // 128, 128, T))

def pcols(v):
    return np.ascontiguousarray(v.reshape(-1, 128).T)

def prep_A(inp, l, j):
    DL = 1024
    w_in = inp["w_in"][l]
    blk = [2 * j, 2 * j + 1]
    cols = []
    for n in blk: cols.append(np.arange(n * 128, (n + 1) * 128))
    for n in blk: cols.append(DL + np.arange(n * 128, (n + 1) * 128))
    for part in range(3):
        for h in blk: cols.append(2 * DL + part * 1024 + np.arange(h * 128, (h + 1) * 128))
    for h in blk: cols.append(2 * DL + 3 * 1024 + np.arange(h * 128, (h + 1) * 128))
    base = 2 * DL + 4 * 1024
    cols.append(np.array([base + blk[0], base + blk[1], base + 8 + blk[0], base + 8 + blk[1]]))
    cols = np.concatenate(cols)
    wc = np.ascontiguousarray(w_in[:, cols])
    lru_p = np.zeros((128, 2, 8), np.float32)
    lru_w = np.zeros((128, 2, 2, 128), np.float32)
    for i, n in enumerate(blk):
        sl = slice(n * 128, (n + 1) * 128)
        lru_p[:, i, 0:4] = inp["conv_lru_w"][l][:, sl].T
        lru_p[:, i, 4] = inp["conv_lru_b"][l][sl]
        lru_p[:, i, 5] = inp["lru_b_r"][l][n]
        lru_p[:, i, 6] = inp["lru_b_i"][l][n]
        lru_p[:, i, 7] = inp["lru_lambda"][l][sl]
        lru_w[:, i, 0, :] = inp["lru_w_r"][l][n]
        lru_w[:, i, 1, :] = inp["lru_w_i"][l][n]
    dn_cw = np.zeros((128, 6, 4), np.float32)
    cq = inp["conv_qkv_w"][l]
    for part in range(3):
        for i, h in enumerate(blk):
            dn_cw[:, part * 2 + i, :] = cq[:, part * 1024 + h * 128: part * 1024 + (h + 1) * 128].T
    dn_p4 = np.zeros((4, 2), np.float32)
    dn_p4[2:4, 0] = inp["dn_dt_bias"][l][blk]
    dn_p4[2:4, 1] = inp["dn_a_log"][l][blk]
    return {"wc": wc, "ngain": pcols(inp["norm_mix"][l]), "lru_p": lru_p, "lru_w": lru_w, "dn_cw": dn_cw,
            "dn_p4": dn_p4, "dn_g": np.ascontiguousarray(inp["dn_out_norm"][l].reshape(128, 1))}

S_FULL = 8192
NTC = 2048
CAP = 1024
FE_ = 3072
FF_ = 6144


def _fm(a):
    T, C = a.shape
    return np.ascontiguousarray(a.T.reshape(C // 128, 128, T))


def _pcols(v):
    return np.ascontiguousarray(v.reshape(-1, 128).T)


_NC_CACHE = {}
LAST_COUNTS = None


def _get(name, fn):
    if name not in _NC_CACHE:
        _NC_CACHE[name] = fn()
    return _NC_CACHE[name]


def kernel(**inp):
    inp = {k: np.asarray(v) for k, v in inp.items()}
    x = inp["x"].astype(np.float32)
    cores = list(range(8))
    cstA = consts_A()
    cstM = consts_M(CAP)
    out = None
    for l in range(2):
        ncA = _get("A", lambda: build_A(S_FULL))
        xTb = [_fm(x[b]) for b in range(2)]
        maps = []
        for c in cores:
            b, j = c // 4, c % 4
            m = {"xT": xTb[b]}
            m.update(prep_A(inp, l, j))
            m.update(cstA)
            maps.append(m)
        resA = run_bass_kernel_spmd(ncA, maps, core_ids=cores)
        yA = [r["yT"] for r in resA.results]
        del maps
        mapsB = []
        for c in cores:
            b, q = c // 4, c % 4
            ts = slice(q * NTC, (q + 1) * NTC)
            ymT = np.empty((16, 128, NTC), np.float32)
            for n in range(8):
                ymT[n] = yA[b * 4 + n // 2][n % 2][:, ts]
                ymT[8 + n] = yA[b * 4 + n // 2][2 + n % 2][:, ts]
            m = {"ymT": ymT, "xT": np.ascontiguousarray(xTb[b][:, :, ts]), "wout": inp["w_out"][l],
                 "lgain": _pcols(inp["lru_out_norm"][l]), "ngain": _pcols(inp["norm_ffn"][l])}
            if l == 0:
                m.update({"wg": inp["ffn_w_gate"][0], "wu": inp["ffn_w_up"][0], "wd": inp["ffn_w_down"][0]})
            else:
                m.update({"ngrow": np.ascontiguousarray(inp["norm_ffn"][l].reshape(1, -1)),
                          "fgrow": np.ascontiguousarray(inp["norm_final"].reshape(1, -1)),
                          "router": inp["moe_router"][0], "wg": inp["moe_w_gate"][0], "wu": inp["moe_w_up"][0],
                          "wd": inp["moe_w_down"][0]})
                m.update(cstM)
            mapsB.append(m)
        del yA
        if l == 0:
            ncB = _get("B", lambda: build_B(NTC, FF_))
            resB = run_bass_kernel_spmd(ncB, mapsB, core_ids=cores)
            xn = np.empty_like(x)
            for c in cores:
                b, q = c // 4, c % 4
                xn[b, q * NTC:(q + 1) * NTC, :] = resB.results[c]["x2T"].reshape(2048, NTC).T
            x = xn
        else:
            ncM = _get("M", lambda: build_M(NTC, FE_, CAP))
            resM = run_bass_kernel_spmd(ncM, mapsB, core_ids=cores)
            out = np.empty((2, S_FULL, 2048), np.float32)
            for c in cores:
                b, q = c // 4, c % 4
                out[b, q * NTC:(q + 1) * NTC, :] = resM.results[c]["out"]
            global LAST_COUNTS
            LAST_COUNTS = np.stack([resM.results[c]["cnt_out"][0] for c in cores])
        del mapsB
    return out
```

```python
import numpy as np
from contextlib import ExitStack
import concourse.bass as bass
import concourse.mybir as mybir
from concourse.bass_utils import run_bass_kernel_spmd


F32 = mybir.dt.float32
BF16 = mybir.dt.bfloat16
I32 = mybir.dt.int32
AF = mybir.ActivationFunctionType
ALU = mybir.AluOpType
AX = mybir.AxisListType

SEM_ROLL = 30000


class Prog:
    ENGS = ("pe", "dve", "act", "pool", "sp")

    def __init__(self, nc, stack):
        self.nc = nc
        self.stack = stack
        self.q = {e: [] for e in self.ENGS}
        self.cnt = {e: 0 for e in self.ENGS}
        self.sem = {}
        self.nsem = 0
        for e in self.ENGS:
            self.sem[e] = self._newsem("e_" + e)
        self.seen = {e: {} for e in self.ENGS}
        self.lastw = {}
        self.readers = {}
        self.dsem = {}
        self.n_ops = 0

    def _newsem(self, name):
        self.nsem += 1
        sm = self.stack.enter_context(self.nc.semaphore(f"{name}_{self.nsem}"))
        if not hasattr(self, "semname"):
            self.semname = {}
        self.semname[id(sm)] = f"{name}_{self.nsem}"
        return sm

    def sb(self, name, shape, dt):
        return self.stack.enter_context(self.nc.sbuf_tensor(name, list(shape), dt))

    def ps(self, name, shape, dt=F32):
        return self.stack.enter_context(self.nc.psum_tensor(name, list(shape), dt))

    def _deps(self, eng, reads, writes):
        need = []
        for k in reads:
            w = self.lastw.get(k)
            if w is not None:
                need.append(w)
        for k in writes:
            w = self.lastw.get(k)
            if w is not None:
                need.append(w)
            need.extend(self.readers.get(k, ()))
        best = {}
        for s, v in need:
            if best.get(id(s), (None, -1))[1] < v:
                best[id(s)] = (s, v)
        out = []
        seen = self.seen[eng]
        for sid, (s, v) in best.items():
            if eng == "pe" and s is self.sem["pe"]:
                continue
            if seen.get(sid, 0) >= v:
                continue
            seen[sid] = v
            out.append((s, v))
        return out

    def _commit(self, reads, writes, tok):
        for k in writes:
            self.lastw[k] = tok
            self.readers[k] = []
        for k in reads:
            self.readers.setdefault(k, []).append(tok)

    def defer_start(self):
        self._defer = []

    def defer_stop(self):
        d = self._defer
        self._defer = None
        return d

    def run_deferred(self, lst, n=None):
        n = len(lst) if n is None else min(n, len(lst))
        for _ in range(n):
            kind, a, kw = lst.pop(0)
            getattr(self, kind)(*a, **kw)

    def op(self, eng, fn, reads=(), writes=()):
        if getattr(self, "_defer", None) is not None:
            self._defer.append(("op", (eng, fn), {"reads": list(reads), "writes": list(writes)}))
            return
        if self.cnt[eng] >= SEM_ROLL:
            self.sem[eng] = self._newsem("e_" + eng)
            self.cnt[eng] = 0
        waits = self._deps(eng, reads, writes)
        self.cnt[eng] += 1
        sem = self.sem[eng]
        tok = (sem, self.cnt[eng])
        self.q[eng].append((fn, waits, sem, 1))
        self._commit(reads, writes, tok)
        self.n_ops += 1
        if getattr(self, "log", None) is not None:
            self.log.append((eng, self.cnt[eng], list(reads), list(writes), [(self.semname.get(id(s), "?"), v) for s, v in waits]))

    def dma(self, eng, out, in_, reads=(), writes=(), key=None, **kw):
        assert eng in ("sp", "pool", "act")
        if getattr(self, "_defer", None) is not None:
            kw2 = dict(kw); kw2.update({"reads": list(reads), "writes": list(writes), "key": key})
            self._defer.append(("dma", (eng, out, in_), kw2))
            return
        if key is None:
            key = ("dma",) + tuple(writes) + tuple(reads)
        if key not in self.dsem:
            self.dsem[key] = [self._newsem("d"), 0]
        ent = self.dsem[key]
        sem = ent[0]
        waits = self._deps(eng, reads, writes)
        if ent[1] > 0 and self.seen[eng].get(id(sem), 0) < ent[1]:
            self.seen[eng][id(sem)] = ent[1]
            waits.append((sem, ent[1]))
        ent[1] += 16
        tok = (sem, ent[1])

        def fn(e, out=out, in_=in_, kw=kw):
            o = out(e) if callable(out) else out
            i = in_(e) if callable(in_) else in_
            return e.dma_start(out=o, in_=i, **kw)
        self.q[eng].append((fn, waits, sem, 16))
        self._commit(reads, writes, tok)
        self.n_ops += 1
        return tok

    def idma(self, out, out_off, in_, in_off, reads=(), writes=(), key=None, bounds=None):
        eng = "pool"
        if key not in self.dsem:
            self.dsem[key] = [self._newsem("d"), 0]
        ent = self.dsem[key]
        sem = ent[0]
        waits = self._deps(eng, reads, writes)
        if ent[1] > 0 and self.seen[eng].get(id(sem), 0) < ent[1]:
            self.seen[eng][id(sem)] = ent[1]
            waits.append((sem, ent[1]))
        ent[1] += 16
        tok = (sem, ent[1])

        def fn(e):
            oo = bass.IndirectOffsetOnAxis(ap=out_off, axis=0) if out_off is not None else None
            io = bass.IndirectOffsetOnAxis(ap=in_off, axis=0) if in_off is not None else None
            bc = None
            if bounds is not None:
                regs = self.__dict__.setdefault("_bregs", {})
                if bounds not in regs:
                    regs[bounds] = e.to_reg(bounds)
                bc = regs[bounds]
            return e.indirect_dma_start(out=out, out_offset=oo, in_=in_, in_offset=io, bounds_check=bc, oob_is_err=False)
        self.q[eng].append((fn, waits, sem, 16))
        self._commit(reads, writes, tok)
        return tok

    def wait_all_dma(self, eng="sp"):
        waits = []
        for key, (sem, val) in self.dsem.items():
            if val > 0:
                waits.append((sem, val))
        self.q[eng].append((None, waits, None, 0))

    def emit(self):
        nc = self.nc
        engmap = {"pe": "tensor", "dve": "vector", "act": "scalar", "pool": "gpsimd", "sp": "sync"}
        with nc.Block() as block:
            for ename in self.ENGS:
                lst = self.q[ename]

                def body(e, lst=lst):
                    for fn, waits, sem, inc in lst:
                        for s, v in waits:
                            e.wait_ge(s, v)
                        if fn is not None:
                            fn(e).then_inc(sem, inc)
                getattr(block, engmap[ename])(body)


EPS = 1e-6
D = 2048
KC = 16
NCOL = 1540
TB = 512
NCH = TB // 64


def consts_A():
    i = np.arange(64)
    ms = (i[:, None] > i[None, :]).astype(np.float32)
    msT = (i[:, None] < i[None, :]).astype(np.float32)
    miT = (i[:, None] <= i[None, :]).astype(np.float32)
    idn = np.eye(64, dtype=np.float32)
    c64 = np.stack([np.tile(m, (1, NCH)) for m in (ms, msT, miT, idn)], 0)
    c64p = np.zeros((4, 128, TB), np.float32)
    c64p[:, :64] = c64
    ident = np.eye(128, dtype=np.float32)
    small = np.zeros((128, 4 + 4 * 128 + TB), np.float32)
    small[:4, 0:4] = np.eye(4)
    for r in range(4):
        small[r, 4 + r * 128: 4 + (r + 1) * 128] = 1.0
    rm = np.ones(TB, np.float32)
    rm[::64] = 0.0
    small[:4, 4 + 512:] = rm[None, :]
    return {"c64": c64p, "ident": ident, "small": small}


HORDER = [0, 1]
INV_BF16 = True
DEBUG = False


def build_A(S):
    nc = bass.Bass("TRN2", target_bir_lowering=False)
    NB = S // TB
    dr = lambda n, s, k="ExternalInput": nc.dram_tensor(n, list(s), F32, kind=k).ap()
    xT = dr("xT", [KC, 128, S])
    wc = dr("wc", [D, NCOL])
    ngain = dr("ngain", [128, KC])
    lru_p = dr("lru_p", [128, 2, 8])
    lru_w = dr("lru_w", [128, 2, 2, 128])
    dn_cw = dr("dn_cw", [128, 6, 4])
    dn_p4 = dr("dn_p4", [4, 2])
    dn_g = dr("dn_g", [128, 1])
    c64 = dr("c64", [4, 128, TB])
    identd = dr("ident", [128, 128])
    smalld = dr("small", [128, 4 + 512 + TB])
    yT = dr("yT", [4, 128, S], "ExternalOutput")
    dbg = dr("dbg", [24, 128, TB], "ExternalOutput") if DEBUG else None

    xv = xT.rearrange("c p t -> p c t")
    wcv = wc.rearrange("(kc p) n -> p kc n", p=128)

    with ExitStack() as st:
        P = Prog(nc, st)
        sb, ps_ = P.sb, P.ps
        W = sb("W", [128, KC, NCOL], BF16)
        ng = sb("ng", [128, KC], F32)
        lp = sb("lp", [128, 2, 8], F32)
        lw32 = sb("lw32", [128, 2, 2, 128], F32)
        lw = sb("lw", [128, 2, 2, 128], BF16)
        c1 = sb("c1", [128, 2], F32)
        dcw = sb("dcw", [128, 6, 4], F32)
        p4 = sb("p4", [4, 2], F32)
        negA = sb("negA", [4, 1], F32)
        dng = sb("dng", [128, 1], F32)
        cm = sb("cm", [128, 4, TB], BF16)
        ident = sb("identf", [128, 128], F32)
        identb = sb("identb", [128, 128], BF16)
        small = sb("smallc", [128, 4 + 512 + TB], F32)
        ones = sb("ones", [128, 128], BF16)
        I4 = small[0:4, 0:4]
        sel = lambda r: small[0:4, 4 + r * 128: 4 + (r + 1) * 128]
        rmask = small[0:4, 4 + 512: 4 + 512 + TB]
        Ms, MsT, MiT, Id8 = cm[0:64, 0, :], cm[0:64, 1, :], cm[0:64, 2, :], cm[0:64, 3, :]

        x32 = [sb(f"x32_{k}", [128, 4, TB], F32) for k in range(2)]
        sq = sb("sq", [128, 4, TB], BF16)
        hT = sb("hT", [128, KC, TB], BF16)
        rs = sb("rs", [128, TB], F32)
        xbuf = [sb(f"xbuf{n}", [128, TB + 3], F32) for n in range(2)]
        hlast = [sb(f"hlast{n}", [128, 1], F32) for n in range(2)]
        gl = [sb(f"gl{n}", [128, TB], F32) for n in range(2)]
        cb = [sb(f"cb{k}", [128, TB + 3], F32) for k in range(6)]
        sgate2 = [[sb(f"sgate{pb_}_{h}", [128, TB], F32) for h in range(2)] for pb_ in range(2)]
        r4 = sb("r4", [4, 5, TB], F32)
        colt = sb("colt", [64, NCH, 16], F32)
        cole = sb("cole", [64, NCH, 2], F32)
        qkv = [sb(f"qkv{k}", [128, TB], F32) for k in range(3)]
        qkvb = [sb(f"qkvb{k}", [128, TB], BF16) for k in range(3)]
        Gb = sb("Gb", [128, TB], F32)
        betab = sb("betab", [64, TB], F32)
        EGb = sb("EGb", [128, TB], F32)
        qdTb = sb("qdTb", [128, TB], BF16)
        t1 = sb("t1", [64, TB], F32)
        eT = sb("eT", [64, TB], F32)
        eLf = sb("eLf", [128, TB], F32)
        eL = eLf[0:64, :]
        IDT = BF16 if INV_BF16 else F32
        Nm = [sb(f"Nm{k}", [64, TB], IDT) for k in range(2)]
        NmT = [sb(f"NmT{k}", [64, TB], IDT) for k in range(2)]
        Pm = sb("Pm", [64, TB], IDT)
        qkT = sb("qkT", [64, TB], BF16)
        vb = sb("vb", [64, NCH, 128], IDT)
        kbg = sb("kbg", [64, NCH, 128], IDT)
        kdec = sb("kdec", [64, NCH, 128], BF16)
        u_t = sb("u_t", [64, NCH, 128], F32)
        wTb = sb("wTb", [128, TB], BF16)
        vnew = sb("vnew", [64, 128], BF16)
        S32 = [sb(f"S32_{h}", [128, 128], F32) for h in range(2)]
        Sb = [sb(f"Sb_{h}", [128, 128], BF16) for h in range(2)]
        o32 = sb("o32", [128, TB], F32)
        yo = [sb(f"yo{k}", [128, TB], F32) for k in range(2)]
        l2r0 = sb("l2r0", [128, TB], F32)
        xc, r_t, i_t = qkv[0], qkv[1], qkv[2]
        xcb = qkvb[0]
        a_t, m_t, h_t = Gb, EGb, o32
        XC, XCB, RT, IT, ATk, MTk, HTk = "qkv0", "qkvb0", "qkv1", "qkv2", "Gb", "EGb", "o32"
        def t1_full(k):
            return l2r0[:] if k == 0 else eLf[:]
        L2K = ["l2r0", "eL"]

        psn = ps_("psn", [128, 512])
        pp = [ps_(f"pp{k}", [128, 512]) for k in range(2)]
        pA = ps_("pA", [128, 512])
        pB = ps_("pB", [128, 512])
        pC = ps_("pC", [128, 512])
        pTk = pC[:].bitcast(BF16)
        pR = ps_("pR", [128, 512])
        pO = ps_("pO", [128, 512])

        P.dma("sp", ng[:], ngain, writes=["ng"])
        P.dma("sp", lp[:], lru_p, writes=["lp"])
        P.dma("sp", lw32[:], lru_w, writes=["lw32"])
        P.dma("sp", dcw[:], dn_cw, writes=["dcw"])
        P.dma("sp", p4[:], dn_p4, writes=["p4"])
        P.dma("sp", dng[:], dn_g, writes=["dng"])
        P.dma("pool", cm[:], c64.rearrange("m p t -> p m t"), writes=["cm"])
        P.dma("sp", ident[:], identd, writes=["ident"])
        P.dma("sp", small[:], smalld, writes=["small"])
        for k4 in range(4):
            c0 = k4 * 385
            P.dma("pool", W[:, :, c0:c0 + 385], wcv[:, :, c0:c0 + 385], writes=[("W", k4)], key=("W", k4))
        Wk = [("W", k4) for k4 in range(4)]
        P.op("dve", lambda e: e.memset(ones[:], 1.0), writes=["ones"])
        P.op("dve", lambda e: e.tensor_copy(out=identb[:], in_=ident[:]), reads=["ident"], writes=["identb"])
        P.op("dve", lambda e: e.tensor_copy(out=lw[:], in_=lw32[:]), reads=["lw32"], writes=["lw"])
        P.op("act", lambda e: e.activation(out=c1[:], in_=lp[:, :, 7], func=AF.Exp, scale=-1.0), reads=["lp"], writes=["c1"])
        P.op("act", lambda e: e.activation(out=c1[:], in_=c1[:], func=AF.Ln, bias=1.0), reads=["c1"], writes=["c1"])
        P.op("dve", lambda e: e.tensor_scalar(out=c1[:], in0=c1[:], scalar1=-8.0, scalar2=None, op0=ALU.mult), reads=["c1"], writes=["c1"])
        P.op("act", lambda e: e.activation(out=negA[:], in_=p4[:, 1:2], func=AF.Exp), reads=["p4"], writes=["negA"])
        P.op("dve", lambda e: e.tensor_scalar(out=negA[:], in0=negA[:], scalar1=-1.0, scalar2=None, op0=ALU.mult), reads=["negA"], writes=["negA"])
        for n in range(2):
            P.op("pool", lambda e, n=n: e.memset(xbuf[n][:, 0:3], 0.0), writes=[f"xbuf{n}"])
            P.op("pool", lambda e, n=n: e.memset(hlast[n][:], 0.0), writes=[f"hlast{n}"])
            P.op("pool", lambda e, n=n: e.memset(S32[n][:], 0.0), writes=[f"S32_{n}"])
            P.op("pool", lambda e, n=n: e.memset(Sb[n][:], 0.0), writes=[f"Sb_{n}"])
        for k in range(6):
            P.op("pool", lambda e, k=k: e.memset(cb[k][:, 0:3], 0.0), writes=[f"cb{k}"])

        def conv(eng, out, buf, wtile, widx, bias, rk, wk, outk):
            if bias is None:
                P.op(eng, lambda e: e.tensor_scalar(out=out, in0=buf[:, 0:TB], scalar1=wtile[:, widx, 0:1], scalar2=None, op0=ALU.mult),
                     reads=[rk, wk], writes=[outk])
            else:
                P.op(eng, lambda e: e.tensor_scalar(out=out, in0=buf[:, 0:TB], scalar1=wtile[:, widx, 0:1], scalar2=bias, op0=ALU.mult, op1=ALU.add),
                     reads=[rk, wk], writes=[outk])
            for k in range(1, 4):
                P.op(eng, lambda e, k=k: e.scalar_tensor_tensor(out=out, in0=buf[:, k:k + TB], scalar=wtile[:, widx, k:k + 1], in1=out,
                                                              op0=ALU.mult, op1=ALU.add),
                     reads=[rk, wk, outk], writes=[outk])

        ycnt = [0]
        dcnt = [0]

        def dump(ap, key, npart=128):
            if not DEBUG:
                return
            slot = dcnt[0]
            dcnt[0] += 1
            P.dma("sp", dbg[slot, 0:npart, :], ap, reads=[key], key=("dbg", slot))

        def store_y(tile_idx, t0, src_key, src):
            P.dma("sp", yT[tile_idx, :, t0:t0 + TB], src, reads=[src_key], key=("yst", src_key))

        pending = []

        def pump(n):
            P.run_deferred(pending, n)

        for blk in range(NB):
            t0 = blk * TB
            def emit_front(blk):
                t0 = blk * TB
                sgate = sgate2[blk % 2]
                SGK = [f"sgate{blk % 2}_{h}" for h in range(2)]
                for q4 in range(4):
                    xb = x32[q4 % 2]
                    xk = f"x32_{q4 % 2}"
                    P.dma("sp", xb[:], xv[:, q4 * 4:(q4 + 1) * 4, t0:t0 + TB], writes=[xk])
                    P.op("act", lambda e, xb=xb: e.activation(out=sq[:], in_=xb[:], func=AF.Square), reads=[xk], writes=["sq"])
                    for j in range(4):
                        jj = q4 * 4 + j
                        P.op("pe", lambda e, j=j, jj=jj: e.matmul(psn[:], ones[:], sq[:, j, :], start=(jj == 0), stop=(jj == KC - 1)),
                             reads=["ones", "sq"], writes=["psn"])
                        P.op("dve", lambda e, j=j, jj=jj, xb=xb: e.tensor_scalar(out=hT[:, jj, :], in0=xb[:, j, :], scalar1=ng[:, jj:jj + 1], scalar2=None, op0=ALU.mult),
                             reads=[xk, "ng"], writes=[("hT", jj)])
                P.op("act", lambda e: e.activation(out=rs[:], in_=psn[:], func=AF.Ln, bias=EPS, scale=1.0 / D), reads=["psn"], writes=["rs"])
                P.op("act", lambda e: e.activation(out=rs[:], in_=rs[:], func=AF.Exp, scale=-0.5), reads=["rs"], writes=["rs"])

                def proj(ct, M):
                    pb = pp[ct % 2]
                    c0 = ct * 128
                    for kc in range(KC):
                        P.op("pe", lambda e, kc=kc: e.matmul(pb[0:M, :], W[:, kc, c0:c0 + M], hT[:, kc, :], start=(kc == 0), stop=(kc == KC - 1)),
                             reads=Wk + [("hT", kc)], writes=[f"pp{ct % 2}"])
                    return pb, f"pp{ct % 2}"

                for n in range(2):
                    pb, pk = proj(n, 128)
                    P.op("dve", lambda e, n=n, pb=pb: e.tensor_tensor(out=xbuf[n][:, 3:3 + TB], in0=pb[:], in1=rs[:], op=ALU.mult),
                         reads=[pk, "rs"], writes=[f"xbuf{n}"])
                for n in range(2):
                    pb, pk = proj(2 + n, 128)
                    P.op("dve", lambda e, n=n, pb=pb: e.tensor_tensor(out=gl[n][:], in0=pb[:], in1=rs[:], op=ALU.mult),
                         reads=[pk, "rs"], writes=[f"gl{n}"])
                    P.op("act", lambda e, n=n: e.activation(out=gl[n][:], in_=gl[n][:], func=AF.Gelu_apprx_tanh), reads=[f"gl{n}"], writes=[f"gl{n}"])
                for k in range(6):
                    pb, pk = proj(4 + k, 128)
                    P.op("dve", lambda e, k=k, pb=pb: e.tensor_tensor(out=cb[k][:, 3:3 + TB], in0=pb[:], in1=rs[:], op=ALU.mult),
                         reads=[pk, "rs"], writes=[f"cb{k}"])
                for h in range(2):
                    pb, pk = proj(10 + h, 128)
                    P.op("dve", lambda e, h=h, pb=pb: e.tensor_tensor(out=sgate[h][:], in0=pb[:], in1=rs[:], op=ALU.mult),
                         reads=[pk, "rs"], writes=[SGK[h]])
                    P.op("act", lambda e, h=h: e.activation(out=sgate[h][:], in_=sgate[h][:], func=AF.Silu), reads=[SGK[h]], writes=[SGK[h]])
                pb, pk = proj(12, 4)
                P.op("dve", lambda e, pb=pb: e.tensor_tensor(out=r4[:, 0, :], in0=pb[0:4, :], in1=rs[0:4, :], op=ALU.mult),
                     reads=[pk, "rs"], writes=["s4"])


            sgate = sgate2[blk % 2]
            SGK = [f"sgate{blk % 2}_{h}" for h in range(2)]
            if blk == 0:
                emit_front(0)
            else:
                P.run_deferred(pending)
            for n in range(2):
                conv("dve", xc[:], xbuf[n], lp, n, lp[:, n, 4:5], f"xbuf{n}", "lp", XC)
                P.op("pool", lambda e, n=n: e.tensor_copy(out=xbuf[n][:, 0:3], in_=xbuf[n][:, TB:TB + 3]), reads=[XC], writes=[f"xbuf{n}"])
                P.op("act", lambda e: e.activation(out=xcb[:], in_=xc[:], func=AF.Copy), reads=[XC], writes=[XCB])
                P.op("pe", lambda e, n=n: e.matmul(pA[:], lw[:, n, 0, :], xcb[:], start=True, stop=True), reads=["lw", XCB], writes=["pA"])
                P.op("pe", lambda e, n=n: e.matmul(pB[:], lw[:, n, 1, :], xcb[:], start=True, stop=True), reads=["lw", XCB], writes=["pB"])
                P.op("act", lambda e, n=n: e.activation(out=r_t[:], in_=pA[:], func=AF.Sigmoid, bias=lp[:, n, 5:6]), reads=["pA", "lp"], writes=[RT])
                P.op("act", lambda e, n=n: e.activation(out=i_t[:], in_=pB[:], func=AF.Sigmoid, bias=lp[:, n, 6:7]), reads=["pB", "lp"], writes=[IT])
                P.op("act", lambda e, n=n: e.activation(out=a_t[:], in_=r_t[:], func=AF.Exp, scale=c1[:, n:n + 1]), reads=[RT, "c1"], writes=[ATk])
                P.op("act", lambda e: e.activation(out=m_t[:], in_=a_t[:], func=AF.Square), reads=[ATk], writes=[MTk])
                P.op("act", lambda e: e.activation(out=m_t[:], in_=m_t[:], func=AF.Sqrt, bias=1.0, scale=-1.0), reads=[MTk], writes=[MTk])
                P.op("dve", lambda e: e.tensor_tensor(out=i_t[:], in0=i_t[:], in1=xc[:], op=ALU.mult), reads=[IT, XC], writes=[IT])
                P.op("dve", lambda e: e.tensor_tensor(out=m_t[:], in0=m_t[:], in1=i_t[:], op=ALU.mult), reads=[MTk, IT], writes=[MTk])
                P.op("dve", lambda e, n=n: e.tensor_tensor_scan(out=h_t[:], data0=a_t[:], data1=m_t[:], initial=hlast[n][:, 0:1],
                                                               op0=ALU.mult, op1=ALU.add),
                     reads=[ATk, MTk, f"hlast{n}"], writes=[HTk])
                P.op("pool", lambda e, n=n: e.tensor_copy(out=hlast[n][:], in_=h_t[:, TB - 1:TB]), reads=[HTk], writes=[f"hlast{n}"])
                yk = ycnt[0] % 2
                ycnt[0] += 1
                P.op("dve", lambda e, n=n, yk=yk: e.tensor_tensor(out=yo[yk][:], in0=h_t[:], in1=gl[n][:], op=ALU.mult),
                     reads=[HTk, f"gl{n}"], writes=[f"yo{yk}"])
                store_y(n, t0, f"yo{yk}", yo[yk][:])

            s4, B4, g4, G4, EG4 = (r4[:, k, :] for k in range(5))
            P.op("act", lambda e: e.activation(out=B4, in_=s4, func=AF.Sigmoid), reads=["s4"], writes=["B4"])
            P.op("act", lambda e: e.activation(out=g4, in_=s4, func=AF.Exp, bias=p4[:, 0:1]), reads=["s4", "p4"], writes=["g4"])
            P.op("act", lambda e: e.activation(out=g4, in_=g4, func=AF.Ln, bias=1.0), reads=["g4"], writes=["g4"])
            P.op("dve", lambda e: e.tensor_scalar(out=g4, in0=g4, scalar1=negA[:, 0:1], scalar2=None, op0=ALU.mult), reads=["g4", "negA"], writes=["g4"])
            P.op("dve", lambda e: e.tensor_tensor_scan(out=G4, data0=rmask, data1=g4, initial=0.0, op0=ALU.mult, op1=ALU.add),
                 reads=["g4", "small"], writes=["G4"])
            P.op("act", lambda e: e.activation(out=EG4, in_=G4, func=AF.Exp), reads=["G4"], writes=["EG4"])
            ED4 = s4
            G4c = G4.rearrange("p (c t) -> p c t", t=64)
            P.op("dve", lambda e: e.tensor_tensor(out=ED4.rearrange("p (c t) -> p c t", t=64), in0=G4c[:, :, 63:64].to_broadcast([4, NCH, 64]),
                                                  in1=G4c, op=ALU.subtract), reads=["G4"], writes=["s4"])
            P.op("act", lambda e: e.activation(out=ED4, in_=ED4, func=AF.Exp), reads=["s4"], writes=["s4"])
            quants = [(B4, "B4"), (G4, "G4"), (EG4, "EG4"), (ED4, "s4")]
            for c in range(NCH):
                for qi, (qt, qk_) in enumerate(quants):
                    P.op("pe", lambda e, c=c, qi=qi, qt=qt: e.matmul(pR[0:64, 256 + c * 16 + qi * 4: 256 + c * 16 + qi * 4 + 4],
                                                                     qt[:, c * 64:(c + 1) * 64], I4, start=True, stop=True),
                         reads=[qk_, "small"], writes=["pR"])
            P.op("dve", lambda e: e.tensor_copy(out=colt[:].rearrange("p c q -> p (c q)"), in_=pR[0:64, 256:256 + NCH * 16]), reads=["pR"], writes=["colt"])
            for h in range(2):
                P.op("dve", lambda e, h=h: e.tensor_tensor(out=cole[:, :, h:h + 1], in0=colt[:, :, h:h + 1], in1=colt[:, :, 10 + h:11 + h], op=ALU.mult),
                     reads=["colt"], writes=[("cole", h)])

            for h in HORDER:
                for k in range(3):
                    conv("dve", qkv[k][:], cb[k * 2 + h], dcw, k * 2 + h, None, f"cb{k * 2 + h}", "dcw", f"qkv{k}")
                    P.op("pool", lambda e, k=k, h=h: e.tensor_copy(out=cb[k * 2 + h][:, 0:3], in_=cb[k * 2 + h][:, TB:TB + 3]),
                         reads=[f"qkv{k}"], writes=[f"cb{k * 2 + h}"])
                    P.op("act", lambda e, k=k: e.activation(out=qkv[k][:], in_=qkv[k][:], func=AF.Silu), reads=[f"qkv{k}"], writes=[f"qkv{k}"])
                for k in range(2):
                    P.op("act", lambda e, k=k: e.activation(out=sq[:, 0, :], in_=qkv[k][:], func=AF.Square), reads=[f"qkv{k}"], writes=["sq"])
                    P.op("pe", lambda e: e.matmul(psn[:], ones[:], sq[:, 0, :], start=True, stop=True), reads=["ones", "sq"], writes=["psn"])
                    P.op("act", lambda e, k=k: e.activation(out=t1_full(k), in_=psn[:], func=AF.Ln, bias=EPS, scale=1.0), reads=["psn"], writes=[L2K[k]])
                    P.op("act", lambda e, k=k: e.activation(out=t1_full(k), in_=t1_full(k), func=AF.Exp, scale=-0.5), reads=[L2K[k]], writes=[L2K[k]])
                    sc = (128.0 ** -0.5) if k == 0 else 1.0
                    P.op("dve", lambda e, k=k, sc=sc: e.scalar_tensor_tensor(out=qkvb[k][:], in0=qkv[k][:], scalar=sc, in1=t1_full(k), op0=ALU.mult, op1=ALU.mult),
                         reads=[f"qkv{k}", L2K[k]], writes=[f"qkvb{k}"])
                    if k == 0:
                        P.op("dve", lambda e: e.tensor_tensor(out=qkv[0][:], in0=qkv[0][:], in1=t1_full(0), op=ALU.mult),
                             reads=["qkv0", "l2r0"], writes=["qkv0"])
                P.op("act", lambda e: e.activation(out=qkvb[2][:], in_=qkv[2][:], func=AF.Copy), reads=["qkv2"], writes=["qkvb2"])
                qnb, knb, vcb = qkvb
                overlap = (h == HORDER[-1]) and (blk + 1 < NB)
                if overlap:
                    P.defer_start()
                    emit_front(blk + 1)
                    pending[:] = P.defer_stop()
                    pump(4 * (1 + 1 + 4 + 4) + 2)
                P.op("pe", lambda e, h=h: e.matmul(pA[:], sel(2 + h), G4, start=True, stop=True), reads=["G4", "small"], writes=["pA"])
                P.op("act", lambda e: e.activation(out=Gb[:], in_=pA[:], func=AF.Copy), reads=["pA"], writes=["Gb"])
                P.op("act", lambda e: e.activation(out=EGb[:], in_=pA[:], func=AF.Exp), reads=["pA"], writes=["EGb"])
                P.op("pe", lambda e, h=h: e.matmul(pB[:], sel(h), B4, start=True, stop=True), reads=["B4", "small"], writes=["pB"])
                P.op("act", lambda e: e.activation(out=betab[:], in_=pB[0:64, :], func=AF.Copy), reads=["pB"], writes=["betab"])
                P.op("dve", lambda e: e.scalar_tensor_tensor(out=qdTb[:], in0=qkv[0][:], scalar=128.0 ** -0.5, in1=EGb[:], op0=ALU.mult, op1=ALU.mult),
                     reads=["qkv0", "EGb"], writes=["qdTb"])
                if blk == 0 and h == HORDER[0]:
                    dump(Gb[:], "Gb"); dump(EGb[:], "EGb"); dump(qkv[0][:], "qkv0"); dump(betab[:], "betab", 64)
                for c in range(NCH):
                    cs = slice(c * 64, (c + 1) * 64)
                    P.op("pe", lambda e, cs=cs: e.matmul(pA[0:64, cs], knb[:, cs], knb[:, cs], start=True, stop=True), reads=["qkvb1"], writes=["pA"])
                for c in range(NCH):
                    cs = slice(c * 64, (c + 1) * 64)
                    P.op("pe", lambda e, cs=cs: e.matmul(pB[0:64, cs], knb[:, cs], qnb[:, cs], start=True, stop=True), reads=["qkvb1", "qkvb0"], writes=["pB"])
                for c in range(NCH):
                    cs = slice(c * 64, (c + 1) * 64)
                    P.op("pe", lambda e, c=c, cs=cs: e.transpose(pTk[0:64, c * 128:(c + 1) * 128], knb[:, cs], identb[:]), reads=["qkvb1", "identb"], writes=["pC"])
                GcolB = colt[:, :, 6 + h:7 + h].to_broadcast([64, NCH, 64])
                bcolB = colt[:, :, h:h + 1].to_broadcast([64, NCH, 64])
                v3 = lambda t: t.rearrange("p (c t) -> p c t", t=64)
                P.op("dve", lambda e, GcolB=GcolB: e.tensor_tensor(out=v3(t1[:]), in0=v3(Gb[0:64, :]), in1=GcolB, op=ALU.subtract), reads=["Gb", "colt"], writes=["t1"])
                P.op("dve", lambda e: e.tensor_scalar(out=eT[:], in0=t1[:], scalar1=0.0, scalar2=None, op0=ALU.min), reads=["t1"], writes=["eT"])
                P.op("act", lambda e: e.activation(out=eT[:], in_=eT[:], func=AF.Exp), reads=["eT"], writes=["eT"])
                P.op("dve", lambda e: e.tensor_scalar(out=eL[:], in0=t1[:], scalar1=0.0, scalar2=None, op0=ALU.max), reads=["t1"], writes=["eL"])
                P.op("act", lambda e: e.activation(out=eL[:], in_=eL[:], func=AF.Exp, scale=-1.0), reads=["eL"], writes=["eL"])
                if blk == 0 and h == HORDER[0]:
                    dump(t1[:], "t1", 64); dump(eL, "eL", 64)
                P.op("dve", lambda e: e.tensor_tensor(out=eL[:], in0=eL[:], in1=Ms, op=ALU.mult), reads=["eL", "cm"], writes=["eL"])
                P.op("dve", lambda e, bcolB=bcolB: e.tensor_tensor(out=v3(eL[:]), in0=v3(eL[:]), in1=bcolB, op=ALU.mult), reads=["eL", "colt"], writes=["eL"])
                P.op("dve", lambda e: e.scalar_tensor_tensor(out=Nm[0][:], in0=pA[0:64, :], scalar=-1.0, in1=eL[:], op0=ALU.mult, op1=ALU.mult),
                     reads=["pA", "eL"], writes=["Nm0"])
                if blk == 0 and h == HORDER[0]:
                    dump(eL, "eL", 64)
                P.op("dve", lambda e: e.tensor_tensor(out=t1[:], in0=eT[:], in1=MiT, op=ALU.mult), reads=["eT", "cm"], writes=["t1"])
                P.op("dve", lambda e: e.tensor_tensor(out=qkT[:], in0=pB[0:64, :], in1=t1[:], op=ALU.mult), reads=["pB", "t1"], writes=["qkT"])
                P.op("dve", lambda e: e.tensor_tensor(out=eT[:], in0=eT[:], in1=MsT, op=ALU.mult), reads=["eT", "cm"], writes=["eT"])
                P.op("dve", lambda e: e.tensor_tensor(out=eT[:], in0=eT[:], in1=betab[:], op=ALU.mult), reads=["eT", "betab"], writes=["eT"])
                P.op("dve", lambda e: e.scalar_tensor_tensor(out=NmT[0][:], in0=pA[0:64, :], scalar=-1.0, in1=eT[:], op0=ALU.mult, op1=ALU.mult),
                     reads=["pA", "eT"], writes=["NmT0"])
                pk3 = pTk[0:64, :].rearrange("p (c d) -> p c d", d=128)
                P.op("dve", lambda e, h=h: e.tensor_tensor(out=kbg[:], in0=pk3, in1=cole[:, :, h:h + 1].to_broadcast([64, NCH, 128]), op=ALU.mult),
                     reads=["pC", ("cole", h)], writes=["kbg"])
                P.op("dve", lambda e, h=h: e.tensor_tensor(out=kdec[:], in0=pk3, in1=colt[:, :, 14 + h:15 + h].to_broadcast([64, NCH, 128]), op=ALU.mult),
                     reads=["pC", "colt"], writes=["kdec"])
                for c in range(NCH):
                    cs = slice(c * 64, (c + 1) * 64)
                    P.op("pe", lambda e, c=c, cs=cs: e.transpose(pTk[0:64, c * 128:(c + 1) * 128], vcb[:, cs], identb[:]), reads=["qkvb2", "identb"], writes=["pC"])
                P.op("dve", lambda e, h=h: e.tensor_tensor(out=vb[:], in0=pk3, in1=colt[:, :, h:h + 1].to_broadcast([64, NCH, 128]), op=ALU.mult),
                     reads=["pC", "colt"], writes=["vb"])
                if blk == 0 and h == HORDER[0]:
                    dump(Nm[0][:], "Nm0", 64); dump(NmT[0][:], "NmT0", 64); dump(vb[:, 0:4, :].rearrange("p c d -> p (c d)"), "vb", 64); dump(kbg[:, 0:4, :].rearrange("p c d -> p (c d)"), "kbg", 64)
                P.op("dve", lambda e: e.tensor_tensor(out=Pm[:], in0=NmT[0][:], in1=Id8, op=ALU.add), reads=["NmT0", "cm"], writes=["Pm"])
                cur = 0
                for lvl in range(1, 6):
                    nxt = 1 - cur
                    for c in range(NCH):
                        cs = slice(c * 64, (c + 1) * 64)
                        P.op("pe", lambda e, cs=cs, cur=cur: e.matmul(pA[0:64, cs], NmT[cur][:, cs], Nm[cur][:, cs], start=True, stop=True),
                             reads=[f"NmT{cur}", f"Nm{cur}"], writes=["pA"])
                    P.op("act", lambda e, nxt=nxt: e.activation(out=Nm[nxt][:], in_=pA[0:64, :], func=AF.Copy), reads=["pA"], writes=[f"Nm{nxt}"])
                    if lvl < 5:
                        for c in range(NCH):
                            cs = slice(c * 64, (c + 1) * 64)
                            P.op("pe", lambda e, cs=cs, cur=cur: e.matmul(pB[0:64, cs], Nm[cur][:, cs], NmT[cur][:, cs], start=True, stop=True),
                                 reads=[f"NmT{cur}", f"Nm{cur}"], writes=["pB"])
                        P.op("act", lambda e, nxt=nxt: e.activation(out=NmT[nxt][:], in_=pB[0:64, :], func=AF.Copy), reads=["pB"], writes=[f"NmT{nxt}"])
                    for c in range(NCH):
                        cs = slice(c * 64, (c + 1) * 64)
                        P.op("pe", lambda e, cs=cs, nxt=nxt: e.matmul(pC[0:64, cs], Nm[nxt][:, cs], Pm[:, cs], start=True, stop=True),
                             reads=[f"Nm{nxt}", "Pm"], writes=["pC"])
                    P.op("dve", lambda e: e.tensor_tensor(out=Pm[:], in0=Pm[:], in1=pC[0:64, :], op=ALU.add), reads=["pC", "Pm"], writes=["Pm"])
                    cur = nxt
                    if overlap:
                        pump(18)
                for half in range(2):
                    for c4 in range(4):
                        c = half * 4 + c4
                        cs = slice(c * 64, (c + 1) * 64)
                        pu = pA if half == 0 else pB
                        P.op("pe", lambda e, c=c, c4=c4, cs=cs, pu=pu: e.matmul(pu[0:64, c4 * 128:(c4 + 1) * 128], Pm[:, cs], vb[:, c, :], start=True, stop=True),
                             reads=["Pm", "vb"], writes=[("pA" if half == 0 else "pB")])
                    pu = pA if half == 0 else pB
                    P.op("act", lambda e, half=half, pu=pu: e.activation(out=u_t[:, half * 4:(half + 1) * 4, :].rearrange("p c d -> p (c d)"), in_=pu[0:64, :], func=AF.Copy),
                         reads=[("pA" if half == 0 else "pB")], writes=[("u", half)])
                for c in range(NCH):
                    cs = slice(c * 64, (c + 1) * 64)
                    P.op("pe", lambda e, c=c, cs=cs: e.matmul(pC[:, cs], kbg[:, c, :], Pm[:, cs], start=True, stop=True), reads=["Pm", "kbg"], writes=["pC"])
                P.op("act", lambda e: e.activation(out=wTb[:], in_=pC[:], func=AF.Copy), reads=["pC"], writes=["wTb"])
                if blk == 0 and h == HORDER[0]:
                    dump(Pm[:], "Pm", 64); dump(u_t[:, 0:4, :].rearrange("p c d -> p (c d)"), ("u", 0), 64)
                Sk, Sbk = f"S32_{h}", f"Sb_{h}"
                for c in range(NCH):
                    cs = slice(c * 64, (c + 1) * 64)
                    P.op("pe", lambda e, cs=cs, h=h: e.matmul(pR[0:64, 0:128], wTb[:, cs], Sb[h][:], start=True, stop=True), reads=["wTb", Sbk], writes=["pR"])
                    P.op("dve", lambda e, c=c: e.tensor_tensor(out=vnew[:], in0=u_t[:, c, :], in1=pR[0:64, 0:128], op=ALU.subtract),
                         reads=["pR", ("u", c // 4)], writes=["vnew"])
                    P.op("pe", lambda e, cs=cs, h=h: e.matmul(pO[:, cs], Sb[h][:], qdTb[:, cs], start=True, stop=False), reads=[Sbk, "qdTb"], writes=["pO"])
                    P.op("pe", lambda e, cs=cs: e.matmul(pO[:, cs], vnew[:], qkT[:, cs], start=False, stop=True), reads=["vnew", "qkT"], writes=["pO"])
                    P.op("pe", lambda e, c=c: e.matmul(pR[:, 128:256], kdec[:, c, :], vnew[:], start=True, stop=True), reads=["vnew", "kdec"], writes=["pR"])
                    P.op("dve", lambda e, h=h, c=c: e.scalar_tensor_tensor(out=S32[h][:], in0=S32[h][:], scalar=EGb[:, c * 64 + 63:c * 64 + 64], in1=pR[:, 128:256],
                                                                          op0=ALU.mult, op1=ALU.add), reads=["pR", Sk, "EGb"], writes=[Sk])
                    P.op("act", lambda e, h=h: e.activation(out=Sb[h][:], in_=S32[h][:], func=AF.Copy), reads=[Sk], writes=[Sbk])
                    if overlap:
                        pump(18)
                P.op("act", lambda e: e.activation(out=o32[:], in_=pO[:], func=AF.Copy), reads=["pO"], writes=["o32"])
                P.op("act", lambda e: e.activation(out=sq[:, 0, :], in_=pO[:], func=AF.Square), reads=["pO"], writes=["sq"])
                P.op("pe", lambda e: e.matmul(psn[:], ones[:], sq[:, 0, :], start=True, stop=True), reads=["ones", "sq"], writes=["psn"])
                P.op("act", lambda e: e.activation(out=t1_full(0), in_=psn[:], func=AF.Ln, bias=EPS, scale=1.0 / 128), reads=["psn"], writes=["l2r0"])
                P.op("act", lambda e: e.activation(out=t1_full(0), in_=t1_full(0), func=AF.Exp, scale=-0.5), reads=["l2r0"], writes=["l2r0"])
                if blk == 0 and h == HORDER[0]:
                    dump(o32[:], "o32")
                yk = ycnt[0] % 2
                ycnt[0] += 1
                P.op("dve", lambda e, yk=yk: e.scalar_tensor_tensor(out=yo[yk][:], in0=o32[:], scalar=dng[:, 0:1], in1=t1_full(0), op0=ALU.mult, op1=ALU.mult),
                     reads=["o32", "dng", "l2r0"], writes=[f"yo{yk}"])
                P.op("dve", lambda e, yk=yk, sg_=sgate[h]: e.tensor_tensor(out=yo[yk][:], in0=yo[yk][:], in1=sg_[:], op=ALU.mult),
                     reads=[f"yo{yk}", SGK[h]], writes=[f"yo{yk}"])
                store_y(2 + h, t0, f"yo{yk}", yo[yk][:])
        P.wait_all_dma("sp")
        P.emit()
    return nc


EPS = 1e-6
D = 2048
KC = 16


def build_B(NT, FF, mode="dense", final_norm=False, PASS=1024):
    nc = bass.Bass("TRN2", target_bir_lowering=False)
    TT = 512
    PASS = min(PASS, NT)
    npass = NT // PASS
    tpp = PASS // TT
    NF = FF // 128
    ymT = nc.dram_tensor("ymT", [KC, 128, NT], F32, kind="ExternalInput").ap()
    xT = nc.dram_tensor("xT", [KC, 128, NT], F32, kind="ExternalInput").ap()
    wout = nc.dram_tensor("wout", [D, D], F32, kind="ExternalInput").ap()
    lgain = nc.dram_tensor("lgain", [128, 8], F32, kind="ExternalInput").ap()
    ngain = nc.dram_tensor("ngain", [128, KC], F32, kind="ExternalInput").ap()
    wg = nc.dram_tensor("wg", [D, FF], F32, kind="ExternalInput").ap()
    wu = nc.dram_tensor("wu", [D, FF], F32, kind="ExternalInput").ap()
    wd = nc.dram_tensor("wd", [FF, D], F32, kind="ExternalInput").ap()
    x2T = nc.dram_tensor("x2T", [KC, 128, NT], F32, kind="ExternalOutput").ap()
    x1s = nc.dram_tensor("x1s", [KC, 128, NT], F32, kind="Internal").ap()

    ymv = ymT.rearrange("c p t -> p c t")
    xv = xT.rearrange("c p t -> p c t")
    x1v = x1s.rearrange("c p t -> p c t")
    woutv = wout.rearrange("(kc p) n -> p kc n", p=128)
    wgv = wg.rearrange("(kc p) n -> p kc n", p=128)
    wuv = wu.rearrange("(kc p) n -> p kc n", p=128)
    wdv = wd.rearrange("(f p) n -> p f n", p=128)

    with ExitStack() as st:
        P = Prog(nc, st)
        ones = P.sb("ones", [128, 128], BF16)
        lg = P.sb("lg", [128, 8], F32)
        ng = P.sb("ng", [128, KC], F32)
        AR = max(24576, NF * PASS // 2)
        arena = P.sb("arena", [128, AR], F32)
        ym32 = arena[:, 0:8192].rearrange("p (c t) -> p c t", c=KC)
        x32 = arena[:, 8192:16384].rearrange("p (c t) -> p c t", c=KC)
        ymb = arena[:, 16384:20480].bitcast(BF16).rearrange("p (c t) -> p c t", c=KC)
        sq = arena[:, 20480:24576].bitcast(BF16).rearrange("p (c t) -> p c t", c=KC)
        actT = arena[:, 0:NF * PASS // 2].bitcast(BF16).rearrange("p (f t) -> p f t", f=NF)
        h2T = P.sb("h2T", [128, KC, PASS], BF16)
        rs = P.sb("rs", [128, TT], F32)
        wo = [P.sb(f"wo{i}", [128, KC, 128], BF16) for i in range(2)]
        wbuf = [P.sb(f"wbuf{i}", [128, 8192], BF16) for i in range(2)]
        wgt = [w[:, 0:4096].rearrange("p (c n) -> p c n", c=KC) for w in wbuf]
        wut = [w[:, 4096:8192].rearrange("p (c n) -> p c n", c=KC) for w in wbuf]
        wdt = [w[:, 0:NF * 128].rearrange("p (f n) -> p f n", f=NF) for w in wbuf]
        dummy = P.sb("dmy", [128, 8], F32)
        sg = [P.sb(f"sg{i}", [128, TT], F32) for i in range(2)]
        xr = [P.sb(f"xr{i}", [128, TT], F32) for i in range(2)]
        xo = [P.sb(f"xo{i}", [128, TT], F32) for i in range(2)]
        ps = [P.ps(f"ps{i}", [128, 512]) for i in range(8)]

        P.op("dve", lambda e: e.memset(ones[:], 1.0), writes=["ones"])
        P.dma("sp", lg[:], lgain, writes=["lg"])
        P.dma("sp", ng[:], ngain, writes=["ng"])

        cnt = {"wo": 0, "w": 0, "wd": 0, "po": 0, "g": 0, "x": 0}
        for ps_i in range(npass):
            for tq in range(tpp):
                t0 = ps_i * PASS + tq * TT
                P.dma("sp", ym32, ymv[:, :, t0:t0 + TT], writes=["ym32"])
                P.dma("sp", x32, xv[:, :, t0:t0 + TT], writes=["x32"] + [("x1", j) for j in range(KC)])
                P.op("act", lambda e: e.activation(out=sq[:, 0:8, :], in_=ym32[:, 0:8, :], func=AF.Square),
                     reads=["ym32"], writes=["sq"])
                for j in range(8):
                    P.op("pe", lambda e, j=j: e.matmul(ps[0][:], ones[:], sq[:, j, :], start=(j == 0), stop=(j == 7)),
                         reads=["ones", "sq"], writes=["ps0"])
                P.op("act", lambda e: e.activation(out=rs[:], in_=ps[0][:], func=AF.Sqrt, bias=EPS, scale=1.0 / 1024),
                     reads=["ps0"], writes=["rs"])
                P.op("dve", lambda e: e.reciprocal(out=rs[:], in_=rs[:]), reads=["rs"], writes=["rs"])
                for j in range(8):
                    P.op("dve", lambda e, j=j: e.scalar_tensor_tensor(out=ymb[:, j, :], in0=ym32[:, j, :], scalar=lg[:, j:j + 1],
                                                                        in1=rs[:], op0=ALU.mult, op1=ALU.mult),
                         reads=["ym32", "lg", "rs"], writes=[("ymb", j)])
                P.op("pool", lambda e: e.tensor_copy(out=ymb[:, 8:16, :], in_=ym32[:, 8:16, :]),
                     reads=["ym32"], writes=[("ymb", j) for j in range(8, 16)])
                for dt in range(KC):
                    b = cnt["wo"] % 2
                    cnt["wo"] += 1
                    P.dma("pool", wo[b][:], woutv[:, :, dt * 128:dt * 128 + 128], writes=[f"wo{b}"])
                    pb = 1 + cnt["po"] % 2
                    cnt["po"] += 1
                    for kc in range(KC):
                        P.op("pe", lambda e, kc=kc, b=b, pb=pb: e.matmul(
                            ps[pb][:], wo[b][:, kc, :], ymb[:, kc, :], start=(kc == 0), stop=(kc == KC - 1)),
                            reads=[f"wo{b}", ("ymb", kc)], writes=[f"ps{pb}"])
                    P.op("dve", lambda e, dt=dt, pb=pb: e.tensor_tensor(out=x32[:, dt, :], in0=x32[:, dt, :], in1=ps[pb][:], op=ALU.add),
                         reads=[f"ps{pb}", "x32"], writes=[("x1", dt)])
                x1keys = [("x1", dt) for dt in range(KC)]
                P.dma("sp", x1v[:, :, t0:t0 + TT], x32, reads=x1keys, writes=["x1s"], key="x1st")
                P.op("act", lambda e: e.activation(out=sq[:], in_=x32[:], func=AF.Square),
                     reads=x1keys, writes=["sq"])
                for j in range(KC):
                    P.op("pe", lambda e, j=j: e.matmul(ps[0][:], ones[:], sq[:, j, :], start=(j == 0), stop=(j == KC - 1)),
                         reads=["ones", "sq"], writes=["ps0"])
                P.op("act", lambda e: e.activation(out=rs[:], in_=ps[0][:], func=AF.Sqrt, bias=EPS, scale=1.0 / D),
                     reads=["ps0"], writes=["rs"])
                P.op("dve", lambda e: e.reciprocal(out=rs[:], in_=rs[:]), reads=["rs"], writes=["rs"])
                for j in range(KC):
                    P.op("dve", lambda e, j=j, tq=tq: e.scalar_tensor_tensor(
                        out=h2T[:, j, tq * TT:(tq + 1) * TT], in0=x32[:, j, :], scalar=ng[:, j:j + 1],
                        in1=rs[:], op0=ALU.mult, op1=ALU.mult),
                        reads=[("x1", j), "ng", "rs"], writes=[("h2T", tq)])
            b1keys = ["ym32", "x32", "sq"] + [("ymb", j) for j in range(KC)] + [("x1", j) for j in range(KC)]
            P.op("pool", lambda e: e.memset(dummy[:], 1.0), writes=b1keys + ["actT"])
            for fc in range(FF // 256):
                b = cnt["w"] % 2
                cnt["w"] += 1
                P.dma("pool", wgt[b], wgv[:, :, fc * 256:(fc + 1) * 256], writes=[f"wbuf{b}"], key=f"wg{b}")
                P.dma("pool", wut[b], wuv[:, :, fc * 256:(fc + 1) * 256], writes=[f"wbufu{b}"], key=f"wu{b}")
                for half in range(2):
                    f = fc * 2 + half
                    for tq in range(tpp):
                        g = cnt["g"] % 2
                        cnt["g"] += 1
                        pg, pu = 3 + g, 5 + g
                        for kc in range(KC):
                            P.op("pe", lambda e, kc=kc, b=b, pg=pg, half=half, tq=tq: e.matmul(
                                ps[pg][:], wgt[b][:, kc, half * 128:(half + 1) * 128], h2T[:, kc, tq * TT:(tq + 1) * TT],
                                start=(kc == 0), stop=(kc == KC - 1)),
                                reads=[f"wbuf{b}", ("h2T", tq)], writes=[f"ps{pg}"])
                        for kc in range(KC):
                            P.op("pe", lambda e, kc=kc, b=b, pu=pu, half=half, tq=tq: e.matmul(
                                ps[pu][:], wut[b][:, kc, half * 128:(half + 1) * 128], h2T[:, kc, tq * TT:(tq + 1) * TT],
                                start=(kc == 0), stop=(kc == KC - 1)),
                                reads=[f"wbufu{b}", ("h2T", tq)], writes=[f"ps{pu}"])
                        P.op("act", lambda e, g=g, pg=pg: e.activation(out=sg[g][:], in_=ps[pg][:], func=AF.Silu),
                             reads=[f"ps{pg}"], writes=[f"sg{g}"])
                        P.op("dve", lambda e, g=g, pu=pu, f=f, tq=tq: e.tensor_tensor(
                            out=actT[:, f, tq * TT:(tq + 1) * TT], in0=sg[g][:], in1=ps[pu][:], op=ALU.mult),
                            reads=[f"sg{g}", f"ps{pu}", "actT"], writes=[("act", f, tq)])
            for dt in range(KC):
                b = cnt["wd"] % 2
                cnt["wd"] += 1
                P.dma("pool", wdt[b], wdv[:, :, dt * 128:(dt + 1) * 128], writes=[f"wbuf{b}", f"wbufu{b}"], key=f"wg{b}")
                for tq in range(tpp):
                    t0 = ps_i * PASS + tq * TT
                    pb = 1 + cnt["po"] % 2
                    cnt["po"] += 1
                    xb = cnt["x"] % 2
                    cnt["x"] += 1
                    P.dma("sp", xr[xb][:], x1v[:, dt, t0:t0 + TT], reads=["x1s"], writes=[f"xr{xb}"])
                    for f in range(NF):
                        P.op("pe", lambda e, f=f, b=b, pb=pb, tq=tq: e.matmul(
                            ps[pb][:], wdt[b][:, f, :], actT[:, f, tq * TT:(tq + 1) * TT],
                            start=(f == 0), stop=(f == NF - 1)),
                            reads=[f"wbuf{b}", ("act", f, tq)], writes=[f"ps{pb}"])
                    P.op("dve", lambda e, xb=xb, pb=pb: e.tensor_tensor(out=xo[xb][:], in0=xr[xb][:], in1=ps[pb][:], op=ALU.add),
                         reads=[f"xr{xb}", f"ps{pb}"], writes=[f"xo{xb}"])
                    P.dma("sp", x2T[dt, :, t0:t0 + TT], xo[xb][:], reads=[f"xo{xb}"], key=f"xo{xb}")
            allact = [("act", f, tq) for f in range(NF) for tq in range(tpp)]
            P.op("pool", lambda e: e.memset(dummy[:], 1.0), writes=allact + b1keys + ["actT"])
        P.wait_all_dma("sp")
        P.emit()
    return nc


EPS = 1e-6
D = 2048
KC = 16
NE = 8


def consts_M(C):
    t = np.arange(128)
    U = (t[:, None] < t[None, :]).astype(np.float32)
    ebase = np.tile((np.arange(NE) * C).astype(np.float32)[None, :], (128, 1))
    return {"Umat": U, "identm": np.eye(128, dtype=np.float32), "ebase": ebase}


def build_M(NT, FE, C):
    nc = bass.Bass("TRN2", target_bir_lowering=False)
    TT = 512
    ntile = NT // TT
    NS = NT // 128
    NF = FE // 128
    NSB = C // 128
    CH = C // 2
    dr = lambda n, s, k="ExternalInput", dt=F32: nc.dram_tensor(n, list(s), dt, kind=k).ap()
    ymT = dr("ymT", [KC, 128, NT])
    xT = dr("xT", [KC, 128, NT])
    wout = dr("wout", [D, D])
    lgain = dr("lgain", [128, 8])
    ngain = dr("ngain", [128, KC])
    ngrow = dr("ngrow", [1, D])
    fgrow = dr("fgrow", [1, D])
    router = dr("router", [D, NE])
    wg = dr("wg", [NE, D, FE])
    wu = dr("wu", [NE, D, FE])
    wd = dr("wd", [NE, FE, D])
    Umat = dr("Umat", [128, 128])
    identm = dr("identm", [128, 128])
    ebased = dr("ebase", [128, NE])
    out = dr("out", [NT, D], "ExternalOutput")
    cnt_out = dr("cnt_out", [128, NE], "ExternalOutput")
    x1tok_s = dr("x1tok_s", [NT, D], "Internal")
    Xe = dr("Xe", [NE * C, D], "Internal", BF16)
    Yd = dr("Yd", [NE * C, D], "Internal")

    ymv = ymT.rearrange("c p t -> p c t")
    xv = xT.rearrange("c p t -> p c t")
    woutv = wout.rearrange("(kc p) n -> p kc n", p=128)

    with ExitStack() as st:
        P = Prog(nc, st)
        sb, ps_ = P.sb, P.ps
        ones = sb("ones", [128, 128], BF16)
        ones32 = sb("ones32", [128, 128], F32)
        U = sb("U", [128, 128], F32)
        ident = sb("ident_sb", [128, 128], F32)
        identb = sb("identb", [128, 128], BF16)
        ebase = sb("ebase_sb", [128, NE], F32)
        lg = sb("lg", [128, 8], F32)
        ng = sb("ng", [128, KC], F32)
        ngb = sb("ngb", [128, D], F32)
        fgb = sb("fgb", [128, D], F32)
        rt = sb("rt", [128, KC, NE], F32)
        gr = sb("gr", [128, KC, NE], F32)
        arena = sb("arena", [128, 24576], F32)
        ym32 = arena[:, 0:8192].rearrange("p (c t) -> p c t", c=KC)
        x32 = arena[:, 8192:16384].rearrange("p (c t) -> p c t", c=KC)
        ymb = arena[:, 16384:20480].bitcast(BF16).rearrange("p (c t) -> p c t", c=KC)
        sq = arena[:, 20480:24576].bitcast(BF16).rearrange("p (c t) -> p c t", c=KC)
        o1 = 8 * C
        XeT = arena[:, 0:o1].bitcast(BF16).rearrange("p (c t) -> p c t", c=KC)
        xrow = [arena[:, o1 + i * 1024: o1 + (i + 1) * 1024].bitcast(BF16) for i in range(2)]
        o2 = o1 + 2048
        o3 = o2 + NF * C // 2
        actT = arena[:, o2:o3].bitcast(BF16).rearrange("p (f t) -> p f t", f=NF)
        ystg = [arena[:, o3 + i * 512: o3 + (i + 1) * 512] for i in range(3)]
        assert o3 + 1536 <= 24576
        cy1 = [arena[:, i * 8192: i * 8192 + 2048] for i in range(2)]
        cy2 = [arena[:, i * 8192 + 2048: i * 8192 + 4096] for i in range(2)]
        cx1 = [arena[:, i * 8192 + 4096: i * 8192 + 6144] for i in range(2)]
        cjunk = arena[:, 16384:18432]
        rs = sb("rs", [128, TT], F32)
        wo = [sb(f"wo{i}", [128, KC, 128], BF16) for i in range(2)]
        wbuf = [sb(f"wbuf{i}", [128, 8192], BF16) for i in range(2)]
        wgt = [w[:, 0:4096].rearrange("p (c n) -> p c n", c=KC) for w in wbuf]
        wut = [w[:, 4096:8192].rearrange("p (c n) -> p c n", c=KC) for w in wbuf]
        wdt = [w[:, 0:NF * 256].rearrange("p (f n) -> p f n", f=NF) for w in wbuf]
        dmy = sb("dmy", [128, 8], F32)
        x1tok = sb("x1tok", [128, D], F32)
        h2tok = sb("h2tok", [128, D], BF16)
        sg = [sb(f"sg{i}", [128, CH], F32) for i in range(2)]
        cnt = sb("cnt", [128, NE], F32)
        lgt = sb("lgt", [128, NE], F32)
        lg2 = sb("lg2", [128, NE], F32)
        mk1 = sb("mk1", [128, NE], F32)
        mk2 = sb("mk2", [128, NE], F32)
        mk = sb("mk", [128, NE], F32)
        slot = sb("slot", [128, NE], F32)
        tmp8 = sb("tmp8", [128, NE], F32)
        m12 = sb("m12", [128, 4], F32)
        ssum = sb("ssum", [128, 2], F32)
        rstd = sb("rstd", [128, 1], F32)
        dstf = sb("dstf", [128, 2], F32)
        dst = sb("dst", [128, NS, 2], I32)
        wts = sb("wts", [128, NS, 2], F32)
        ps = [ps_(f"ps{i}", [128, 512]) for i in range(8)]

        P.op("dve", lambda e: e.memset(ones[:], 1.0), writes=["ones"])
        P.op("dve", lambda e: e.memset(ones32[:], 1.0), writes=["ones32"])
        P.op("dve", lambda e: e.memset(cnt[:], 0.0), writes=["cnt"])
        P.dma("sp", lg[:], lgain, writes=["lg"])
        P.dma("sp", ng[:], ngain, writes=["ng"])
        P.dma("sp", U[:], Umat, writes=["U"])
        P.dma("sp", ident[:], identm, writes=["ident"])
        P.dma("sp", ebase[:], ebased, writes=["ebase"])
        P.dma("sp", ngb[:], ngrow.partition_broadcast(128), writes=["ngb"])
        P.dma("sp", fgb[:], fgrow.partition_broadcast(128), writes=["fgb"])
        P.dma("sp", rt[:], router.rearrange("(kc p) e -> p kc e", p=128), writes=["rt"])
        P.op("dve", lambda e: e.tensor_copy(out=identb[:], in_=ident[:]), reads=["ident"], writes=["identb"])
        for kc in range(KC):
            P.op("dve", lambda e, kc=kc: e.tensor_scalar(out=gr[:, kc, :], in0=rt[:, kc, :], scalar1=ng[:, kc:kc + 1], scalar2=None, op0=ALU.mult),
                 reads=["rt", "ng"], writes=["gr"])

        cntr = {"wo": 0, "po": 0}
        b1keys = ["ym32", "x32", "sq"] + [("ymb", j) for j in range(KC)] + [("x1", j) for j in range(KC)]
        for tq in range(ntile):
            t0 = tq * TT
            P.dma("sp", ym32, ymv[:, :, t0:t0 + TT], writes=["ym32"])
            P.dma("sp", x32, xv[:, :, t0:t0 + TT], writes=["x32"] + [("x1", j) for j in range(KC)])
            P.op("act", lambda e: e.activation(out=sq[:, 0:8, :], in_=ym32[:, 0:8, :], func=AF.Square), reads=["ym32"], writes=["sq"])
            for j in range(8):
                P.op("pe", lambda e, j=j: e.matmul(ps[0][:], ones[:], sq[:, j, :], start=(j == 0), stop=(j == 7)), reads=["ones", "sq"], writes=["ps0"])
            P.op("act", lambda e: e.activation(out=rs[:], in_=ps[0][:], func=AF.Sqrt, bias=EPS, scale=1.0 / 1024), reads=["ps0"], writes=["rs"])
            P.op("dve", lambda e: e.reciprocal(out=rs[:], in_=rs[:]), reads=["rs"], writes=["rs"])
            for j in range(8):
                P.op("dve", lambda e, j=j: e.scalar_tensor_tensor(out=ymb[:, j, :], in0=ym32[:, j, :], scalar=lg[:, j:j + 1], in1=rs[:], op0=ALU.mult, op1=ALU.mult),
                     reads=["ym32", "lg", "rs"], writes=[("ymb", j)])
            P.op("pool", lambda e: e.tensor_copy(out=ymb[:, 8:16, :], in_=ym32[:, 8:16, :]), reads=["ym32"], writes=[("ymb", j) for j in range(8, 16)])
            for dt in range(KC):
                b = cntr["wo"] % 2
                cntr["wo"] += 1
                P.dma("pool", wo[b][:], woutv[:, :, dt * 128:dt * 128 + 128], writes=[f"wo{b}"])
                pb = 1 + cntr["po"] % 2
                cntr["po"] += 1
                for kc in range(KC):
                    P.op("pe", lambda e, kc=kc, b=b, pb=pb: e.matmul(ps[pb][:], wo[b][:, kc, :], ymb[:, kc, :], start=(kc == 0), stop=(kc == KC - 1)),
                         reads=[f"wo{b}", ("ymb", kc)], writes=[f"ps{pb}"])
                P.op("dve", lambda e, dt=dt, pb=pb: e.tensor_tensor(out=x32[:, dt, :], in0=x32[:, dt, :], in1=ps[pb][:], op=ALU.add),
                     reads=[f"ps{pb}", "x32"], writes=[("x1", dt)])
            x1keys = [("x1", dt) for dt in range(KC)]
            for s4 in range(4):
                sidx = tq * 4 + s4
                ts = slice(s4 * 128, (s4 + 1) * 128)
                for kc in range(KC):
                    P.op("pe", lambda e, kc=kc, ts=ts: e.matmul(ps[3][:, 0:NE], x32[:, kc, ts], gr[:, kc, :], start=(kc == 0), stop=(kc == KC - 1)),
                         reads=[("x1", kc), "gr"], writes=["ps3"])
                for q in range(4):
                    pb = 4 + q % 2
                    for d4 in range(4):
                        dc = q * 4 + d4
                        P.op("pe", lambda e, dc=dc, d4=d4, ts=ts, pb=pb: e.transpose(ps[pb][:, d4 * 128:(d4 + 1) * 128], x32[:, dc, ts], ident[:]),
                             reads=[("x1", dc), "ident"], writes=[f"ps{pb}"])
                    P.op("act", lambda e, q=q, pb=pb: e.activation(out=x1tok[:, q * 512:(q + 1) * 512], in_=ps[pb][:], func=AF.Copy),
                         reads=[f"ps{pb}"], writes=[("x1tok", q)])
                xtk = [("x1tok", q) for q in range(4)]
                P.dma("sp", x1tok_s[tq * TT + s4 * 128: tq * TT + (s4 + 1) * 128, :], x1tok[:], reads=xtk, writes=["x1tok_s"], key="x1tst")
                P.op("act", lambda e: e.activation(out=h2tok[:], in_=x1tok[:], func=AF.Square, accum_out=ssum[:, 0:1]), reads=xtk, writes=["h2tok", "ssum"])
                P.op("act", lambda e: e.activation(out=rstd[:], in_=ssum[:, 0:1], func=AF.Sqrt, bias=EPS, scale=1.0 / D), reads=["ssum"], writes=["rstd"])
                P.op("dve", lambda e: e.reciprocal(out=rstd[:], in_=rstd[:]), reads=["rstd"], writes=["rstd"])
                P.op("dve", lambda e: e.scalar_tensor_tensor(out=h2tok[:], in0=x1tok[:], scalar=rstd[:, 0:1], in1=ngb[:], op0=ALU.mult, op1=ALU.mult),
                     reads=xtk + ["rstd", "ngb"], writes=["h2tok"])
                P.op("dve", lambda e: e.tensor_scalar(out=lgt[:], in0=ps[3][:, 0:NE], scalar1=rstd[:, 0:1], scalar2=None, op0=ALU.mult), reads=["ps3", "rstd"], writes=["lgt"])
                P.op("dve", lambda e: e.tensor_reduce(out=m12[:, 0:1], in_=lgt[:], axis=AX.X, op=ALU.max), reads=["lgt"], writes=["m1"])
                P.op("dve", lambda e: e.tensor_scalar(out=mk1[:], in0=lgt[:], scalar1=m12[:, 0:1], scalar2=None, op0=ALU.is_equal), reads=["lgt", "m1"], writes=["mk1"])
                P.op("dve", lambda e: e.scalar_tensor_tensor(out=lg2[:], in0=mk1[:], scalar=-1e30, in1=lgt[:], op0=ALU.mult, op1=ALU.add), reads=["mk1", "lgt"], writes=["lg2"])
                P.op("dve", lambda e: e.tensor_reduce(out=m12[:, 1:2], in_=lg2[:], axis=AX.X, op=ALU.max), reads=["lg2"], writes=["m2"])
                P.op("dve", lambda e: e.tensor_scalar(out=mk2[:], in0=lg2[:], scalar1=m12[:, 1:2], scalar2=None, op0=ALU.is_equal), reads=["lg2", "m2"], writes=["mk2"])
                P.op("dve", lambda e: e.tensor_tensor(out=m12[:, 2:3], in0=m12[:, 0:1], in1=m12[:, 1:2], op=ALU.subtract), reads=["m1", "m2"], writes=["md"])
                P.op("act", lambda e, sidx=sidx: e.activation(out=wts[:, sidx, 0:1], in_=m12[:, 2:3], func=AF.Sigmoid), reads=["md"], writes=[("wts", sidx)])
                P.op("dve", lambda e, sidx=sidx: e.tensor_scalar(out=wts[:, sidx, 1:2], in0=wts[:, sidx, 0:1], scalar1=-1.0, scalar2=1.0, op0=ALU.mult, op1=ALU.add),
                     reads=[("wts", sidx)], writes=[("wts2", sidx)])
                P.op("dve", lambda e: e.tensor_tensor(out=mk[:], in0=mk1[:], in1=mk2[:], op=ALU.add), reads=["mk1", "mk2"], writes=["mk"])
                P.op("pe", lambda e: e.matmul(ps[6][:, 0:NE], U[:], mk[:], start=True, stop=True), reads=["U", "mk"], writes=["ps6"])
                P.op("pe", lambda e: e.matmul(ps[6][:, NE:2 * NE], ones32[:], mk[:], start=True, stop=True), reads=["ones32", "mk"], writes=["ps6"])
                P.op("dve", lambda e: e.tensor_tensor(out=slot[:], in0=ps[6][:, 0:NE], in1=cnt[:], op=ALU.add), reads=["ps6", "cnt"], writes=["slot"])
                P.op("dve", lambda e: e.tensor_tensor(out=slot[:], in0=slot[:], in1=ebase[:], op=ALU.add), reads=["slot", "ebase"], writes=["slot"])
                P.op("dve", lambda e: e.tensor_tensor(out=cnt[:], in0=ps[6][:, NE:2 * NE], in1=cnt[:], op=ALU.add), reads=["ps6", "cnt", "slot"], writes=["cnt"])
                for k2, mkk in enumerate((mk1, mk2)):
                    P.op("dve", lambda e, mkk=mkk: e.tensor_tensor(out=tmp8[:], in0=mkk[:], in1=slot[:], op=ALU.mult), reads=["slot", "mk1", "mk2"], writes=["tmp8"])
                    P.op("dve", lambda e, k2=k2: e.tensor_reduce(out=dstf[:, k2:k2 + 1], in_=tmp8[:], axis=AX.X, op=ALU.add), reads=["tmp8"], writes=[("dstf", k2)])
                P.op("dve", lambda e, sidx=sidx: e.tensor_copy(out=dst[:, sidx, :], in_=dstf[:]), reads=[("dstf", 0), ("dstf", 1)], writes=[("dst", sidx)])
                for k2 in range(2):
                    P.idma(Xe, dst[:, sidx, k2:k2 + 1], h2tok[:], None, reads=["h2tok", ("dst", sidx)], writes=["Xe"], key=("xsc", k2), bounds=NE * C - 1)
        P.dma("sp", cnt_out, cnt[:], reads=["cnt"], key="cntout")
        P.op("pool", lambda e: e.memset(dmy[:], 1.0), writes=b1keys + ["earena"])
        ec = {"xr": 0, "w": 0, "g": 0, "y": 0}
        for ex in range(NE):
            for sbk in range(NSB):
                xb = ec["xr"] % 2
                ec["xr"] += 1
                P.dma("sp", xrow[xb], Xe[ex * C + sbk * 128: ex * C + (sbk + 1) * 128, :], reads=["Xe", "earena"], writes=[f"xrow{xb}"])
                for half in range(2):
                    pbT = ps[1 + half][:].bitcast(BF16)
                    for d8 in range(8):
                        dc = half * 8 + d8
                        P.op("pe", lambda e, xb=xb, dc=dc, d8=d8, pbT=pbT: e.transpose(pbT[:, d8 * 128:(d8 + 1) * 128], xrow[xb][:, dc * 128:(dc + 1) * 128], identb[:]),
                             reads=[f"xrow{xb}", "identb"], writes=[f"ps{1 + half}"])
                    P.op("act" if half == 0 else "dve",
                         (lambda e, half=half, sbk=sbk, pbT=pbT: e.activation(out=XeT[:, half * 8:(half + 1) * 8, sbk * 128:(sbk + 1) * 128],
                                                                             in_=pbT.rearrange("p (c t) -> p c t", c=8), func=AF.Copy)) if half == 0 else
                         (lambda e, half=half, sbk=sbk, pbT=pbT: e.tensor_copy(out=XeT[:, half * 8:(half + 1) * 8, sbk * 128:(sbk + 1) * 128],
                                                                              in_=pbT.rearrange("p (c t) -> p c t", c=8))),
                         reads=[f"ps{1 + half}", "earena"], writes=[("XeT", sbk, half)])
            xek = [("XeT", sbk, half) for sbk in range(NSB) for half in range(2)]
            for fc in range(FE // 256):
                b = ec["w"] % 2
                ec["w"] += 1
                P.dma("pool", wgt[b], wg[ex].rearrange("(kc p) n -> p kc n", p=128)[:, :, fc * 256:(fc + 1) * 256], writes=[f"wbuf{b}"], key=f"wg{b}")
                P.dma("pool", wut[b], wu[ex].rearrange("(kc p) n -> p kc n", p=128)[:, :, fc * 256:(fc + 1) * 256], writes=[f"wbufu{b}"], key=f"wu{b}")
                for half in range(2):
                    f = fc * 2 + half
                    for ch in range(2):
                        g = ec["g"] % 2
                        ec["g"] += 1
                        pg, pu = 3 + g, 5 + g
                        cs = slice(ch * CH, (ch + 1) * CH)
                        for kc in range(KC):
                            P.op("pe", lambda e, kc=kc, b=b, pg=pg, half=half, cs=cs: e.matmul(ps[pg][:, 0:CH], wgt[b][:, kc, half * 128:(half + 1) * 128], XeT[:, kc, cs],
                                                                                               start=(kc == 0), stop=(kc == KC - 1)),
                                 reads=[f"wbuf{b}"] + xek, writes=[f"ps{pg}"])
                        for kc in range(KC):
                            P.op("pe", lambda e, kc=kc, b=b, pu=pu, half=half, cs=cs: e.matmul(ps[pu][:, 0:CH], wut[b][:, kc, half * 128:(half + 1) * 128], XeT[:, kc, cs],
                                                                                               start=(kc == 0), stop=(kc == KC - 1)),
                                 reads=[f"wbufu{b}"] + xek, writes=[f"ps{pu}"])
                        P.op("act", lambda e, g=g, pg=pg: e.activation(out=sg[g][:], in_=ps[pg][:, 0:CH], func=AF.Silu), reads=[f"ps{pg}"], writes=[f"sg{g}"])
                        P.op("dve", lambda e, g=g, pu=pu, f=f, cs=cs: e.tensor_tensor(out=actT[:, f, cs], in0=sg[g][:], in1=ps[pu][:, 0:CH], op=ALU.mult),
                             reads=[f"sg{g}", f"ps{pu}", "earena"], writes=[("act", f)])
            actk = [("act", f) for f in range(NF)]
            for dc in range(D // 256):
                b = ec["w"] % 2
                ec["w"] += 1
                P.dma("pool", wdt[b], wd[ex].rearrange("(f p) n -> p f n", p=128)[:, :, dc * 256:(dc + 1) * 256], writes=[f"wbuf{b}", f"wbufu{b}"], key=f"wg{b}")
                for sbk in range(NSB):
                    pb = 1 + cntr["po"] % 2
                    cntr["po"] += 1
                    for f in range(NF):
                        P.op("pe", lambda e, f=f, b=b, pb=pb, sbk=sbk: e.matmul(ps[pb][:, 0:256], actT[:, f, sbk * 128:(sbk + 1) * 128], wdt[b][:, f, :],
                                                                               start=(f == 0), stop=(f == NF - 1)),
                             reads=[f"wbuf{b}"] + actk, writes=[f"ps{pb}"])
                    yb = ec["y"] % 3
                    ec["y"] += 1
                    P.op("act", lambda e, yb=yb, pb=pb: e.activation(out=ystg[yb][:, 0:256], in_=ps[pb][:, 0:256], func=AF.Copy),
                         reads=[f"ps{pb}", "earena"], writes=[f"ystg{yb}"])
                    P.dma("sp", Yd[ex * C + sbk * 128: ex * C + (sbk + 1) * 128, dc * 256:(dc + 1) * 256], ystg[yb][:, 0:256], reads=[f"ystg{yb}"], writes=[("Yd", ex)], key=f"yst{yb}")
        retire = [("XeT", sbk, half) for sbk in range(NSB) for half in range(2)] + [("act", f) for f in range(NF)] + \
                 [f"ystg{i}" for i in range(3)] + ["xrow0", "xrow1", "earena"]
        P.op("pool", lambda e: e.memset(dmy[:], 1.0), writes=retire + ["carena"])
        for sidx in range(NS):
            cb_ = sidx % 2
            P.idma(cy1[cb_], None, Yd, dst[:, sidx, 0:1], reads=[("Yd", ex_) for ex_ in range(NE)] + [("dst", sidx), "carena"], writes=[f"cy1_{cb_}"], key=("g1", cb_), bounds=NE * C - 1)
            P.idma(cy2[cb_], None, Yd, dst[:, sidx, 1:2], reads=[("Yd", ex_) for ex_ in range(NE)] + [("dst", sidx), "carena"], writes=[f"cy2_{cb_}"], key=("g2", cb_), bounds=NE * C - 1)
            P.dma("sp", cx1[cb_], x1tok_s[sidx * 128:(sidx + 1) * 128, :], reads=["x1tok_s", "carena"], writes=[f"cx1_{cb_}"])
            P.op("dve", lambda e, cb_=cb_, sidx=sidx: e.scalar_tensor_tensor(out=cx1[cb_], in0=cy1[cb_], scalar=wts[:, sidx, 0:1], in1=cx1[cb_], op0=ALU.mult, op1=ALU.add),
                 reads=[f"cy1_{cb_}", f"cx1_{cb_}", ("wts", sidx)], writes=[f"cx1_{cb_}"])
            P.op("dve", lambda e, cb_=cb_, sidx=sidx: e.scalar_tensor_tensor(out=cx1[cb_], in0=cy2[cb_], scalar=wts[:, sidx, 1:2], in1=cx1[cb_], op0=ALU.mult, op1=ALU.add),
                 reads=[f"cy2_{cb_}", f"cx1_{cb_}", ("wts2", sidx)], writes=[f"cx1_{cb_}"])
            P.op("act", lambda e, cb_=cb_: e.activation(out=cy1[cb_], in_=cx1[cb_], func=AF.Square, accum_out=ssum[:, 1:2]), reads=[f"cx1_{cb_}"], writes=[f"cy1_{cb_}", "ssum2"])
            P.op("act", lambda e: e.activation(out=rstd[:], in_=ssum[:, 1:2], func=AF.Sqrt, bias=EPS, scale=1.0 / D), reads=["ssum2"], writes=["rstd"])
            P.op("dve", lambda e: e.reciprocal(out=rstd[:], in_=rstd[:]), reads=["rstd"], writes=["rstd"])
            P.op("dve", lambda e, cb_=cb_: e.scalar_tensor_tensor(out=cy2[cb_], in0=cx1[cb_], scalar=rstd[:, 0:1], in1=fgb[:], op0=ALU.mult, op1=ALU.mult),
                 reads=[f"cx1_{cb_}", "rstd", "fgb"], writes=[f"cy2_{cb_}"])
            P.dma("sp", out[sidx * 128:(sidx + 1) * 128, :], cy2[cb_], reads=[f"cy2_{cb_}"], key=("ost", cb_))
        P.wait_all_dma("sp")
        P.emit()
    return nc


def fm(a):
    T, C = a.shape
    return np.ascontiguousarray(a.T.reshape(C // 128, 128, T))

def pcols(v):
    return np.ascontiguousarray(v.reshape(-1, 128).T)

def prep_A(inp, l, j):
    DL = 1024
    w_in = inp["w_in"][l]
    blk = [2 * j, 2 * j + 1]
    cols = []
    for n in blk: cols.append(np.arange(n * 128, (n + 1) * 128))
    for n in blk: cols.append(DL + np.arange(n * 128, (n + 1) * 128))
    for part in range(3):
        for h in blk: cols.append(2 * DL + part * 1024 + np.arange(h * 128, (h + 1) * 128))
    for h in blk: cols.append(2 * DL + 3 * 1024 + np.arange(h * 128, (h + 1) * 128))
    base = 2 * DL + 4 * 1024
    cols.append(np.array([base + blk[0], base + blk[1], base + 8 + blk[0], base + 8 + blk[1]]))
    cols = np.concatenate(cols)
    wc = np.ascontiguousarray(w_in[:, cols])
    lru_p = np.zeros((128, 2, 8), np.float32)
    lru_w = np.zeros((128, 2, 2, 128), np.float32)
    for i, n in enumerate(blk):
        sl = slice(n * 128, (n + 1) * 128)
        lru_p[:, i, 0:4] = inp["conv_lru_w"][l][:, sl].T
        lru_p[:, i, 4] = inp["conv_lru_b"][l][sl]
        lru_p[:, i, 5] = inp["lru_b_r"][l][n]
        lru_p[:, i, 6] = inp["lru_b_i"][l][n]
        lru_p[:, i, 7] = inp["lru_lambda"][l][sl]
        lru_w[:, i, 0, :] = inp["lru_w_r"][l][n]
        lru_w[:, i, 1, :] = inp["lru_w_i"][l][n]
    dn_cw = np.zeros((128, 6, 4), np.float32)
    cq = inp["conv_qkv_w"][l]
    for part in range(3):
        for i, h in enumerate(blk):
            dn_cw[:, part * 2 + i, :] = cq[:, part * 1024 + h * 128: part * 1024 + (h + 1) * 128].T
    dn_p4 = np.zeros((4, 2), np.float32)
    dn_p4[2:4, 0] = inp["dn_dt_bias"][l][blk]
    dn_p4[2:4, 1] = inp["dn_a_log"][l][blk]
    return {"wc": wc, "ngain": pcols(inp["norm_mix"][l]), "lru_p": lru_p, "lru_w": lru_w, "dn_cw": dn_cw,
            "dn_p4": dn_p4, "dn_g": np.ascontiguousarray(inp["dn_out_norm"][l].reshape(128, 1))}

S_FULL = 8192
NTC = 2048
CAP = 1024
FE_ = 3072
FF_ = 6144


def _fm(a):
    T, C = a.shape
    return np.ascontiguousarray(a.T.reshape(C // 128, 128, T))


def _pcols(v):
    return np.ascontiguousarray(v.reshape(-1, 128).T)


_NC_CACHE = {}
LAST_COUNTS = None


def _get(name, fn):
    if name not in _NC_CACHE:
        _NC_CACHE[name] = fn()
    return _NC_CACHE[name]


def kernel(**inp):
    inp = {k: np.asarray(v) for k, v in inp.items()}
    x = inp["x"].astype(np.float32)
    cores = list(range(8))
    cstA = consts_A()
    cstM = consts_M(CAP)
    out = None
    for l in range(2):
        ncA = _get("A", lambda: build_A(S_FULL))
        xTb = [_fm(x[b]) for b in range(2)]
        maps = []
        for c in cores:
            b, j = c // 4, c % 4
            m = {"xT": xTb[b]}
            m.update(prep_A(inp, l, j))
            m.update(cstA)
            maps.append(m)
        resA = run_bass_kernel_spmd(ncA, maps, core_ids=cores)
        yA = [r["yT"] for r in resA.results]
        del maps
        mapsB = []
        for c in cores:
            b, q = c // 4, c % 4
            ts = slice(q * NTC, (q + 1) * NTC)
            ymT = np.empty((16, 128, NTC), np.float32)
            for n in range(8):
                ymT[n] = yA[b * 4 + n // 2][n % 2][:, ts]
                ymT[8 + n] = yA[b * 4 + n // 2][2 + n % 2][:, ts]
            m = {"ymT": ymT, "xT": np.ascontiguousarray(xTb[b][:, :, ts]), "wout": inp["w_out"][l],
                 "lgain": _pcols(inp["lru_out_norm"][l]), "ngain": _pcols(inp["norm_ffn"][l])}
            if l == 0:
                m.update({"wg": inp["ffn_w_gate"][0], "wu": inp["ffn_w_up"][0], "wd": inp["ffn_w_down"][0]})
            else:
                m.update({"ngrow": np.ascontiguousarray(inp["norm_ffn"][l].reshape(1, -1)),
                          "fgrow": np.ascontiguousarray(inp["norm_final"].reshape(1, -1)),
                          "router": inp["moe_router"][0], "wg": inp["moe_w_gate"][0], "wu": inp["moe_w_up"][0],
                          "wd": inp["moe_w_down"][0]})
                m.update(cstM)
            mapsB.append(m)
        del yA
        if l == 0:
            ncB = _get("B", lambda: build_B(NTC, FF_))
            resB = run_bass_kernel_spmd(ncB, mapsB, core_ids=cores)
            xn = np.empty_like(x)
            for c in cores:
                b, q = c // 4, c % 4
                xn[b, q * NTC:(q + 1) * NTC, :] = resB.results[c]["x2T"].reshape(2048, NTC).T
            x = xn
        else:
            ncM = _get("M", lambda: build_M(NTC, FE_, CAP))
            resM = run_bass_kernel_spmd(ncM, mapsB, core_ids=cores)
            out = np.empty((2, S_FULL, 2048), np.float32)
            for c in cores:
                b, q = c // 4, c % 4
                out[b, q * NTC:(q + 1) * NTC, :] = resM.results[c]["out"]
            global LAST_COUNTS
            LAST_COUNTS = np.stack([resM.results[c]["cnt_out"][0] for c in cores])
        del mapsB
    return out
```

```python
import numpy as np
from contextlib import ExitStack
import concourse.bass as bass
import concourse.mybir as mybir
from concourse.bass_utils import run_bass_kernel_spmd


F32 = mybir.dt.float32
BF16 = mybir.dt.bfloat16
I32 = mybir.dt.int32
AF = mybir.ActivationFunctionType
ALU = mybir.AluOpType
AX = mybir.AxisListType

SEM_ROLL = 30000


class Prog:
    ENGS = ("pe", "dve", "act", "pool", "sp")

    def __init__(self, nc, stack):
        self.nc = nc
        self.stack = stack
        self.q = {e: [] for e in self.ENGS}
        self.cnt = {e: 0 for e in self.ENGS}
        self.sem = {}
        self.nsem = 0
        for e in self.ENGS:
            self.sem[e] = self._newsem("e_" + e)
        self.seen = {e: {} for e in self.ENGS}
        self.lastw = {}
        self.readers = {}
        self.dsem = {}
        self.n_ops = 0

    def _newsem(self, name):
        self.nsem += 1
        sm = self.stack.enter_context(self.nc.semaphore(f"{name}_{self.nsem}"))
        if not hasattr(self, "semname"):
            self.semname = {}
        self.semname[id(sm)] = f"{name}_{self.nsem}"
        return sm

    def sb(self, name, shape, dt):
        return self.stack.enter_context(self.nc.sbuf_tensor(name, list(shape), dt))

    def ps(self, name, shape, dt=F32):
        return self.stack.enter_context(self.nc.psum_tensor(name, list(shape), dt))

    def _deps(self, eng, reads, writes):
        need = []
        for k in reads:
            w = self.lastw.get(k)
            if w is not None:
                need.append(w)
        for k in writes:
            w = self.lastw.get(k)
            if w is not None:
                need.append(w)
            need.extend(self.readers.get(k, ()))
        best = {}
        for s, v in need:
            if best.get(id(s), (None, -1))[1] < v:
                best[id(s)] = (s, v)
        out = []
        seen = self.seen[eng]
        for sid, (s, v) in best.items():
            if eng == "pe" and s is self.sem["pe"]:
                continue
            if seen.get(sid, 0) >= v:
                continue
            seen[sid] = v
            out.append((s, v))
        return out

    def _commit(self, reads, writes, tok):
        for k in writes:
            self.lastw[k] = tok
            self.readers[k] = []
        for k in reads:
            self.readers.setdefault(k, []).append(tok)

    def defer_start(self):
        self._defer = []

    def defer_stop(self):
        d = self._defer
        self._defer = None
        return d

    def run_deferred(self, lst, n=None):
        n = len(lst) if n is None else min(n, len(lst))
        for _ in range(n):
            kind, a, kw = lst.pop(0)
            getattr(self, kind)(*a, **kw)

    def op(self, eng, fn, reads=(), writes=()):
        if getattr(self, "_defer", None) is not None:
            self._defer.append(("op", (eng, fn), {"reads": list(reads), "writes": list(writes)}))
            return
        if self.cnt[eng] >= SEM_ROLL:
            self.sem[eng] = self._newsem("e_" + eng)
            self.cnt[eng] = 0
        waits = self._deps(eng, reads, writes)
        self.cnt[eng] += 1
        sem = self.sem[eng]
        tok = (sem, self.cnt[eng])
        self.q[eng].append((fn, waits, sem, 1))
        self._commit(reads, writes, tok)
        self.n_ops += 1
        if getattr(self, "log", None) is not None:
            self.log.append((eng, self.cnt[eng], list(reads), list(writes), [(self.semname.get(id(s), "?"), v) for s, v in waits]))

    def dma(self, eng, out, in_, reads=(), writes=(), key=None, **kw):
        assert eng in ("sp", "pool", "act")
        if getattr(self, "_defer", None) is not None:
            kw2 = dict(kw); kw2.update({"reads": list(reads), "writes": list(writes), "key": key})
            self._defer.append(("dma", (eng, out, in_), kw2))
            return
        if key is None:
            key = ("dma",) + tuple(writes) + tuple(reads)
        if key not in self.dsem:
            self.dsem[key] = [self._newsem("d"), 0]
        ent = self.dsem[key]
        sem = ent[0]
        waits = self._deps(eng, reads, writes)
        if ent[1] > 0 and self.seen[eng].get(id(sem), 0) < ent[1]:
            self.seen[eng][id(sem)] = ent[1]
            waits.append((sem, ent[1]))
        ent[1] += 16
        tok = (sem, ent[1])

        def fn(e, out=out, in_=in_, kw=kw):
            o = out(e) if callable(out) else out
            i = in_(e) if callable(in_) else in_
            return e.dma_start(out=o, in_=i, **kw)
        self.q[eng].append((fn, waits, sem, 16))
        self._commit(reads, writes, tok)
        self.n_ops += 1
        return tok

    def idma(self, out, out_off, in_, in_off, reads=(), writes=(), key=None, bounds=None):
        eng = "pool"
        if key not in self.dsem:
            self.dsem[key] = [self._newsem("d"), 0]
        ent = self.dsem[key]
        sem = ent[0]
        waits = self._deps(eng, reads, writes)
        if ent[1] > 0 and self.seen[eng].get(id(sem), 0) < ent[1]:
            self.seen[eng][id(sem)] = ent[1]
            waits.append((sem, ent[1]))
        ent[1] += 16
        tok = (sem, ent[1])

        def fn(e):
            oo = bass.IndirectOffsetOnAxis(ap=out_off, axis=0) if out_off is not None else None
            io = bass.IndirectOffsetOnAxis(ap=in_off, axis=0) if in_off is not None else None
            bc = None
            if bounds is not None:
                regs = self.__dict__.setdefault("_bregs", {})
                if bounds not in regs:
                    regs[bounds] = e.to_reg(bounds)
                bc = regs[bounds]
            return e.indirect_dma_start(out=out, out_offset=oo, in_=in_, in_offset=io, bounds_check=bc, oob_is_err=False)
        self.q[eng].append((fn, waits, sem, 16))
        self._commit(reads, writes, tok)
        return tok

    def wait_all_dma(self, eng="sp"):
        waits = []
        for key, (sem, val) in self.dsem.items():
            if val > 0:
                waits.append((sem, val))
        self.q[eng].append((None, waits, None, 0))

    def emit(self):
        nc = self.nc
        engmap = {"pe": "tensor", "dve": "vector", "act": "scalar", "pool": "gpsimd", "sp": "sync"}
        with nc.Block() as block:
            for ename in self.ENGS:
                lst = self.q[ename]

                def body(e, lst=lst):
                    for fn, waits, sem, inc in lst:
                        for s, v in waits:
                            e.wait_ge(s, v)
                        if fn is not None:
                            fn(e).then_inc(sem, inc)
                getattr(block, engmap[ename])(body)


EPS = 1e-6
D = 2048
KC = 16
NCOL = 1540
TB = 512
NCH = TB // 64


def consts_A():
    i = np.arange(64)
    ms = (i[:, None] > i[None, :]).astype(np.float32)
    msT = (i[:, None] < i[None, :]).astype(np.float32)
    miT = (i[:, None] <= i[None, :]).astype(np.float32)
    idn = np.eye(64, dtype=np.float32)
    c64 = np.stack([np.tile(m, (1, NCH)) for m in (ms, msT, miT, idn)], 0)
    c64p = np.zeros((4, 128, TB), np.float32)
    c64p[:, :64] = c64
    ident = np.eye(128, dtype=np.float32)
    small = np.zeros((128, 4 + 4 * 128 + TB), np.float32)
    small[:4, 0:4] = np.eye(4)
    for r in range(4):
        small[r, 4 + r * 128: 4 + (r + 1) * 128] = 1.0
    rm = np.ones(TB, np.float32)
    rm[::64] = 0.0
    small[:4, 4 + 512:] = rm[None, :]
    return {"c64": c64p, "ident": ident, "small": small}


HORDER = [0, 1]
INV_BF16 = True
DEBUG = False


def build_A(S):
    nc = bass.Bass("TRN2", target_bir_lowering=False)
    NB = S // TB
    dr = lambda n, s, k="ExternalInput": nc.dram_tensor(n, list(s), F32, kind=k).ap()
    xT = dr("xT", [KC, 128, S])
    wc = dr("wc", [D, NCOL])
    ngain = dr("ngain", [128, KC])
    lru_p = dr("lru_p", [128, 2, 8])
    lru_w = dr("lru_w", [128, 2, 2, 128])
    dn_cw = dr("dn_cw", [128, 6, 4])
    dn_p4 = dr("dn_p4", [4, 2])
    dn_g = dr("dn_g", [128, 1])
    c64 = dr("c64", [4, 128, TB])
    identd = dr("ident", [128, 128])
    smalld = dr("small", [128, 4 + 512 + TB])
    yT = dr("yT", [4, 128, S], "ExternalOutput")
    dbg = dr("dbg", [24, 128, TB], "ExternalOutput") if DEBUG else None

    xv = xT.rearrange("c p t -> p c t")
    wcv = wc.rearrange("(kc p) n -> p kc n", p=128)

    with ExitStack() as st:
        P = Prog(nc, st)
        sb, ps_ = P.sb, P.ps
        W = sb("W", [128, KC, NCOL], BF16)
        ng = sb("ng", [128, KC], F32)
        lp = sb("lp", [128, 2, 8], F32)
        lw32 = sb("lw32", [128, 2, 2, 128], F32)
        lw = sb("lw", [128, 2, 2, 128], BF16)
        c1 = sb("c1", [128, 2], F32)
        dcw = sb("dcw", [128, 6, 4], F32)
        p4 = sb("p4", [4, 2], F32)
        negA = sb("negA", [4, 1], F32)
        dng = sb("dng", [128, 1], F32)
        cm = sb("cm", [128, 4, TB], BF16)
        ident = sb("identf", [128, 128], F32)
        identb = sb("identb", [128, 128], BF16)
        small = sb("smallc", [128, 4 + 512 + TB], F32)
        ones = sb("ones", [128, 128], BF16)
        I4 = small[0:4, 0:4]
        sel = lambda r: small[0:4, 4 + r * 128: 4 + (r + 1) * 128]
        rmask = small[0:4, 4 + 512: 4 + 512 + TB]
        Ms, MsT, MiT, Id8 = cm[0:64, 0, :], cm[0:64, 1, :], cm[0:64, 2, :], cm[0:64, 3, :]

        x32 = [sb(f"x32_{k}", [128, 4, TB], F32) for k in range(2)]
        sq = sb("sq", [128, 4, TB], BF16)
        hT = sb("hT", [128, KC, TB], BF16)
        rs = sb("rs", [128, TB], F32)
        xbuf = [sb(f"xbuf{n}", [128, TB + 3], F32) for n in range(2)]
        hlast = [sb(f"hlast{n}", [128, 1], F32) for n in range(2)]
        gl = [sb(f"gl{n}", [128, TB], F32) for n in range(2)]
        cb = [sb(f"cb{k}", [128, TB + 3], F32) for k in range(6)]
        sgate2 = [[sb(f"sgate{pb_}_{h}", [128, TB], F32) for h in range(2)] for pb_ in range(2)]
        r4 = sb("r4", [4, 5, TB], F32)
        colt = sb("colt", [64, NCH, 16], F32)
        cole = sb("cole", [64, NCH, 2], F32)
        qkv = [sb(f"qkv{k}", [128, TB], F32) for k in range(3)]
        qkvb = [sb(f"qkvb{k}", [128, TB], BF16) for k in range(3)]
        Gb = sb("Gb", [128, TB], F32)
        betab = sb("betab", [64, TB], F32)
        EGb = sb("EGb", [128, TB], F32)
        qdTb = sb("qdTb", [128, TB], BF16)
        t1 = sb("t1", [64, TB], F32)
        eT = sb("eT", [64, TB], F32)
        eLf = sb("eLf", [128, TB], F32)
        eL = eLf[0:64, :]
        IDT = BF16 if INV_BF16 else F32
        Nm = [sb(f"Nm{k}", [64, TB], IDT) for k in range(2)]
        NmT = [sb(f"NmT{k}", [64, TB], IDT) for k in range(2)]
        Pm = sb("Pm", [64, TB], IDT)
        qkT = sb("qkT", [64, TB], BF16)
        vb = sb("vb", [64, NCH, 128], IDT)
        kbg = sb("kbg", [64, NCH, 128], IDT)
        kdec = sb("kdec", [64, NCH, 128], BF16)
        u_t = sb("u_t", [64, NCH, 128], F32)
        wTb = sb("wTb", [128, TB], BF16)
        vnew = sb("vnew", [64, 128], BF16)
        S32 = [sb(f"S32_{h}", [128, 128], F32) for h in range(2)]
        Sb = [sb(f"Sb_{h}", [128, 128], BF16) for h in range(2)]
        o32 = sb("o32", [128, TB], F32)
        yo = [sb(f"yo{k}", [128, TB], F32) for k in range(2)]
        l2r0 = sb("l2r0", [128, TB], F32)
        xc, r_t, i_t = qkv[0], qkv[1], qkv[2]
        xcb = qkvb[0]
        a_t, m_t, h_t = Gb, EGb, o32
        XC, XCB, RT, IT, ATk, MTk, HTk = "qkv0", "qkvb0", "qkv1", "qkv2", "Gb", "EGb", "o32"
        def t1_full(k):
            return l2r0[:] if k == 0 else eLf[:]
        L2K = ["l2r0", "eL"]

        psn = ps_("psn", [128, 512])
        pp = [ps_(f"pp{k}", [128, 512]) for k in range(2)]
        pA = ps_("pA", [128, 512])
        pB = ps_("pB", [128, 512])
        pC = ps_("pC", [128, 512])
        pTk = pC[:].bitcast(BF16)
        pR = ps_("pR", [128, 512])
        pO = ps_("pO", [128, 512])

        P.dma("sp", ng[:], ngain, writes=["ng"])
        P.dma("sp", lp[:], lru_p, writes=["lp"])
        P.dma("sp", lw32[:], lru_w, writes=["lw32"])
        P.dma("sp", dcw[:], dn_cw, writes=["dcw"])
        P.dma("sp", p4[:], dn_p4, writes=["p4"])
        P.dma("sp", dng[:], dn_g, writes=["dng"])
        P.dma("pool", cm[:], c64.rearrange("m p t -> p m t"), writes=["cm"])
        P.dma("sp", ident[:], identd, writes=["ident"])
        P.dma("sp", small[:], smalld, writes=["small"])
        for k4 in range(4):
            c0 = k4 * 385
            P.dma("pool", W[:, :, c0:c0 + 385], wcv[:, :, c0:c0 + 385], writes=[("W", k4)], key=("W", k4))
        Wk = [("W", k4) for k4 in range(4)]
        P.op("dve", lambda e: e.memset(ones[:], 1.0), writes=["ones"])
        P.op("dve", lambda e: e.tensor_copy(out=identb[:], in_=ident[:]), reads=["ident"], writes=["identb"])
        P.op("dve", lambda e: e.tensor_copy(out=lw[:], in_=lw32[:]), reads=["lw32"], writes=["lw"])
        P.op("act", lambda e: e.activation(out=c1[:], in_=lp[:, :, 7], func=AF.Exp, scale=-1.0), reads=["lp"], writes=["c1"])
        P.op("act", lambda e: e.activation(out=c1[:], in_=c1[:], func=AF.Ln, bias=1.0), reads=["c1"], writes=["c1"])
        P.op("dve", lambda e: e.tensor_scalar(out=c1[:], in0=c1[:], scalar1=-8.0, scalar2=None, op0=ALU.mult), reads=["c1"], writes=["c1"])
        P.op("act", lambda e: e.activation(out=negA[:], in_=p4[:, 1:2], func=AF.Exp), reads=["p4"], writes=["negA"])
        P.op("dve", lambda e: e.tensor_scalar(out=negA[:], in0=negA[:], scalar1=-1.0, scalar2=None, op0=ALU.mult), reads=["negA"], writes=["negA"])
        for n in range(2):
            P.op("pool", lambda e, n=n: e.memset(xbuf[n][:, 0:3], 0.0), writes=[f"xbuf{n}"])
            P.op("pool", lambda e, n=n: e.memset(hlast[n][:], 0.0), writes=[f"hlast{n}"])
            P.op("pool", lambda e, n=n: e.memset(S32[n][:], 0.0), writes=[f"S32_{n}"])
            P.op("pool", lambda e, n=n: e.memset(Sb[n][:], 0.0), writes=[f"Sb_{n}"])
        for k in range(6):
            P.op("pool", lambda e, k=k: e.memset(cb[k][:, 0:3], 0.0), writes=[f"cb{k}"])

        def conv(eng, out, buf, wtile, widx, bias, rk, wk, outk):
            if bias is None:
                P.op(eng, lambda e: e.tensor_scalar(out=out, in0=buf[:, 0:TB], scalar1=wtile[:, widx, 0:1], scalar2=None, op0=ALU.mult),
                     reads=[rk, wk], writes=[outk])
            else:
                P.op(eng, lambda e: e.tensor_scalar(out=out, in0=buf[:, 0:TB], scalar1=wtile[:, widx, 0:1], scalar2=bias, op0=ALU.mult, op1=ALU.add),
                     reads=[rk, wk], writes=[outk])
            for k in range(1, 4):
                P.op(eng, lambda e, k=k: e.scalar_tensor_tensor(out=out, in0=buf[:, k:k + TB], scalar=wtile[:, widx, k:k + 1], in1=out,
                                                              op0=ALU.mult, op1=ALU.add),
                     reads=[rk, wk, outk], writes=[outk])

        ycnt = [0]
        dcnt = [0]

        def dump(ap, key, npart=128):
            if not DEBUG:
                return
            slot = dcnt[0]
            dcnt[0] += 1
            P.dma("sp", dbg[slot, 0:npart, :], ap, reads=[key], key=("dbg", slot))

        def store_y(tile_idx, t0, src_key, src):
            P.dma("sp", yT[tile_idx, :, t0:t0 + TB], src, reads=[src_key], key=("yst", src_key))

        pending = []

        def pump(n):
            P.run_deferred(pending, n)

        for blk in range(NB):
            t0 = blk * TB
            def emit_front(blk):
                t0 = blk * TB
                sgate = sgate2[blk % 2]
                SGK = [f"sgate{blk % 2}_{h}" for h in range(2)]
                for q4 in range(4):
                    xb = x32[q4 % 2]
                    xk = f"x32_{q4 % 2}"
                    P.dma("sp", xb[:], xv[:, q4 * 4:(q4 + 1) * 4, t0:t0 + TB], writes=[xk])
                    P.op("act", lambda e, xb=xb: e.activation(out=sq[:], in_=xb[:], func=AF.Square), reads=[xk], writes=["sq"])
                    for j in range(4):
                        jj = q4 * 4 + j
                        P.op("pe", lambda e, j=j, jj=jj: e.matmul(psn[:], ones[:], sq[:, j, :], start=(jj == 0), stop=(jj == KC - 1)),
                             reads=["ones", "sq"], writes=["psn"])
                        P.op("dve", lambda e, j=j, jj=jj, xb=xb: e.tensor_scalar(out=hT[:, jj, :], in0=xb[:, j, :], scalar1=ng[:, jj:jj + 1], scalar2=None, op0=ALU.mult),
                             reads=[xk, "ng"], writes=[("hT", jj)])
                P.op("act", lambda e: e.activation(out=rs[:], in_=psn[:], func=AF.Ln, bias=EPS, scale=1.0 / D), reads=["psn"], writes=["rs"])
                P.op("act", lambda e: e.activation(out=rs[:], in_=rs[:], func=AF.Exp, scale=-0.5), reads=["rs"], writes=["rs"])

                def proj(ct, M):
                    pb = pp[ct % 2]
                    c0 = ct * 128
                    for kc in range(KC):
                        P.op("pe", lambda e, kc=kc: e.matmul(pb[0:M, :], W[:, kc, c0:c0 + M], hT[:, kc, :], start=(kc == 0), stop=(kc == KC - 1)),
                             reads=Wk + [("hT", kc)], writes=[f"pp{ct % 2}"])
                    return pb, f"pp{ct % 2}"

                for n in range(2):
                    pb, pk = proj(n, 128)
                    P.op("dve", lambda e, n=n, pb=pb: e.tensor_tensor(out=xbuf[n][:, 3:3 + TB], in0=pb[:], in1=rs[:], op=ALU.mult),
                         reads=[pk, "rs"], writes=[f"xbuf{n}"])
                for n in range(2):
                    pb, pk = proj(2 + n, 128)
                    P.op("dve", lambda e, n=n, pb=pb: e.tensor_tensor(out=gl[n][:], in0=pb[:], in1=rs[:], op=ALU.mult),
                         reads=[pk, "rs"], writes=[f"gl{n}"])
                    P.op("act", lambda e, n=n: e.activation(out=gl[n][:], in_=gl[n][:], func=AF.Gelu_apprx_tanh), reads=[f"gl{n}"], writes=[f"gl{n}"])
                for k in range(6):
                    pb, pk = proj(4 + k, 128)
                    P.op("dve", lambda e, k=k, pb=pb: e.tensor_tensor(out=cb[k][:, 3:3 + TB], in0=pb[:], in1=rs[:], op=ALU.mult),
                         reads=[pk, "rs"], writes=[f"cb{k}"])
                for h in range(2):
                    pb, pk = proj(10 + h, 128)
                    P.op("dve", lambda e, h=h, pb=pb: e.tensor_tensor(out=sgate[h][:], in0=pb[:], in1=rs[:], op=ALU.mult),
                         reads=[pk, "rs"], writes=[SGK[h]])
                    P.op("act", lambda e, h=h: e.activation(out=sgate[h][:], in_=sgate[h][:], func=AF.Silu), reads=[SGK[h]], writes=[SGK[h]])
                pb, pk = proj(12, 4)
                P.op("dve", lambda e, pb=pb: e.tensor_tensor(out=r4[:, 0, :], in0=pb[0:4, :], in1=rs[0:4, :], op=ALU.mult),
                     reads=[pk, "rs"], writes=["s4"])


            sgate = sgate2[blk % 2]
            SGK = [f"sgate{blk % 2}_{h}" for h in range(2)]
            if blk == 0:
                emit_front(0)
            else:
                P.run_deferred(pending)
            for n in range(2):
                conv("dve", xc[:], xbuf[n], lp, n, lp[:, n, 4:5], f"xbuf{n}", "lp", XC)
                P.op("pool", lambda e, n=n: e.tensor_copy(out=xbuf[n][:, 0:3], in_=xbuf[n][:, TB:TB + 3]), reads=[XC], writes=[f"xbuf{n}"])
                P.op("act", lambda e: e.activation(out=xcb[:], in_=xc[:], func=AF.Copy), reads=[XC], writes=[XCB])
                P.op("pe", lambda e, n=n: e.matmul(pA[:], lw[:, n, 0, :], xcb[:], start=True, stop=True), reads=["lw", XCB], writes=["pA"])
                P.op("pe", lambda e, n=n: e.matmul(pB[:], lw[:, n, 1, :], xcb[:], start=True, stop=True), reads=["lw", XCB], writes=["pB"])
                P.op("act", lambda e, n=n: e.activation(out=r_t[:], in_=pA[:], func=AF.Sigmoid, bias=lp[:, n, 5:6]), reads=["pA", "lp"], writes=[RT])
                P.op("act", lambda e, n=n: e.activation(out=i_t[:], in_=pB[:], func=AF.Sigmoid, bias=lp[:, n, 6:7]), reads=["pB", "lp"], writes=[IT])
                P.op("act", lambda e, n=n: e.activation(out=a_t[:], in_=r_t[:], func=AF.Exp, scale=c1[:, n:n + 1]), reads=[RT, "c1"], writes=[ATk])
                P.op("act", lambda e: e.activation(out=m_t[:], in_=a_t[:], func=AF.Square), reads=[ATk], writes=[MTk])
                P.op("act", lambda e: e.activation(out=m_t[:], in_=m_t[:], func=AF.Sqrt, bias=1.0, scale=-1.0), reads=[MTk], writes=[MTk])
                P.op("dve", lambda e: e.tensor_tensor(out=i_t[:], in0=i_t[:], in1=xc[:], op=ALU.mult), reads=[IT, XC], writes=[IT])
                P.op("dve", lambda e: e.tensor_tensor(out=m_t[:], in0=m_t[:], in1=i_t[:], op=ALU.mult), reads=[MTk, IT], writes=[MTk])
                P.op("dve", lambda e, n=n: e.tensor_tensor_scan(out=h_t[:], data0=a_t[:], data1=m_t[:], initial=hlast[n][:, 0:1],
                                                               op0=ALU.mult, op1=ALU.add),
                     reads=[ATk, MTk, f"hlast{n}"], writes=[HTk])
                P.op("pool", lambda e, n=n: e.tensor_copy(out=hlast[n][:], in_=h_t[:, TB - 1:TB]), reads=[HTk], writes=[f"hlast{n}"])
                yk = ycnt[0] % 2
                ycnt[0] += 1
                P.op("dve", lambda e, n=n, yk=yk: e.tensor_tensor(out=yo[yk][:], in0=h_t[:], in1=gl[n][:], op=ALU.mult),
                     reads=[HTk, f"gl{n}"], writes=[f"yo{yk}"])
                store_y(n, t0, f"yo{yk}", yo[yk][:])

            s4, B4, g4, G4, EG4 = (r4[:, k, :] for k in range(5))
            P.op("act", lambda e: e.activation(out=B4, in_=s4, func=AF.Sigmoid), reads=["s4"], writes=["B4"])
            P.op("act", lambda e: e.activation(out=g4, in_=s4, func=AF.Exp, bias=p4[:, 0:1]), reads=["s4", "p4"], writes=["g4"])
            P.op("act", lambda e: e.activation(out=g4, in_=g4, func=AF.Ln, bias=1.0), reads=["g4"], writes=["g4"])
            P.op("dve", lambda e: e.tensor_scalar(out=g4, in0=g4, scalar1=negA[:, 0:1], scalar2=None, op0=ALU.mult), reads=["g4", "negA"], writes=["g4"])
            P.op("dve", lambda e: e.tensor_tensor_scan(out=G4, data0=rmask, data1=g4, initial=0.0, op0=ALU.mult, op1=ALU.add),
                 reads=["g4", "small"], writes=["G4"])
            P.op("act", lambda e: e.activation(out=EG4, in_=G4, func=AF.Exp), reads=["G4"], writes=["EG4"])
            ED4 = s4
            G4c = G4.rearrange("p (c t) -> p c t", t=64)
            P.op("dve", lambda e: e.tensor_tensor(out=ED4.rearrange("p (c t) -> p c t", t=64), in0=G4c[:, :, 63:64].to_broadcast([4, NCH, 64]),
                                                  in1=G4c, op=ALU.subtract), reads=["G4"], writes=["s4"])
            P.op("act", lambda e: e.activation(out=ED4, in_=ED4, func=AF.Exp), reads=["s4"], writes=["s4"])
            quants = [(B4, "B4"), (G4, "G4"), (EG4, "EG4"), (ED4, "s4")]
            for c in range(NCH):
                for qi, (qt, qk_) in enumerate(quants):
                    P.op("pe", lambda e, c=c, qi=qi, qt=qt: e.matmul(pR[0:64, 256 + c * 16 + qi * 4: 256 + c * 16 + qi * 4 + 4],
                                                                     qt[:, c * 64:(c + 1) * 64], I4, start=True, stop=True),
                         reads=[qk_, "small"], writes=["pR"])
            P.op("dve", lambda e: e.tensor_copy(out=colt[:].rearrange("p c q -> p (c q)"), in_=pR[0:64, 256:256 + NCH * 16]), reads=["pR"], writes=["colt"])
            for h in range(2):
                P.op("dve", lambda e, h=h: e.tensor_tensor(out=cole[:, :, h:h + 1], in0=colt[:, :, h:h + 1], in1=colt[:, :, 10 + h:11 + h], op=ALU.mult),
                     reads=["colt"], writes=[("cole", h)])

            for h in HORDER:
                for k in range(3):
                    conv("dve", qkv[k][:], cb[k * 2 + h], dcw, k * 2 + h, None, f"cb{k * 2 + h}", "dcw", f"qkv{k}")
                    P.op("pool", lambda e, k=k, h=h: e.tensor_copy(out=cb[k * 2 + h][:, 0:3], in_=cb[k * 2 + h][:, TB:TB + 3]),
                         reads=[f"qkv{k}"], writes=[f"cb{k * 2 + h}"])
                    P.op("act", lambda e, k=k: e.activation(out=qkv[k][:], in_=qkv[k][:], func=AF.Silu), reads=[f"qkv{k}"], writes=[f"qkv{k}"])
                for k in range(2):
                    P.op("act", lambda e, k=k: e.activation(out=sq[:, 0, :], in_=qkv[k][:], func=AF.Square), reads=[f"qkv{k}"], writes=["sq"])
                    P.op("pe", lambda e: e.matmul(psn[:], ones[:], sq[:, 0, :], start=True, stop=True), reads=["ones", "sq"], writes=["psn"])
                    P.op("act", lambda e, k=k: e.activation(out=t1_full(k), in_=psn[:], func=AF.Ln, bias=EPS, scale=1.0), reads=["psn"], writes=[L2K[k]])
                    P.op("act", lambda e, k=k: e.activation(out=t1_full(k), in_=t1_full(k), func=AF.Exp, scale=-0.5), reads=[L2K[k]], writes=[L2K[k]])
                    sc = (128.0 ** -0.5) if k == 0 else 1.0
                    P.op("dve", lambda e, k=k, sc=sc: e.scalar_tensor_tensor(out=qkvb[k][:], in0=qkv[k][:], scalar=sc, in1=t1_full(k), op0=ALU.mult, op1=ALU.mult),
                         reads=[f"qkv{k}", L2K[k]], writes=[f"qkvb{k}"])
                    if k == 0:
                        P.op("dve", lambda e: e.tensor_tensor(out=qkv[0][:], in0=qkv[0][:], in1=t1_full(0), op=ALU.mult),
                             reads=["qkv0", "l2r0"], writes=["qkv0"])
                P.op("act", lambda e: e.activation(out=qkvb[2][:], in_=qkv[2][:], func=AF.Copy), reads=["qkv2"], writes=["qkvb2"])
                qnb, knb, vcb = qkvb
                overlap = (h == HORDER[-1]) and (blk + 1 < NB)
                if overlap:
                    P.defer_start()
                    emit_front(blk + 1)
                    pending[:] = P.defer_stop()
                    pump(4 * (1 + 1 + 4 + 4) + 2)
                P.op("pe", lambda e, h=h: e.matmul(pA[:], sel(2 + h), G4, start=True, stop=True), reads=["G4", "small"], writes=["pA"])
                P.op("act", lambda e: e.activation(out=Gb[:], in_=pA[:], func=AF.Copy), reads=["pA"], writes=["Gb"])
                P.op("act", lambda e: e.activation(out=EGb[:], in_=pA[:], func=AF.Exp), reads=["pA"], writes=["EGb"])
                P.op("pe", lambda e, h=h: e.matmul(pB[:], sel(h), B4, start=True, stop=True), reads=["B4", "small"], writes=["pB"])
                P.op("act", lambda e: e.activation(out=betab[:], in_=pB[0:64, :], func=AF.Copy), reads=["pB"], writes=["betab"])
                P.op("dve", lambda e: e.scalar_tensor_tensor(out=qdTb[:], in0=qkv[0][:], scalar=128.0 ** -0.5, in1=EGb[:], op0=ALU.mult, op1=ALU.mult),
                     reads=["qkv0", "EGb"], writes=["qdTb"])
                if blk == 0 and h == HORDER[0]:
                    dump(Gb[:], "Gb"); dump(EGb[:], "EGb"); dump(qkv[0][:], "qkv0"); dump(betab[:], "betab", 64)
                for c in range(NCH):
                    cs = slice(c * 64, (c + 1) * 64)
                    P.op("pe", lambda e, cs=cs: e.matmul(pA[0:64, cs], knb[:, cs], knb[:, cs], start=True, stop=True), reads=["qkvb1"], writes=["pA"])
                for c in range(NCH):
                    cs = slice(c * 64, (c + 1) * 64)
                    P.op("pe", lambda e, cs=cs: e.matmul(pB[0:64, cs], knb[:, cs], qnb[:, cs], start=True, stop=True), reads=["qkvb1", "qkvb0"], writes=["pB"])
                for c in range(NCH):
                    cs = slice(c * 64, (c + 1) * 64)
                    P.op("pe", lambda e, c=c, cs=cs: e.transpose(pTk[0:64, c * 128:(c + 1) * 128], knb[:, cs], identb[:]), reads=["qkvb1", "identb"], writes=["pC"])
                GcolB = colt[:, :, 6 + h:7 + h].to_broadcast([64, NCH, 64])
                bcolB = colt[:, :, h:h + 1].to_broadcast([64, NCH, 64])
                v3 = lambda t: t.rearrange("p (c t) -> p c t", t=64)
                P.op("dve", lambda e, GcolB=GcolB: e.tensor_tensor(out=v3(t1[:]), in0=v3(Gb[0:64, :]), in1=GcolB, op=ALU.subtract), reads=["Gb", "colt"], writes=["t1"])
                P.op("dve", lambda e: e.tensor_scalar(out=eT[:], in0=t1[:], scalar1=0.0, scalar2=None, op0=ALU.min), reads=["t1"], writes=["eT"])
                P.op("act", lambda e: e.activation(out=eT[:], in_=eT[:], func=AF.Exp), reads=["eT"], writes=["eT"])
                P.op("dve", lambda e: e.tensor_scalar(out=eL[:], in0=t1[:], scalar1=0.0, scalar2=None, op0=ALU.max), reads=["t1"], writes=["eL"])
                P.op("act", lambda e: e.activation(out=eL[:], in_=eL[:], func=AF.Exp, scale=-1.0), reads=["eL"], writes=["eL"])
                if blk == 0 and h == HORDER[0]:
                    dump(t1[:], "t1", 64); dump(eL, "eL", 64)
                P.op("dve", lambda e: e.tensor_tensor(out=eL[:], in0=eL[:], in1=Ms, op=ALU.mult), reads=["eL", "cm"], writes=["eL"])
                P.op("dve", lambda e, bcolB=bcolB: e.tensor_tensor(out=v3(eL[:]), in0=v3(eL[:]), in1=bcolB, op=ALU.mult), reads=["eL", "colt"], writes=["eL"])
                P.op("dve", lambda e: e.scalar_tensor_tensor(out=Nm[0][:], in0=pA[0:64, :], scalar=-1.0, in1=eL[:], op0=ALU.mult, op1=ALU.mult),
                     reads=["pA", "eL"], writes=["Nm0"])
                if blk == 0 and h == HORDER[0]:
                    dump(eL, "eL", 64)
                P.op("dve", lambda e: e.tensor_tensor(out=t1[:], in0=eT[:], in1=MiT, op=ALU.mult), reads=["eT", "cm"], writes=["t1"])
                P.op("dve", lambda e: e.tensor_tensor(out=qkT[:], in0=pB[0:64, :], in1=t1[:], op=ALU.mult), reads=["pB", "t1"], writes=["qkT"])
                P.op("dve", lambda e: e.tensor_tensor(out=eT[:], in0=eT[:], in1=MsT, op=ALU.mult), reads=["eT", "cm"], writes=["eT"])
                P.op("dve", lambda e: e.tensor_tensor(out=eT[:], in0=eT[:], in1=betab[:], op=ALU.mult), reads=["eT", "betab"], writes=["eT"])
                P.op("dve", lambda e: e.scalar_tensor_tensor(out=NmT[0][:], in0=pA[0:64, :], scalar=-1.0, in1=eT[:], op0=ALU.mult, op1=ALU.mult),
                     reads=["pA", "eT"], writes=["NmT0"])
                pk3 = pTk[0:64, :].rearrange("p (c d) -> p c d", d=128)
                P.op("dve", lambda e, h=h: e.tensor_tensor(out=kbg[:], in0=pk3, in1=cole[:, :, h:h + 1].to_broadcast([64, NCH, 128]), op=ALU.mult),
                     reads=["pC", ("cole", h)], writes=["kbg"])
                P.op("dve", lambda e, h=h: e.tensor_tensor(out=kdec[:], in0=pk3, in1=colt[:, :, 14 + h:15 + h].to_broadcast([64, NCH, 128]), op=ALU.mult),
                     reads=["pC", "colt"], writes=["kdec"])
                for c in range(NCH):
                    cs = slice(c * 64, (c + 1) * 64)
                    P.op("pe", lambda e, c=c, cs=cs: e.transpose(pTk[0:64, c * 128:(c + 1) * 128], vcb[:, cs], identb[:]), reads=["qkvb2", "identb"], writes=["pC"])
                P.op("dve", lambda e, h=h: e.tensor_tensor(out=vb[:], in0=pk3, in1=colt[:, :, h:h + 1].to_broadcast([64, NCH, 128]), op=ALU.mult),
                     reads=["pC", "colt"], writes=["vb"])
                if blk == 0 and h == HORDER[0]:
                    dump(Nm[0][:], "Nm0", 64); dump(NmT[0][:], "NmT0", 64); dump(vb[:, 0:4, :].rearrange("p c d -> p (c d)"), "vb", 64); dump(kbg[:, 0:4, :].rearrange("p c d -> p (c d)"), "kbg", 64)
                P.op("dve", lambda e: e.tensor_tensor(out=Pm[:], in0=NmT[0][:], in1=Id8, op=ALU.add), reads=["NmT0", "cm"], writes=["Pm"])
                cur = 0
                for lvl in range(1, 6):
                    nxt = 1 - cur
                    for c in range(NCH):
                        cs = slice(c * 64, (c + 1) * 64)
                        P.op("pe", lambda e, cs=cs, cur=cur: e.matmul(pA[0:64, cs], NmT[cur][:, cs], Nm[cur][:, cs], start=True, stop=True),
                             reads=[f"NmT{cur}", f"Nm{cur}"], writes=["pA"])
                    P.op("act", lambda e, nxt=nxt: e.activation(out=Nm[nxt][:], in_=pA[0:64, :], func=AF.Copy), reads=["pA"], writes=[f"Nm{nxt}"])
                    if lvl < 5:
                        for c in range(NCH):
                            cs = slice(c * 64, (c + 1) * 64)
                            P.op("pe", lambda e, cs=cs, cur=cur: e.matmul(pB[0:64, cs], Nm[cur][:, cs], NmT[cur][:, cs], start=True, stop=True),
                                 reads=[f"NmT{cur}", f"Nm{cur}"], writes=["pB"])
                        P.op("act", lambda e, nxt=nxt: e.activation(out=NmT[nxt][:], in_=pB[0:64, :], func=AF.Copy), reads=["pB"], writes=[f"NmT{nxt}"])
                    for c in range(NCH):
                        cs = slice(c * 64, (c + 1) * 64)
                        P.op("pe", lambda e, cs=cs, nxt=nxt: e.matmul(pC[0:64, cs], Nm[nxt][:, cs], Pm[:, cs], start=True, stop=True),
                             reads=[f"Nm{nxt}", "Pm"], writes=["pC"])
                    P.op("dve", lambda e: e.tensor_tensor(out=Pm[:], in0=Pm[:], in1=pC[0:64, :], op=ALU.add), reads=["pC", "Pm"], writes=["Pm"])
                    cur = nxt
                    if overlap:
                        pump(18)
                for half in range(2):
                    for c4 in range(4):
                        c = half * 4 + c4
                        cs = slice(c * 64, (c + 1) * 64)
                        pu = pA if half == 0 else pB
                        P.op("pe", lambda e, c=c, c4=c4, cs=cs, pu=pu: e.matmul(pu[0:64, c4 * 128:(c4 + 1) * 128], Pm[:, cs], vb[:, c, :], start=True, stop=True),
                             reads=["Pm", "vb"], writes=[("pA" if half == 0 else "pB")])
                    pu = pA if half == 0 else pB
                    P.op("act", lambda e, half=half, pu=pu: e.activation(out=u_t[:, half * 4:(half + 1) * 4, :].rearrange("p c d -> p (c d)"), in_=pu[0:64, :], func=AF.Copy),
                         reads=[("pA" if half == 0 else "pB")], writes=[("u", half)])
                for c in range(NCH):
                    cs = slice(c * 64, (c + 1) * 64)
                    P.op("pe", lambda e, c=c, cs=cs: e.matmul(pC[:, cs], kbg[:, c, :], Pm[:, cs], start=True, stop=True), reads=["Pm", "kbg"], writes=["pC"])
                P.op("act", lambda e: e.activation(out=wTb[:], in_=pC[:], func=AF.Copy), reads=["pC"], writes=["wTb"])
                if blk == 0 and h == HORDER[0]:
                    dump(Pm[:], "Pm", 64); dump(u_t[:, 0:4, :].rearrange("p c d -> p (c d)"), ("u", 0), 64)
                Sk, Sbk = f"S32_{h}", f"Sb_{h}"
                for c in range(NCH):
                    cs = slice(c * 64, (c + 1) * 64)
                    P.op("pe", lambda e, cs=cs, h=h: e.matmul(pR[0:64, 0:128], wTb[:, cs], Sb[h][:], start=True, stop=True), reads=["wTb", Sbk], writes=["pR"])
                    P.op("dve", lambda e, c=c: e.tensor_tensor(out=vnew[:], in0=u_t[:, c, :], in1=pR[0:64, 0:128], op=ALU.subtract),
                         reads=["pR", ("u", c // 4)], writes=["vnew"])
                    P.op("pe", lambda e, cs=cs, h=h: e.matmul(pO[:, cs], Sb[h][:], qdTb[:, cs], start=True, stop=False), reads=[Sbk, "qdTb"], writes=["pO"])
                    P.op("pe", lambda e, cs=cs: e.matmul(pO[:, cs], vnew[:], qkT[:, cs], start=False, stop=True), reads=["vnew", "qkT"], writes=["pO"])
                    P.op("pe", lambda e, c=c: e.matmul(pR[:, 128:256], kdec[:, c, :], vnew[:], start=True, stop=True), reads=["vnew", "kdec"], writes=["pR"])
                    P.op("dve", lambda e, h=h, c=c: e.scalar_tensor_tensor(out=S32[h][:], in0=S32[h][:], scalar=EGb[:, c * 64 + 63:c * 64 + 64], in1=pR[:, 128:256],
                                                                          op0=ALU.mult, op1=ALU.add), reads=["pR", Sk, "EGb"], writes=[Sk])
                    P.op("act", lambda e, h=h: e.activation(out=Sb[h][:], in_=S32[h][:], func=AF.Copy), reads=[Sk], writes=[Sbk])
                    if overlap:
                        pump(18)
                P.op("act", lambda e: e.activation(out=o32[:], in_=pO[:], func=AF.Copy), reads=["pO"], writes=["o32"])
                P.op("act", lambda e: e.activation(out=sq[:, 0, :], in_=pO[:], func=AF.Square), reads=["pO"], writes=["sq"])
                P.op("pe", lambda e: e.matmul(psn[:], ones[:], sq[:, 0, :], start=True, stop=True), reads=["ones", "sq"], writes=["psn"])
                P.op("act", lambda e: e.activation(out=t1_full(0), in_=psn[:], func=AF.Ln, bias=EPS, scale=1.0 / 128), reads=["psn"], writes=["l2r0"])
                P.op("act", lambda e: e.activation(out=t1_full(0), in_=t1_full(0), func=AF.Exp, scale=-0.5), reads=["l2r0"], writes=["l2r0"])
                if blk == 0 and h == HORDER[0]:
                    dump(o32[:], "o32")
                yk = ycnt[0] % 2
                ycnt[0] += 1
                P.op("dve", lambda e, yk=yk: e.scalar_tensor_tensor(out=yo[yk][:], in0=o32[:], scalar=dng[:, 0:1], in1=t1_full(0), op0=ALU.mult, op1=ALU.mult),
                     reads=["o32", "dng", "l2r0"], writes=[f"yo{yk}"])
                P.op("dve", lambda e, yk=yk, sg_=sgate[h]: e.tensor_tensor(out=yo[yk][:], in0=yo[yk][:], in1=sg_[:], op=ALU.mult),
                     reads=[f"yo{yk}", SGK[h]], writes=[f"yo{yk}"])
                store_y(2 + h, t0, f"yo{yk}", yo[yk][:])
        P.wait_all_dma("sp")
        P.emit()
    return nc


EPS = 1e-6
D = 2048
KC = 16


def build_B(NT, FF, mode="dense", final_norm=False, PASS=1024):
    nc = bass.Bass("TRN2", target_bir_lowering=False)
    TT = 512
    PASS = min(PASS, NT)
    npass = NT // PASS
    tpp = PASS // TT
    NF = FF // 128
    ymT = nc.dram_tensor("ymT", [KC, 128, NT], F32, kind="ExternalInput").ap()
    xT = nc.dram_tensor("xT", [KC, 128, NT], F32, kind="ExternalInput").ap()
    wout = nc.dram_tensor("wout", [D, D], F32, kind="ExternalInput").ap()
    lgain = nc.dram_tensor("lgain", [128, 8], F32, kind="ExternalInput").ap()
    ngain = nc.dram_tensor("ngain", [128, KC], F32, kind="ExternalInput").ap()
    wg = nc.dram_tensor("wg", [D, FF], F32, kind="ExternalInput").ap()
    wu = nc.dram_tensor("wu", [D, FF], F32, kind="ExternalInput").ap()
    wd = nc.dram_tensor("wd", [FF, D], F32, kind="ExternalInput").ap()
    x2T = nc.dram_tensor("x2T", [KC, 128, NT], F32, kind="ExternalOutput").ap()
    x1s = nc.dram_tensor("x1s", [KC, 128, NT], F32, kind="Internal").ap()

    ymv = ymT.rearrange("c p t -> p c t")
    xv = xT.rearrange("c p t -> p c t")
    x1v = x1s.rearrange("c p t -> p c t")
    woutv = wout.rearrange("(kc p) n -> p kc n", p=128)
    wgv = wg.rearrange("(kc p) n -> p kc n", p=128)
    wuv = wu.rearrange("(kc p) n -> p kc n", p=128)
    wdv = wd.rearrange("(f p) n -> p f n", p=128)

    with ExitStack() as st:
        P = Prog(nc, st)
        ones = P.sb("ones", [128, 128], BF16)
        lg = P.sb("lg", [128, 8], F32)
        ng = P.sb("ng", [128, KC], F32)
        AR = max(24576, NF * PASS // 2)
        arena = P.sb("arena", [128, AR], F32)
        ym32 = arena[:, 0:8192].rearrange("p (c t) -> p c t", c=KC)
        x32 = arena[:, 8192:16384].rearrange("p (c t) -> p c t", c=KC)
        ymb = arena[:, 16384:20480].bitcast(BF16).rearrange("p (c t) -> p c t", c=KC)
        sq = arena[:, 20480:24576].bitcast(BF16).rearrange("p (c t) -> p c t", c=KC)
        actT = arena[:, 0:NF * PASS // 2].bitcast(BF16).rearrange("p (f t) -> p f t", f=NF)
        h2T = P.sb("h2T", [128, KC, PASS], BF16)
        rs = P.sb("rs", [128, TT], F32)
        wo = [P.sb(f"wo{i}", [128, KC, 128], BF16) for i in range(2)]
        wbuf = [P.sb(f"wbuf{i}", [128, 8192], BF16) for i in range(2)]
        wgt = [w[:, 0:4096].rearrange("p (c n) -> p c n", c=KC) for w in wbuf]
        wut = [w[:, 4096:8192].rearrange("p (c n) -> p c n", c=KC) for w in wbuf]
        wdt = [w[:, 0:NF * 128].rearrange("p (f n) -> p f n", f=NF) for w in wbuf]
        dummy = P.sb("dmy", [128, 8], F32)
        sg = [P.sb(f"sg{i}", [128, TT], F32) for i in range(2)]
        xr = [P.sb(f"xr{i}", [128, TT], F32) for i in range(2)]
        xo = [P.sb(f"xo{i}", [128, TT], F32) for i in range(2)]
        ps = [P.ps(f"ps{i}", [128, 512]) for i in range(8)]

        P.op("dve", lambda e: e.memset(ones[:], 1.0), writes=["ones"])
        P.dma("sp", lg[:], lgain, writes=["lg"])
        P.dma("sp", ng[:], ngain, writes=["ng"])

        cnt = {"wo": 0, "w": 0, "wd": 0, "po": 0, "g": 0, "x": 0}
        for ps_i in range(npass):
            for tq in range(tpp):
                t0 = ps_i * PASS + tq * TT
                P.dma("sp", ym32, ymv[:, :, t0:t0 + TT], writes=["ym32"])
                P.dma("sp", x32, xv[:, :, t0:t0 + TT], writes=["x32"] + [("x1", j) for j in range(KC)])
                P.op("act", lambda e: e.activation(out=sq[:, 0:8, :], in_=ym32[:, 0:8, :], func=AF.Square),
                     reads=["ym32"], writes=["sq"])
                for j in range(8):
                    P.op("pe", lambda e, j=j: e.matmul(ps[0][:], ones[:], sq[:, j, :], start=(j == 0), stop=(j == 7)),
                         reads=["ones", "sq"], writes=["ps0"])
                P.op("act", lambda e: e.activation(out=rs[:], in_=ps[0][:], func=AF.Sqrt, bias=EPS, scale=1.0 / 1024),
                     reads=["ps0"], writes=["rs"])
                P.op("dve", lambda e: e.reciprocal(out=rs[:], in_=rs[:]), reads=["rs"], writes=["rs"])
                for j in range(8):
                    P.op("dve", lambda e, j=j: e.scalar_tensor_tensor(out=ymb[:, j, :], in0=ym32[:, j, :], scalar=lg[:, j:j + 1],
                                                                        in1=rs[:], op0=ALU.mult, op1=ALU.mult),
                         reads=["ym32", "lg", "rs"], writes=[("ymb", j)])
                P.op("pool", lambda e: e.tensor_copy(out=ymb[:, 8:16, :], in_=ym32[:, 8:16, :]),
                     reads=["ym32"], writes=[("ymb", j) for j in range(8, 16)])
                for dt in range(KC):
                    b = cnt["wo"] % 2
                    cnt["wo"] += 1
                    P.dma("pool", wo[b][:], woutv[:, :, dt * 128:dt * 128 + 128], writes=[f"wo{b}"])
                    pb = 1 + cnt["po"] % 2
                    cnt["po"] += 1
                    for kc in range(KC):
                        P.op("pe", lambda e, kc=kc, b=b, pb=pb: e.matmul(
                            ps[pb][:], wo[b][:, kc, :], ymb[:, kc, :], start=(kc == 0), stop=(kc == KC - 1)),
                            reads=[f"wo{b}", ("ymb", kc)], writes=[f"ps{pb}"])
                    P.op("dve", lambda e, dt=dt, pb=pb: e.tensor_tensor(out=x32[:, dt, :], in0=x32[:, dt, :], in1=ps[pb][:], op=ALU.add),
                         reads=[f"ps{pb}", "x32"], writes=[("x1", dt)])
                x1keys = [("x1", dt) for dt in range(KC)]
                P.dma("sp", x1v[:, :, t0:t0 + TT], x32, reads=x1keys, writes=["x1s"], key="x1st")
                P.op("act", lambda e: e.activation(out=sq[:], in_=x32[:], func=AF.Square),
                     reads=x1keys, writes=["sq"])
                for j in range(KC):
                    P.op("pe", lambda e, j=j: e.matmul(ps[0][:], ones[:], sq[:, j, :], start=(j == 0), stop=(j == KC - 1)),
                         reads=["ones", "sq"], writes=["ps0"])
                P.op("act", lambda e: e.activation(out=rs[:], in_=ps[0][:], func=AF.Sqrt, bias=EPS, scale=1.0 / D),
                     reads=["ps0"], writes=["rs"])
                P.op("dve", lambda e: e.reciprocal(out=rs[:], in_=rs[:]), reads=["rs"], writes=["rs"])
                for j in range(KC):
                    P.op("dve", lambda e, j=j, tq=tq: e.scalar_tensor_tensor(
                        out=h2T[:, j, tq * TT:(tq + 1) * TT], in0=x32[:, j, :], scalar=ng[:, j:j + 1],
                        in1=rs[:], op0=ALU.mult, op1=ALU.mult),
                        reads=[("x1", j), "ng", "rs"], writes=[("h2T", tq)])
            b1keys = ["ym32", "x32", "sq"] + [("ymb", j) for j in range(KC)] + [("x1", j) for j in range(KC)]
            P.op("pool", lambda e: e.memset(dummy[:], 1.0), writes=b1keys + ["actT"])
            for fc in range(FF // 256):
                b = cnt["w"] % 2
                cnt["w"] += 1
                P.dma("pool", wgt[b], wgv[:, :, fc * 256:(fc + 1) * 256], writes=[f"wbuf{b}"], key=f"wg{b}")
                P.dma("pool", wut[b], wuv[:, :, fc * 256:(fc + 1) * 256], writes=[f"wbufu{b}"], key=f"wu{b}")
                for half in range(2):
                    f = fc * 2 + half
                    for tq in range(tpp):
                        g = cnt["g"] % 2
                        cnt["g"] += 1
                        pg, pu = 3 + g, 5 + g
                        for kc in range(KC):
                            P.op("pe", lambda e, kc=kc, b=b, pg=pg, half=half, tq=tq: e.matmul(
                                ps[pg][:], wgt[b][:, kc, half * 128:(half + 1) * 128], h2T[:, kc, tq * TT:(tq + 1) * TT],
                                start=(kc == 0), stop=(kc == KC - 1)),
                                reads=[f"wbuf{b}", ("h2T", tq)], writes=[f"ps{pg}"])
                        for kc in range(KC):
                            P.op("pe", lambda e, kc=kc, b=b, pu=pu, half=half, tq=tq: e.matmul(
                                ps[pu][:], wut[b][:, kc, half * 128:(half + 1) * 128], h2T[:, kc, tq * TT:(tq + 1) * TT],
                                start=(kc == 0), stop=(kc == KC - 1)),
                                reads=[f"wbufu{b}", ("h2T", tq)], writes=[f"ps{pu}"])
                        P.op("act", lambda e, g=g, pg=pg: e.activation(out=sg[g][:], in_=ps[pg][:], func=AF.Silu),
                             reads=[f"ps{pg}"], writes=[f"sg{g}"])
                        P.op("dve", lambda e, g=g, pu=pu, f=f, tq=tq: e.tensor_tensor(
                            out=actT[:, f, tq * TT:(tq + 1) * TT], in0=sg[g][:], in1=ps[pu][:], op=ALU.mult),
                            reads=[f"sg{g}", f"ps{pu}", "actT"], writes=[("act", f, tq)])
            for dt in range(KC):
                b = cnt["wd"] % 2
                cnt["wd"] += 1
                P.dma("pool", wdt[b], wdv[:, :, dt * 128:(dt + 1) * 128], writes=[f"wbuf{b}", f"wbufu{b}"], key=f"wg{b}")
                for tq in range(tpp):
                    t0 = ps_i * PASS + tq * TT
                    pb = 1 + cnt["po"] % 2
                    cnt["po"] += 1
                    xb = cnt["x"] % 2
                    cnt["x"] += 1
                    P.dma("sp", xr[xb][:], x1v[:, dt, t0:t0 + TT], reads=["x1s"], writes=[f"xr{xb}"])
                    for f in range(NF):
                        P.op("pe", lambda e, f=f, b=b, pb=pb, tq=tq: e.matmul(
                            ps[pb][:], wdt[b][:, f, :], actT[:, f, tq * TT:(tq + 1) * TT],
                            start=(f == 0), stop=(f == NF - 1)),
                            reads=[f"wbuf{b}", ("act", f, tq)], writes=[f"ps{pb}"])
                    P.op("dve", lambda e, xb=xb, pb=pb: e.tensor_tensor(out=xo[xb][:], in0=xr[xb][:], in1=ps[pb][:], op=ALU.add),
                         reads=[f"xr{xb}", f"ps{pb}"], writes=[f"xo{xb}"])
                    P.dma("sp", x2T[dt, :, t0:t0 + TT], xo[xb][:], reads=[f"xo{xb}"], key=f"xo{xb}")
            allact = [("act", f, tq) for f in range(NF) for tq in range(tpp)]
            P.op("pool", lambda e: e.memset(dummy[:], 1.0), writes=allact + b1keys + ["actT"])
        P.wait_all_dma("sp")
        P.emit()
    return nc


EPS = 1e-6
D = 2048
KC = 16
NE = 8


def consts_M(C):
    t = np.arange(128)
    U = (t[:, None] < t[None, :]).astype(np.float32)
    ebase = np.tile((np.arange(NE) * C).astype(np.float32)[None, :], (128, 1))
    return {"Umat": U, "identm": np.eye(128, dtype=np.float32), "ebase": ebase}


def build_M(NT, FE, C):
    nc = bass.Bass("TRN2", target_bir_lowering=False)
    TT = 512
    ntile = NT // TT
    NS = NT // 128
    NF = FE // 128
    NSB = C // 128
    CH = C // 2
    dr = lambda n, s, k="ExternalInput", dt=F32: nc.dram_tensor(n, list(s), dt, kind=k).ap()
    ymT = dr("ymT", [KC, 128, NT])
    xT = dr("xT", [KC, 128, NT])
    wout = dr("wout", [D, D])
    lgain = dr("lgain", [128, 8])
    ngain = dr("ngain", [128, KC])
    ngrow = dr("ngrow", [1, D])
    fgrow = dr("fgrow", [1, D])
    router = dr("router", [D, NE])
    wg = dr("wg", [NE, D, FE])
    wu = dr("wu", [NE, D, FE])
    wd = dr("wd", [NE, FE, D])
    Umat = dr("Umat", [128, 128])
    identm = dr("identm", [128, 128])
    ebased = dr("ebase", [128, NE])
    out = dr("out", [NT, D], "ExternalOutput")
    cnt_out = dr("cnt_out", [128, NE], "ExternalOutput")
    x1tok_s = dr("x1tok_s", [NT, D], "Internal")
    Xe = dr("Xe", [NE * C, D], "Internal", BF16)
    Yd = dr("Yd", [NE * C, D], "Internal")

    ymv = ymT.rearrange("c p t -> p c t")
    xv = xT.rearrange("c p t -> p c t")
    woutv = wout.rearrange("(kc p) n -> p kc n", p=128)

    with ExitStack() as st:
        P = Prog(nc, st)
        sb, ps_ = P.sb, P.ps
        ones = sb("ones", [128, 128], BF16)
        ones32 = sb("ones32", [128, 128], F32)
        U = sb("U", [128, 128], F32)
        ident = sb("ident_sb", [128, 128], F32)
        identb = sb("identb", [128, 128], BF16)
        ebase = sb("ebase_sb", [128, NE], F32)
        lg = sb("lg", [128, 8], F32)
        ng = sb("ng", [128, KC], F32)
        ngb = sb("ngb", [128, D], F32)
        fgb = sb("fgb", [128, D], F32)
        rt = sb("rt", [128, KC, NE], F32)
        gr = sb("gr", [128, KC, NE], F32)
        arena = sb("arena", [128, 24576], F32)
        ym32 = arena[:, 0:8192].rearrange("p (c t) -> p c t", c=KC)
        x32 = arena[:, 8192:16384].rearrange("p (c t) -> p c t", c=KC)
        ymb = arena[:, 16384:20480].bitcast(BF16).rearrange("p (c t) -> p c t", c=KC)
        sq = arena[:, 20480:24576].bitcast(BF16).rearrange("p (c t) -> p c t", c=KC)
        o1 = 8 * C
        XeT = arena[:, 0:o1].bitcast(BF16).rearrange("p (c t) -> p c t", c=KC)
        xrow = [arena[:, o1 + i * 1024: o1 + (i + 1) * 1024].bitcast(BF16) for i in range(2)]
        o2 = o1 + 2048
        o3 = o2 + NF * C // 2
        actT = arena[:, o2:o3].bitcast(BF16).rearrange("p (f t) -> p f t", f=NF)
        ystg = [arena[:, o3 + i * 512: o3 + (i + 1) * 512] for i in range(3)]
        assert o3 + 1536 <= 24576
        cy1 = [arena[:, i * 8192: i * 8192 + 2048] for i in range(2)]
        cy2 = [arena[:, i * 8192 + 2048: i * 8192 + 4096] for i in range(2)]
        cx1 = [arena[:, i * 8192 + 4096: i * 8192 + 6144] for i in range(2)]
        cjunk = arena[:, 16384:18432]
        rs = sb("rs", [128, TT], F32)
        wo = [sb(f"wo{i}", [128, KC, 128], BF16) for i in range(2)]
        wbuf = [sb(f"wbuf{i}", [128, max(8192, NF * 512)], BF16) for i in range(2)]
        wgt = [w[:, 0:4096].rearrange("p (c n) -> p c n", c=KC) for w in wbuf]
        wut = [w[:, 4096:8192].rearrange("p (c n) -> p c n", c=KC) for w in wbuf]
        wdt = [w[:, 0:NF * 512].rearrange("p (f n) -> p f n", f=NF) for w in wbuf]
        dmy = sb("dmy", [128, 8], F32)
        x1tok = sb("x1tok", [128, D], F32)
        h2tok = sb("h2tok", [128, D], BF16)
        sg = [sb(f"sg{i}", [128, CH], F32) for i in range(2)]
        cnt = sb("cnt", [128, NE], F32)
        lgt = sb("lgt", [128, NE], F32)
        lg2 = sb("lg2", [128, NE], F32)
        mk1 = sb("mk1", [128, NE], F32)
        mk2 = sb("mk2", [128, NE], F32)
        mk = sb("mk", [128, NE], F32)
        slot = sb("slot", [128, NE], F32)
        tmp8 = sb("tmp8", [128, NE], F32)
        m12 = sb("m12", [128, 4], F32)
        ssum = sb("ssum", [128, 2], F32)
        rstd = sb("rstd", [128, 1], F32)
        dstf = sb("dstf", [128, 2], F32)
        dst = sb("dst", [128, NS, 2], I32)
        wts = sb("wts", [128, NS, 2], F32)
        ps = [ps_(f"ps{i}", [128, 512]) for i in range(8)]

        P.op("dve", lambda e: e.memset(ones[:], 1.0), writes=["ones"])
        P.op("dve", lambda e: e.memset(ones32[:], 1.0), writes=["ones32"])
        P.op("dve", lambda e: e.memset(cnt[:], 0.0), writes=["cnt"])
        P.dma("sp", lg[:], lgain, writes=["lg"])
        P.dma("sp", ng[:], ngain, writes=["ng"])
        P.dma("sp", U[:], Umat, writes=["U"])
        P.dma("sp", ident[:], identm, writes=["ident"])
        P.dma("sp", ebase[:], ebased, writes=["ebase"])
        P.dma("sp", ngb[:], ngrow.partition_broadcast(128), writes=["ngb"])
        P.dma("sp", fgb[:], fgrow.partition_broadcast(128), writes=["fgb"])
        P.dma("sp", rt[:], router.rearrange("(kc p) e -> p kc e", p=128), writes=["rt"])
        P.op("dve", lambda e: e.tensor_copy(out=identb[:], in_=ident[:]), reads=["ident"], writes=["identb"])
        for kc in range(KC):
            P.op("dve", lambda e, kc=kc: e.tensor_scalar(out=gr[:, kc, :], in0=rt[:, kc, :], scalar1=ng[:, kc:kc + 1], scalar2=None, op0=ALU.mult),
                 reads=["rt", "ng"], writes=["gr"])

        cntr = {"wo": 0, "po": 0}
        b1keys = ["ym32", "x32", "sq"] + [("ymb", j) for j in range(KC)] + [("x1", j) for j in range(KC)]
        for tq in range(ntile):
            t0 = tq * TT
            P.dma("sp", ym32, ymv[:, :, t0:t0 + TT], writes=["ym32"])
            P.dma("sp", x32, xv[:, :, t0:t0 + TT], writes=["x32"] + [("x1", j) for j in range(KC)])
            P.op("act", lambda e: e.activation(out=sq[:, 0:8, :], in_=ym32[:, 0:8, :], func=AF.Square), reads=["ym32"], writes=["sq"])
            for j in range(8):
                P.op("pe", lambda e, j=j: e.matmul(ps[0][:], ones[:], sq[:, j, :], start=(j == 0), stop=(j == 7)), reads=["ones", "sq"], writes=["ps0"])
            P.op("act", lambda e: e.activation(out=rs[:], in_=ps[0][:], func=AF.Sqrt, bias=EPS, scale=1.0 / 1024), reads=["ps0"], writes=["rs"])
            P.op("dve", lambda e: e.reciprocal(out=rs[:], in_=rs[:]), reads=["rs"], writes=["rs"])
            for j in range(8):
                P.op("dve", lambda e, j=j: e.scalar_tensor_tensor(out=ymb[:, j, :], in0=ym32[:, j, :], scalar=lg[:, j:j + 1], in1=rs[:], op0=ALU.mult, op1=ALU.mult),
                     reads=["ym32", "lg", "rs"], writes=[("ymb", j)])
            P.op("pool", lambda e: e.tensor_copy(out=ymb[:, 8:16, :], in_=ym32[:, 8:16, :]), reads=["ym32"], writes=[("ymb", j) for j in range(8, 16)])
            for dt in range(KC):
                b = cntr["wo"] % 2
                cntr["wo"] += 1
                P.dma("pool", wo[b][:], woutv[:, :, dt * 128:dt * 128 + 128], writes=[f"wo{b}"])
                pb = 1 + cntr["po"] % 2
                cntr["po"] += 1
                for kc in range(KC):
                    P.op("pe", lambda e, kc=kc, b=b, pb=pb: e.matmul(ps[pb][:], wo[b][:, kc, :], ymb[:, kc, :], start=(kc == 0), stop=(kc == KC - 1)),
                         reads=[f"wo{b}", ("ymb", kc)], writes=[f"ps{pb}"])
                P.op("dve", lambda e, dt=dt, pb=pb: e.tensor_tensor(out=x32[:, dt, :], in0=x32[:, dt, :], in1=ps[pb][:], op=ALU.add),
                     reads=[f"ps{pb}", "x32"], writes=[("x1", dt)])
            x1keys = [("x1", dt) for dt in range(KC)]
            for s4 in range(4):
                sidx = tq * 4 + s4
                ts = slice(s4 * 128, (s4 + 1) * 128)
                for kc in range(KC):
                    P.op("pe", lambda e, kc=kc, ts=ts: e.matmul(ps[3][:, 0:NE], x32[:, kc, ts], gr[:, kc, :], start=(kc == 0), stop=(kc == KC - 1)),
                         reads=[("x1", kc), "gr"], writes=["ps3"])
                for q in range(4):
                    pb = 4 + q % 2
                    for d4 in range(4):
                        dc = q * 4 + d4
                        P.op("pe", lambda e, dc=dc, d4=d4, ts=ts, pb=pb: e.transpose(ps[pb][:, d4 * 128:(d4 + 1) * 128], x32[:, dc, ts], ident[:]),
                             reads=[("x1", dc), "ident"], writes=[f"ps{pb}"])
                    P.op("act", lambda e, q=q, pb=pb: e.activation(out=x1tok[:, q * 512:(q + 1) * 512], in_=ps[pb][:], func=AF.Copy),
                         reads=[f"ps{pb}"], writes=[("x1tok", q)])
                xtk = [("x1tok", q) for q in range(4)]
                P.dma("sp", x1tok_s[tq * TT + s4 * 128: tq * TT + (s4 + 1) * 128, :], x1tok[:], reads=xtk, writes=["x1tok_s"], key="x1tst")
                P.op("act", lambda e: e.activation(out=h2tok[:], in_=x1tok[:], func=AF.Square, accum_out=ssum[:, 0:1]), reads=xtk, writes=["h2tok", "ssum"])
                P.op("act", lambda e: e.activation(out=rstd[:], in_=ssum[:, 0:1], func=AF.Sqrt, bias=EPS, scale=1.0 / D), reads=["ssum"], writes=["rstd"])
                P.op("dve", lambda e: e.reciprocal(out=rstd[:], in_=rstd[:]), reads=["rstd"], writes=["rstd"])
                P.op("dve", lambda e: e.scalar_tensor_tensor(out=h2tok[:], in0=x1tok[:], scalar=rstd[:, 0:1], in1=ngb[:], op0=ALU.mult, op1=ALU.mult),
                     reads=xtk + ["rstd", "ngb"], writes=["h2tok"])
                P.op("dve", lambda e: e.tensor_scalar(out=lgt[:], in0=ps[3][:, 0:NE], scalar1=rstd[:, 0:1], scalar2=None, op0=ALU.mult), reads=["ps3", "rstd"], writes=["lgt"])
                P.op("dve", lambda e: e.tensor_reduce(out=m12[:, 0:1], in_=lgt[:], axis=AX.X, op=ALU.max), reads=["lgt"], writes=["m1"])
                P.op("dve", lambda e: e.tensor_scalar(out=mk1[:], in0=lgt[:], scalar1=m12[:, 0:1], scalar2=None, op0=ALU.is_equal), reads=["lgt", "m1"], writes=["mk1"])
                P.op("dve", lambda e: e.scalar_tensor_tensor(out=lg2[:], in0=mk1[:], scalar=-1e30, in1=lgt[:], op0=ALU.mult, op1=ALU.add), reads=["mk1", "lgt"], writes=["lg2"])
                P.op("dve", lambda e: e.tensor_reduce(out=m12[:, 1:2], in_=lg2[:], axis=AX.X, op=ALU.max), reads=["lg2"], writes=["m2"])
                P.op("dve", lambda e: e.tensor_scalar(out=mk2[:], in0=lg2[:], scalar1=m12[:, 1:2], scalar2=None, op0=ALU.is_equal), reads=["lg2", "m2"], writes=["mk2"])
                P.op("dve", lambda e: e.tensor_tensor(out=m12[:, 2:3], in0=m12[:, 0:1], in1=m12[:, 1:2], op=ALU.subtract), reads=["m1", "m2"], writes=["md"])
                P.op("act", lambda e, sidx=sidx: e.activation(out=wts[:, sidx, 0:1], in_=m12[:, 2:3], func=AF.Sigmoid), reads=["md"], writes=[("wts", sidx)])
                P.op("dve", lambda e, sidx=sidx: e.tensor_scalar(out=wts[:, sidx, 1:2], in0=wts[:, sidx, 0:1], scalar1=-1.0, scalar2=1.0, op0=ALU.mult, op1=ALU.add),
                     reads=[("wts", sidx)], writes=[("wts2", sidx)])
                P.op("dve", lambda e: e.tensor_tensor(out=mk[:], in0=mk1[:], in1=mk2[:], op=ALU.add), reads=["mk1", "mk2"], writes=["mk"])
                P.op("pe", lambda e: e.matmul(ps[6][:, 0:NE], U[:], mk[:], start=True, stop=True), reads=["U", "mk"], writes=["ps6"])
                P.op("pe", lambda e: e.matmul(ps[6][:, NE:2 * NE], ones32[:], mk[:], start=True, stop=True), reads=["ones32", "mk"], writes=["ps6"])
                P.op("dve", lambda e: e.tensor_tensor(out=slot[:], in0=ps[6][:, 0:NE], in1=cnt[:], op=ALU.add), reads=["ps6", "cnt"], writes=["slot"])
                P.op("dve", lambda e: e.tensor_tensor(out=slot[:], in0=slot[:], in1=ebase[:], op=ALU.add), reads=["slot", "ebase"], writes=["slot"])
                P.op("dve", lambda e: e.tensor_tensor(out=cnt[:], in0=ps[6][:, NE:2 * NE], in1=cnt[:], op=ALU.add), reads=["ps6", "cnt", "slot"], writes=["cnt"])
                for k2, mkk in enumerate((mk1, mk2)):
                    P.op("dve", lambda e, mkk=mkk: e.tensor_tensor(out=tmp8[:], in0=mkk[:], in1=slot[:], op=ALU.mult), reads=["slot", "mk1", "mk2"], writes=["tmp8"])
                    P.op("dve", lambda e, k2=k2: e.tensor_reduce(out=dstf[:, k2:k2 + 1], in_=tmp8[:], axis=AX.X, op=ALU.add), reads=["tmp8"], writes=[("dstf", k2)])
                P.op("dve", lambda e, sidx=sidx: e.tensor_copy(out=dst[:, sidx, :], in_=dstf[:]), reads=[("dstf", 0), ("dstf", 1)], writes=[("dst", sidx)])
                for k2 in range(2):
                    P.idma(Xe, dst[:, sidx, k2:k2 + 1], h2tok[:], None, reads=["h2tok", ("dst", sidx)], writes=["Xe"], key=("xsc", k2), bounds=NE * C - 1)
        P.dma("sp", cnt_out, cnt[:], reads=["cnt"], key="cntout")
        P.op("pool", lambda e: e.memset(dmy[:], 1.0), writes=b1keys + ["earena"])
        ec = {"xr": 0, "w": 0, "g": 0, "y": 0}
        for ex in range(NE):
            for sbk in range(NSB):
                xb = ec["xr"] % 2
                ec["xr"] += 1
                P.dma("sp", xrow[xb], Xe[ex * C + sbk * 128: ex * C + (sbk + 1) * 128, :], reads=["Xe", "earena"], writes=[f"xrow{xb}"])
                for half in range(2):
                    pbT = ps[1 + half][:].bitcast(BF16)
                    for d8 in range(8):
                        dc = half * 8 + d8
                        P.op("pe", lambda e, xb=xb, dc=dc, d8=d8, pbT=pbT: e.transpose(pbT[:, d8 * 128:(d8 + 1) * 128], xrow[xb][:, dc * 128:(dc + 1) * 128], identb[:]),
                             reads=[f"xrow{xb}", "identb"], writes=[f"ps{1 + half}"])
                    P.op("act" if half == 0 else "dve",
                         (lambda e, half=half, sbk=sbk, pbT=pbT: e.activation(out=XeT[:, half * 8:(half + 1) * 8, sbk * 128:(sbk + 1) * 128],
                                                                             in_=pbT.rearrange("p (c t) -> p c t", c=8), func=AF.Copy)) if half == 0 else
                         (lambda e, half=half, sbk=sbk, pbT=pbT: e.tensor_copy(out=XeT[:, half * 8:(half + 1) * 8, sbk * 128:(sbk + 1) * 128],
                                                                              in_=pbT.rearrange("p (c t) -> p c t", c=8))),
                         reads=[f"ps{1 + half}", "earena"], writes=[("XeT", sbk, half)])
            xek = [("XeT", sbk, half) for sbk in range(NSB) for half in range(2)]
            for fc in range(FE // 256):
                b = ec["w"] % 2
                ec["w"] += 1
                P.dma("pool", wgt[b], wg[ex].rearrange("(kc p) n -> p kc n", p=128)[:, :, fc * 256:(fc + 1) * 256], writes=[f"wbuf{b}"], key=f"wg{b}")
                P.dma("pool", wut[b], wu[ex].rearrange("(kc p) n -> p kc n", p=128)[:, :, fc * 256:(fc + 1) * 256], writes=[f"wbufu{b}"], key=f"wu{b}")
                for half in range(2):
                    f = fc * 2 + half
                    for ch in range(2):
                        g = ec["g"] % 2
                        ec["g"] += 1
                        pg, pu = 3 + g, 5 + g
                        cs = slice(ch * CH, (ch + 1) * CH)
                        for kc in range(KC):
                            P.op("pe", lambda e, kc=kc, b=b, pg=pg, half=half, cs=cs: e.matmul(ps[pg][:, 0:CH], wgt[b][:, kc, half * 128:(half + 1) * 128], XeT[:, kc, cs],
                                                                                               start=(kc == 0), stop=(kc == KC - 1)),
                                 reads=[f"wbuf{b}"] + xek, writes=[f"ps{pg}"])
                        for kc in range(KC):
                            P.op("pe", lambda e, kc=kc, b=b, pu=pu, half=half, cs=cs: e.matmul(ps[pu][:, 0:CH], wut[b][:, kc, half * 128:(half + 1) * 128], XeT[:, kc, cs],
                                                                                               start=(kc == 0), stop=(kc == KC - 1)),
                                 reads=[f"wbufu{b}"] + xek, writes=[f"ps{pu}"])
                        P.op("act", lambda e, g=g, pg=pg: e.activation(out=sg[g][:], in_=ps[pg][:, 0:CH], func=AF.Silu), reads=[f"ps{pg}"], writes=[f"sg{g}"])
                        P.op("dve", lambda e, g=g, pu=pu, f=f, cs=cs: e.tensor_tensor(out=actT[:, f, cs], in0=sg[g][:], in1=ps[pu][:, 0:CH], op=ALU.mult),
                             reads=[f"sg{g}", f"ps{pu}", "earena"], writes=[("act", f)])
            actk = [("act", f) for f in range(NF)]
            for dc in range(D // 512):
                b = ec["w"] % 2
                ec["w"] += 1
                P.dma("pool", wdt[b], wd[ex].rearrange("(f p) n -> p f n", p=128)[:, :, dc * 512:(dc + 1) * 512], writes=[f"wbuf{b}", f"wbufu{b}"], key=f"wg{b}")
                for sbk in range(NSB):
                    pb = 1 + cntr["po"] % 2
                    cntr["po"] += 1
                    for f in range(NF):
                        P.op("pe", lambda e, f=f, b=b, pb=pb, sbk=sbk: e.matmul(ps[pb][:, 0:512], actT[:, f, sbk * 128:(sbk + 1) * 128], wdt[b][:, f, :],
                                                                               start=(f == 0), stop=(f == NF - 1)),
                             reads=[f"wbuf{b}"] + actk, writes=[f"ps{pb}"])
                    yb = ec["y"] % 3
                    ec["y"] += 1
                    P.op("act", lambda e, yb=yb, pb=pb: e.activation(out=ystg[yb][:, 0:512], in_=ps[pb][:, 0:512], func=AF.Copy),
                         reads=[f"ps{pb}", "earena"], writes=[f"ystg{yb}"])
                    P.dma("sp", Yd[ex * C + sbk * 128: ex * C + (sbk + 1) * 128, dc * 512:(dc + 1) * 512], ystg[yb][:, 0:512], reads=[f"ystg{yb}"], writes=[("Yd", ex)], key=f"yst{yb}")
        retire = [("XeT", sbk, half) for sbk in range(NSB) for half in range(2)] + [("act", f) for f in range(NF)] + \
                 [f"ystg{i}" for i in range(3)] + ["xrow0", "xrow1", "earena"]
        P.op("pool", lambda e: e.memset(dmy[:], 1.0), writes=retire + ["carena"])
        for sidx in range(NS):
            cb_ = sidx % 2
            P.idma(cy1[cb_], None, Yd, dst[:, sidx, 0:1], reads=[("Yd", ex_) for ex_ in range(NE)] + [("dst", sidx), "carena"], writes=[f"cy1_{cb_}"], key=("g1", cb_), bounds=NE * C - 1)
            P.idma(cy2[cb_], None, Yd, dst[:, sidx, 1:2], reads=[("Yd", ex_) for ex_ in range(NE)] + [("dst", sidx), "carena"], writes=[f"cy2_{cb_}"], key=("g2", cb_), bounds=NE * C - 1)
            P.dma("sp", cx1[cb_], x1tok_s[sidx * 128:(sidx + 1) * 128, :], reads=["x1tok_s", "carena"], writes=[f"cx1_{cb_}"])
            P.op("dve", lambda e, cb_=cb_, sidx=sidx: e.scalar_tensor_tensor(out=cx1[cb_], in0=cy1[cb_], scalar=wts[:, sidx, 0:1], in1=cx1[cb_], op0=ALU.mult, op1=ALU.add),
                 reads=[f"cy1_{cb_}", f"cx1_{cb_}", ("wts", sidx)], writes=[f"cx1_{cb_}"])
            P.op("dve", lambda e, cb_=cb_, sidx=sidx: e.scalar_tensor_tensor(out=cx1[cb_], in0=cy2[cb_], scalar=wts[:, sidx, 1:2], in1=cx1[cb_], op0=ALU.mult, op1=ALU.add),
                 reads=[f"cy2_{cb_}", f"cx1_{cb_}", ("wts2", sidx)], writes=[f"cx1_{cb_}"])
            P.op("act", lambda e, cb_=cb_: e.activation(out=cy1[cb_], in_=cx1[cb_], func=AF.Square, accum_out=ssum[:, 1:2]), reads=[f"cx1_{cb_}"], writes=[f"cy1_{cb_}", "ssum2"])
            P.op("act", lambda e: e.activation(out=rstd[:], in_=ssum[:, 1:2], func=AF.Sqrt, bias=EPS, scale=1.0 / D), reads=["ssum2"], writes=["rstd"])
            P.op("dve", lambda e: e.reciprocal(out=rstd[:], in_=rstd[:]), reads=["rstd"], writes=["rstd"])
            P.op("dve", lambda e, cb_=cb_: e.scalar_tensor_tensor(out=cy2[cb_], in0=cx1[cb_], scalar=rstd[:, 0:1], in1=fgb[:], op0=ALU.mult, op1=ALU.mult),
                 reads=[f"cx1_{cb_}", "rstd", "fgb"], writes=[f"cy2_{cb_}"])
            P.dma("sp", out[sidx * 128:(sidx + 1) * 128, :], cy2[cb_], reads=[f"cy2_{cb_}"], key=("ost", cb_))
        P.wait_all_dma("sp")
        P.emit()
    return nc


def fm(a):
    T, C = a.shape
    return np.ascontiguousarray(a.T.reshape(C // 128, 128, T))

def pcols(v):
    return np.ascontiguousarray(v.reshape(-1, 128).T)

def prep_A(inp, l, j):
    DL = 1024
    w_in = inp["w_in"][l]
    blk = [2 * j, 2 * j + 1]
    cols = []
    for n in blk: cols.append(np.arange(n * 128, (n + 1) * 128))
    for n in blk: cols.append(DL + np.arange(n * 128, (n + 1) * 128))
    for part in range(3):
        for h in blk: cols.append(2 * DL + part * 1024 + np.arange(h * 128, (h + 1) * 128))
    for h in blk: cols.append(2 * DL + 3 * 1024 + np.arange(h * 128, (h + 1) * 128))
    base = 2 * DL + 4 * 1024
    cols.append(np.array([base + blk[0], base + blk[1], base + 8 + blk[0], base + 8 + blk[1]]))
    cols = np.concatenate(cols)
    wc = np.ascontiguousarray(w_in[:, cols])
    lru_p = np.zeros((128, 2, 8), np.float32)
    lru_w = np.zeros((128, 2, 2, 128), np.float32)
    for i, n in enumerate(blk):
        sl = slice(n * 128, (n + 1) * 128)
        lru_p[:, i, 0:4] = inp["conv_lru_w"][l][:, sl].T
        lru_p[:, i, 4] = inp["conv_lru_b"][l][sl]
        lru_p[:, i, 5] = inp["lru_b_r"][l][n]
        lru_p[:, i, 6] = inp["lru_b_i"][l][n]
        lru_p[:, i, 7] = inp["lru_lambda"][l][sl]
        lru_w[:, i, 0, :] = inp["lru_w_r"][l][n]
        lru_w[:, i, 1, :] = inp["lru_w_i"][l][n]
    dn_cw = np.zeros((128, 6, 4), np.float32)
    cq = inp["conv_qkv_w"][l]
    for part in range(3):
        for i, h in enumerate(blk):
            dn_cw[:, part * 2 + i, :] = cq[:, part * 1024 + h * 128: part * 1024 + (h + 1) * 128].T
    dn_p4 = np.zeros((4, 2), np.float32)
    dn_p4[2:4, 0] = inp["dn_dt_bias"][l][blk]
    dn_p4[2:4, 1] = inp["dn_a_log"][l][blk]
    return {"wc": wc, "ngain": pcols(inp["norm_mix"][l]), "lru_p": lru_p, "lru_w": lru_w, "dn_cw": dn_cw,
            "dn_p4": dn_p4, "dn_g": np.ascontiguousarray(inp["dn_out_norm"][l].reshape(128, 1))}

S_FULL = 8192
NTC = 2048
CAP = 1024
FE_ = 3072
FF_ = 6144


def _fm(a):
    T, C = a.shape
    return np.ascontiguousarray(a.T.reshape(C // 128, 128, T))


def _pcols(v):
    return np.ascontiguousarray(v.reshape(-1, 128).T)


_NC_CACHE = {}
LAST_COUNTS = None


def _get(name, fn):
    if name not in _NC_CACHE:
        _NC_CACHE[name] = fn()
    return _NC_CACHE[name]


def kernel(**inp):
    inp = {k: np.asarray(v) for k, v in inp.items()}
    x = inp["x"].astype(np.float32)
    cores = list(range(8))
    cstA = consts_A()
    cstM = consts_M(CAP)
    out = None
    for l in range(2):
        ncA = _get("A", lambda: build_A(S_FULL))
        xTb = [_fm(x[b]) for b in range(2)]
        maps = []
        for c in cores:
            b, j = c // 4, c % 4
            m = {"xT": xTb[b]}
            m.update(prep_A(inp, l, j))
            m.update(cstA)
            maps.append(m)
        resA = run_bass_kernel_spmd(ncA, maps, core_ids=cores)
        yA = [r["yT"] for r in resA.results]
        del maps
        mapsB = []
        for c in cores:
            b, q = c // 4, c % 4
            ts = slice(q * NTC, (q + 1) * NTC)
            ymT = np.empty((16, 128, NTC), np.float32)
            for n in range(8):
                ymT[n] = yA[b * 4 + n // 2][n % 2][:, ts]
                ymT[8 + n] = yA[b * 4 + n // 2][2 + n % 2][:, ts]
            m = {"ymT": ymT, "xT": np.ascontiguousarray(xTb[b][:, :, ts]), "wout": inp["w_out"][l],
                 "lgain": _pcols(inp["lru_out_norm"][l]), "ngain": _pcols(inp["norm_ffn"][l])}
            if l == 0:
                m.update({"wg": inp["ffn_w_gate"][0], "wu": inp["ffn_w_up"][0], "wd": inp["ffn_w_down"][0]})
            else:
                m.update({"ngrow": np.ascontiguousarray(inp["norm_ffn"][l].reshape(1, -1)),
                          "fgrow": np.ascontiguousarray(inp["norm_final"].reshape(1, -1)),
                          "router": inp["moe_router"][0], "wg": inp["moe_w_gate"][0], "wu": inp["moe_w_up"][0],
                          "wd": inp["moe_w_down"][0]})
                m.update(cstM)
            mapsB.append(m)
        del yA
        if l == 0:
            ncB = _get("B", lambda: build_B(NTC, FF_))
            resB = run_bass_kernel_spmd(ncB, mapsB, core_ids=cores)
            xn = np.empty_like(x)
            for c in cores:
                b, q = c // 4, c % 4
                xn[b, q * NTC:(q + 1) * NTC, :] = resB.results[c]["x2T"].reshape(2048, NTC).T
            x = xn
        else:
            ncM = _get("M", lambda: build_M(NTC, FE_, CAP))
            resM = run_bass_kernel_spmd(ncM, mapsB, core_ids=cores)
            out = np.empty((2, S_FULL, 2048), np.float32)
            for c in cores:
                b, q = c // 4, c % 4
                out[b, q * NTC:(q + 1) * NTC, :] = resM.results[c]["out"]
            global LAST_COUNTS
            LAST_COUNTS = np.stack([resM.results[c]["cnt_out"][0] for c in cores])
        del mapsB
    return out
```
